# Optimizing a Trainium2 kernel written in Bass

```python
import math
import jax, jax.numpy as jnp
from jax import lax
import numpy as np


D_MODEL = 1024
BATCH = 2
SEQ = 8192
DEPTH = 1

CHUNK = 64
N_ATTN_HEADS = 8
HEAD_DIM = 64
D_ATTN = N_ATTN_HEADS * HEAD_DIM
D_LRU = D_MODEL - D_ATTN
N_LRU_BLOCKS = 8
LRU_BLOCK = D_LRU // N_LRU_BLOCKS
CONV_WIDTH = 4
LRU_C = 8.0
N_IDX_HEADS = 8
IDX_DIM = 64
TOPK_MAX = 256
Q_BLOCK = 128
NUM_BUCKETS = 32
MAX_DISTANCE = 128
D_FF = 2816
N_MOD = 9
EPS = 1e-6
SPLIT_SIZES = (D_ATTN, D_ATTN, D_ATTN, N_IDX_HEADS * IDX_DIM, IDX_DIM, N_IDX_HEADS, D_LRU, D_LRU)
D_IN = sum(SPLIT_SIZES)

kernel_name = "hymba_dsa_rglru_macaron_block"


def rmsnorm(x, g):
    xf = x.astype(jnp.float32)
    y = xf * lax.rsqrt(jnp.mean(xf * xf, axis=-1, keepdims=True) + EPS)
    return (y * g.astype(jnp.float32)).astype(x.dtype)


def modulate(h, shift, scale):
    return h * (1 + scale[:, None, :]) + shift[:, None, :]


def swiglu(h, w_gate, w_up, w_down):
    return (jax.nn.silu(h @ w_gate) * (h @ w_up)) @ w_down


def t5_bucket(rel):
    nb = NUM_BUCKETS // 2
    max_exact = nb // 2
    ret = jnp.where(rel > 0, nb, 0).astype(jnp.int32)
    n = jnp.abs(rel)
    nf = jnp.maximum(n, 1).astype(jnp.float32)
    large = max_exact + (jnp.log(nf / max_exact) / math.log(MAX_DISTANCE / max_exact)
                         * (nb - max_exact)).astype(jnp.int32)
    large = jnp.minimum(large, nb - 1)
    return ret + jnp.where(n < max_exact, n, large)


def dsa_attention(q, k, v, q_idx, k_idx, w_idx, rel_bias):
    B, S = q.shape[:2]
    topk = min(TOPK_MAX, S // 4)
    nblk = S // Q_BLOCK

    def to_blocks(a):
        return jnp.swapaxes(a.reshape((B, nblk, Q_BLOCK) + a.shape[2:]), 0, 1)

    key_pos = jnp.arange(S, dtype=jnp.int32)
    q_pos = key_pos.reshape(nblk, Q_BLOCK)
    gather = jax.vmap(lambda a, i: a[i])

    def block(args):
        qb, qib, wb, tpos = args
        idx_logits = jnp.einsum('bqhd,bsd->bqhs', qib, k_idx) * IDX_DIM ** -0.5
        score = jnp.einsum('bqhs,bqh->bqs', jax.nn.relu(idx_logits), wb).astype(jnp.float32)
        limit = (tpos // CHUNK + 1) * CHUNK
        admissible = key_pos[None, :] < limit[:, None]
        score = jnp.where(admissible[None], score, -jnp.inf)
        top_val, top_idx = lax.top_k(score, topk)
        valid = jnp.isfinite(top_val)
        kg = gather(k, top_idx)
        vg = gather(v, top_idx)
        logits = jnp.einsum('bqhd,bqkhd->bhqk', qb, kg).astype(jnp.float32) * HEAD_DIM ** -0.5
        bias = rel_bias[t5_bucket(top_idx - tpos[None, :, None])]
        logits = logits + jnp.transpose(bias, (0, 3, 1, 2)).astype(jnp.float32)
        logits = jnp.where(valid[:, None], logits, -jnp.inf)
        p = jax.nn.softmax(logits, axis=-1).astype(vg.dtype)
        return jnp.einsum('bhqk,bqkhd->bqhd', p, vg)

    out = lax.map(block, (to_blocks(q), to_blocks(q_idx), to_blocks(w_idx), q_pos))
    return jnp.swapaxes(out, 0, 1).reshape(B, S, D_ATTN)


def _lin_combine(left, right):
    a_l, b_l = left
    a_r, b_r = right
    return a_l * a_r, a_r * b_l + b_r


def rg_lru_branch(xr, gate, conv_w, conv_b, w_a, b_a, w_i, b_i, lam):
    B, S, _ = xr.shape
    xp = jnp.pad(xr, ((0, 0), (CONV_WIDTH - 1, 0), (0, 0)))
    xc = conv_b + xp[:, 0:S] * conv_w[0]
    for j in range(1, CONV_WIDTH):
        xc = xc + xp[:, j:j + S] * conv_w[j]
    xg = xc.reshape(B, S, N_LRU_BLOCKS, LRU_BLOCK)
    r = jax.nn.sigmoid(jnp.einsum('bsgi,gij->bsgj', xg, w_a).reshape(B, S, D_LRU) + b_a)
    i = jax.nn.sigmoid(jnp.einsum('bsgi,gij->bsgj', xg, w_i).reshape(B, S, D_LRU) + b_i)
    log_a = -LRU_C * r.astype(jnp.float32) * jax.nn.softplus(-lam.astype(jnp.float32))
    a = jnp.exp(log_a)
    mult = jnp.sqrt(-jnp.expm1(2.0 * log_a))
    b = mult * (i * xc).astype(jnp.float32)
    _, h = lax.associative_scan(_lin_combine, (a, b), axis=1)
    return h.astype(xr.dtype) * jax.nn.gelu(gate)


def setup_inputs(seed: int = 0) -> dict:
    key = jax.random.key(seed)
    ks = jax.random.split(key, 40)
    f32 = jnp.float32

    def nrm(k, shape, scale):
        return jax.random.normal(k, shape, f32) * scale

    def gain(k, shape):
        return 1.0 + 0.1 * jax.random.normal(k, shape, f32)

    L, D = DEPTH, D_MODEL
    a0 = jax.random.uniform(ks[20], (L, D_LRU), f32, 0.9, 0.999)
    a_base = a0 ** (1.0 / LRU_C)
    lru_lambda = jnp.log(a_base) - jnp.log1p(-a_base)
    return {
        "x": nrm(ks[0], (BATCH, SEQ, D), 1.0),
        "c": nrm(ks[1], (BATCH, D), 1.0),
        "w_ada": nrm(ks[2], (L, D, N_MOD * D), 0.5 * D ** -0.5),
        "b_ada": nrm(ks[3], (L, N_MOD * D), 0.02),
        "ffn1_pre_g": gain(ks[4], (L, D)),
        "ffn1_post_g": gain(ks[5], (L, D)),
        "ffn1_w_gate": nrm(ks[6], (L, D, D_FF), D ** -0.5),
        "ffn1_w_up": nrm(ks[7], (L, D, D_FF), D ** -0.5),
        "ffn1_w_down": nrm(ks[8], (L, D_FF, D), D_FF ** -0.5),
        "mix_pre_g": gain(ks[9], (L, D)),
        "mix_post_g": gain(ks[10], (L, D)),
        "w_in": nrm(ks[11], (L, D, D_IN), D ** -0.5),
        "rel_bias": nrm(ks[12], (NUM_BUCKETS, N_ATTN_HEADS), 0.5),
        "lru_conv_w": nrm(ks[13], (L, CONV_WIDTH, D_LRU), CONV_WIDTH ** -0.5),
        "lru_conv_b": nrm(ks[14], (L, D_LRU), 0.02),
        "lru_w_a": nrm(ks[15], (L, N_LRU_BLOCKS, LRU_BLOCK, LRU_BLOCK), LRU_BLOCK ** -0.5),
        "lru_b_a": nrm(ks[16], (L, D_LRU), 0.02),
        "lru_w_i": nrm(ks[17], (L, N_LRU_BLOCKS, LRU_BLOCK, LRU_BLOCK), LRU_BLOCK ** -0.5),
        "lru_b_i": nrm(ks[18], (L, D_LRU), 0.02),
        "lru_lambda": lru_lambda,
        "attn_out_g": gain(ks[21], (L, D_ATTN)),
        "lru_out_g": gain(ks[22], (L, D_LRU)),
        "w_out": nrm(ks[23], (L, D, D), D ** -0.5),
        "ffn2_pre_g": gain(ks[24], (L, D)),
        "ffn2_post_g": gain(ks[25], (L, D)),
        "ffn2_w_gate": nrm(ks[26], (L, D, D_FF), D ** -0.5),
        "ffn2_w_up": nrm(ks[27], (L, D, D_FF), D ** -0.5),
        "ffn2_w_down": nrm(ks[28], (L, D_FF, D), D_FF ** -0.5),
    }


def reference(x, c, w_ada, b_ada, ffn1_pre_g, ffn1_post_g, ffn1_w_gate, ffn1_w_up, ffn1_w_down,
              mix_pre_g, mix_post_g, w_in, rel_bias, lru_conv_w, lru_conv_b, lru_w_a, lru_b_a,
              lru_w_i, lru_b_i, lru_lambda, attn_out_g, lru_out_g, w_out,
              ffn2_pre_g, ffn2_post_g, ffn2_w_gate, ffn2_w_up, ffn2_w_down):
    B, S, D = x.shape
    offsets = np.cumsum(SPLIT_SIZES)[:-1].tolist()
    for l in range(DEPTH):
        mod = (jax.nn.silu(c) @ w_ada[l] + b_ada[l]).reshape(B, N_MOD, D)
        m = [mod[:, j] for j in range(N_MOD)]

        h = modulate(rmsnorm(x, ffn1_pre_g[l]), m[0], m[1])
        y = swiglu(h, ffn1_w_gate[l], ffn1_w_up[l], ffn1_w_down[l])
        x = x + 0.5 * m[2][:, None, :] * rmsnorm(y, ffn1_post_g[l])

        h = modulate(rmsnorm(x, mix_pre_g[l]), m[3], m[4])
        proj = h @ w_in[l]
        q, k, v, q_idx, k_idx, w_idx, x_lru, g_lru = jnp.split(proj, offsets, axis=-1)
        q = q.reshape(B, S, N_ATTN_HEADS, HEAD_DIM)
        k = k.reshape(B, S, N_ATTN_HEADS, HEAD_DIM)
        v = v.reshape(B, S, N_ATTN_HEADS, HEAD_DIM)
        q_idx = q_idx.reshape(B, S, N_IDX_HEADS, IDX_DIM)
        w_idx = w_idx * N_IDX_HEADS ** -0.5
        attn = dsa_attention(q, k, v, q_idx, k_idx, w_idx, rel_bias)
        lru = rg_lru_branch(x_lru, g_lru, lru_conv_w[l], lru_conv_b[l], lru_w_a[l], lru_b_a[l],
                            lru_w_i[l], lru_b_i[l], lru_lambda[l])
        merged = jnp.concatenate([rmsnorm(attn, attn_out_g[l]), rmsnorm(lru, lru_out_g[l])], axis=-1)
        y = merged @ w_out[l]
        x = x + m[5][:, None, :] * rmsnorm(y, mix_post_g[l])

        h = modulate(rmsnorm(x, ffn2_pre_g[l]), m[6], m[7])
        y = swiglu(h, ffn2_w_gate[l], ffn2_w_up[l], ffn2_w_down[l])
        x = x + 0.5 * m[8][:, None, :] * rmsnorm(y, ffn2_post_g[l])
    return x
```

```python
import math
import numpy as np
import ml_dtypes
from contextlib import ExitStack
import concourse.bass as bass
import concourse.mybir as mybir
from concourse.bass_utils import run_bass_kernel_spmd

F32 = mybir.dt.float32
BF16 = mybir.dt.bfloat16
ALU = mybir.AluOpType
AF = mybir.ActivationFunctionType
AX = mybir.AxisListType

D = 1024
S = 8192
DFF = 2816
NF = DFF // 128
DIN = 3144
NBLK = 16
EPS = 1e-6
NBIS = 16
BIG = 3.0e38

V_C = 0
V_BADA = 8
V_G = 80
V_AOG = 128
V_LOG = 132
V_CW = 136
V_CB = 152
V_BA = 156
V_BI = 160
V_LAM = 164
V_RB = 168
V_P2 = 424
V_SEL = 440
V_SD = 444
V_SP = 449
NV = 456


class Tok:
    __slots__ = ("w", "r")

    def __init__(self):
        self.w = None
        self.r = []


class Prog:
    COMPUTE = ("pe", "act", "dve", "pool")
    NDMASEM = 24

    def __init__(self, nc, stack):
        self.nc = nc
        self.streams = {k: [] for k in ("pe", "act", "dve", "pool", "sp")}
        self.count = {k: 0 for k in self.COMPUTE}
        self.sems = {k: stack.enter_context(nc.semaphore("s_" + k)) for k in self.COMPUTE}
        self.dsems = [stack.enter_context(nc.semaphore("d%d" % i)) for i in range(self.NDMASEM)]
        self.dcount = [0] * self.NDMASEM
        self.dnext = 0
        self.waited = {k: {} for k in self.streams}
        self.ninstr = 0

    def _deps(self, stream, reads, writes):
        out = {}

        def add(kv):
            k, v = kv
            if k == stream and k == "pe":
                return
            if out.get(k, 0) < v:
                out[k] = v
        for t in reads:
            if t.w is not None:
                add(t.w)
        for t in writes:
            if t.w is not None:
                add(t.w)
            for kv in t.r:
                if kv[0] == stream and not isinstance(kv[0], int):
                    continue
                add(kv)
        waits = []
        wd = self.waited[stream]
        for k, v in out.items():
            if wd.get(k, 0) >= v:
                continue
            wd[k] = v
            waits.append((k, v))
        return waits

    def _semof(self, k):
        return self.dsems[k] if isinstance(k, int) else self.sems[k]

    def op(self, eng, fn, reads=(), writes=()):
        waits = self._deps(eng, reads, writes)
        self.count[eng] += 1
        idx = self.count[eng]
        self.streams[eng].append((waits, fn, ("c", eng)))
        for t in reads:
            t.r.append((eng, idx))
            if len(t.r) > 600:
                t.r = t.r[-400:]
        for t in writes:
            t.w = (eng, idx)
            t.r = []
        self.ninstr += 1

    def dma(self, q, fn, reads=(), writes=()):
        s = self.dnext
        self.dnext = (self.dnext + 1) % self.NDMASEM
        waits = self._deps(q, reads, writes)
        prev = self.dcount[s]
        if prev > 0 and self.waited[q].get(s, 0) < prev:
            self.waited[q][s] = prev
            waits.append((s, prev))
        self.dcount[s] += 16
        val = self.dcount[s]
        self.streams[q].append((waits, fn, ("d", s)))
        for t in reads:
            t.r.append((s, val))
        for t in writes:
            t.w = (s, val)
            t.r = []
        self.ninstr += 1

    def barrier(self):
        for st in self.streams:
            waits = []
            for k in self.COMPUTE:
                v = self.count[k]
                if v > 0 and self.waited[st].get(k, 0) < v and k != st:
                    self.waited[st][k] = v
                    waits.append((k, v))
            for s in range(self.NDMASEM):
                v = self.dcount[s]
                if v > 0 and self.waited[st].get(s, 0) < v:
                    self.waited[st][s] = v
                    waits.append((s, v))
            if waits:
                self.streams[st].append((waits, None, None))

    def emit(self, block):
        prog = self

        def run(stream, h):
            for waits, fn, kind in prog.streams[stream]:
                for (k, v) in waits:
                    h.wait_ge(prog._semof(k), v)
                if fn is None:
                    continue
                ins = fn(h)
                if kind[0] == "c":
                    ins.then_inc(prog.sems[kind[1]], 1)
                else:
                    ins.then_inc(prog.dsems[kind[1]], 16)

        @block.tensor
        def _(e):
            run("pe", e)

        @block.scalar
        def _(e):
            run("act", e)

        @block.vector
        def _(e):
            run("dve", e)

        @block.gpsimd
        def _(e):
            run("pool", e)

        @block.sync
        def _(e):
            run("sp", e)


def build(NA=16, stop=None, debug=False):
    nc = bass.Bass("TRN2", target_bir_lowering=False)
    st = ExitStack()
    P = Prog(nc, st)

    def dram_in(name, shape, dt=F32):
        return nc.dram_tensor(name, list(shape), dt, kind="ExternalInput").ap()

    def dram_tmp(name, shape, dt):
        if debug and name in ("Kd", "Vd", "KId", "x1d", "qd", "qid", "mgd"):
            return nc.dram_tensor(name, list(shape), dt, kind="ExternalOutput").ap()
        return nc.dram_tensor(name, list(shape), dt).ap()

    xT = dram_in("xT", [D, S])
    vecs_d = dram_in("vecs", [128, NV])
    w_ada = dram_in("w_ada", [D, 9 * D])
    wg1 = dram_in("wg1", [D, DFF]); wu1 = dram_in("wu1", [D, DFF]); wd1 = dram_in("wd1", [DFF, D])
    wg2 = dram_in("wg2", [D, DFF]); wu2 = dram_in("wu2", [D, DFF]); wd2 = dram_in("wd2", [DFF, D])
    w_in = dram_in("w_in", [D, DIN])
    w_out = dram_in("w_out", [D, D])
    wblk_d = dram_in("wblk", [128, 2, 4, 128])
    mb_d = dram_in("mb", [128, 2, 32, 128], BF16)
    ident_d = dram_in("ident", [128, 128], BF16)
    posm_d = dram_in("posm", [128, 4, 128])
    outT = nc.dram_tensor("outT", [D, NBLK * 128], F32, kind="ExternalOutput").ap()

    wg_t = [dram_tmp("wg_t%d" % i, [NF, 128, 8, 128], BF16) for i in range(2)]
    wu_t = [dram_tmp("wu_t%d" % i, [NF, 128, 8, 128], BF16) for i in range(2)]
    wd_t = [dram_tmp("wd_t%d" % i, [8, 128, NF, 128], BF16) for i in range(2)]
    win_t = dram_tmp("win_t", [16, 128, 8, 128], BF16)
    wout_t = dram_tmp("wout_t", [8, 128, 8, 128], BF16)
    Kd = dram_tmp("Kd", [128, 4, S], BF16)
    Vd = dram_tmp("Vd", [S, 520], BF16)
    KId = dram_tmp("KId", [64, S], F32)
    x1d = dram_tmp("x1d", [128, 8, NBLK * 128], F32)
    qd = dram_tmp("qd", [128, 4, NBLK * 128], BF16)
    qid = dram_tmp("qid", [64, NBLK, 8, 128], F32)
    mgd = dram_tmp("mgd", [128, 8, NBLK * 128], BF16)

    AW = 52600
    arena = st.enter_context(nc.sbuf_tensor("arena", [128, AW], F32))
    cur = [0]

    def alloc(words):
        a = cur[0]
        cur[0] += words
        assert cur[0] <= AW, ("sbuf overflow", cur[0])
        return a

    def f32v(off, n):
        return arena[:, off:off + n]

    def bf16v(off, nbf):
        return arena[:, off:off + nbf // 2].bitcast(BF16)

    vecs = f32v(alloc(NV), NV)
    der = f32v(alloc(128), 128)
    modT = f32v(alloc(72), 72)
    wabs = f32v(alloc(NBLK * 8), NBLK * 8)
    wsgn = f32v(alloc(NBLK * 8), NBLK * 8)
    identb = bf16v(alloc(64), 128)
    onesb = bf16v(alloc(64), 128)
    Tb = f32v(alloc(2048), 2048).rearrange("p (t h q) -> p t h q", t=2, h=8)
    lstate = f32v(alloc(4), 4)
    base_mark = cur[0]

    D_SC = [0, 16, 32]
    D_SH = [8, 24, 40]
    D_GT = [48, 56, 64]
    D_CA = 72
    D_MISC = 76

    psb = [st.enter_context(nc.psum_tensor("ps%d" % i, [128, 512], F32)) for i in range(8)]
    pst = [Tok() for _ in range(8)]

    tvec, tder, tmod = Tok(), Tok(), Tok()

    def mm(out, lhsT, rhs, start, stop, reads, writes):
        P.op("pe", lambda e, o=out, l=lhsT, r=rhs, s=start, t=stop:
             e.matmul(o, lhsT=l, rhs=r, start=s, stop=t), reads, writes)

    def tr(out, in_, reads, writes):
        P.op("pe", lambda e, o=out, i=in_: e.transpose(o, i, identb), reads, writes)

    def act(out, in_, func, reads, writes, bias=0.0, scale=1.0, accum=None):
        if accum is None:
            P.op("act", lambda e, o=out, i=in_, f=func, b=bias, s=scale:
                 e.activation(out=o, in_=i, func=f, bias=b, scale=s), reads, writes)
        else:
            P.op("act", lambda e, o=out, i=in_, f=func, b=bias, s=scale, a=accum:
                 e.activation(out=o, in_=i, func=f, bias=b, scale=s, accum_out=a), reads, writes)

    def tt(eng, out, in0, in1, op, reads, writes):
        P.op(eng, lambda e, o=out, a=in0, b=in1, p=op: e.tensor_tensor(out=o, in0=a, in1=b, op=p),
             reads, writes)

    def ts(eng, out, in0, s1, s2, op0, op1, reads, writes, accum=None):
        if op1 is None:
            op1 = ALU.bypass
        if accum is None:
            P.op(eng, lambda e, o=out, a=in0, x=s1, y=s2, p=op0, q=op1:
                 e.tensor_scalar(out=o, in0=a, scalar1=x, scalar2=y, op0=p, op1=q), reads, writes)
        else:
            P.op(eng, lambda e, o=out, a=in0, x=s1, y=s2, p=op0, q=op1, ac=accum:
                 e.tensor_scalar(out=o, in0=a, scalar1=x, scalar2=y, op0=p, op1=q, accum_out=ac),
                 reads, writes)

    def stt(out, in0, scalar, in1, op0, op1, reads, writes):
        P.op("dve", lambda e, o=out, a=in0, s=scalar, b=in1, p=op0, q=op1:
             e.scalar_tensor_tensor(out=o, in0=a, scalar=s, in1=b, op0=p, op1=q), reads, writes)

    def cp(eng, out, in_, reads, writes):
        if eng == "act":
            P.op("act", lambda e, o=out, i=in_: e.copy(out=o, in_=i), reads, writes)
        else:
            P.op(eng, lambda e, o=out, i=in_: e.tensor_copy(out=o, in_=i), reads, writes)

    def dma(q, out, in_, reads, writes):
        P.dma(q, lambda e, o=out, i=in_: e.dma_start(out=o, in_=i), reads, writes)

    dma("sp", vecs, vecs_d, [], [tvec])
    dma("sp", identb, ident_d, [], [tvec])
    P.op("dve", lambda e: e.memset(onesb, 1.0), [], [tvec])

    tw = {}

    def cast_w(name, dst, src_view, n):
        t = Tok()
        tw[name] = t
        for i in range(n):
            dma("pool", dst[i], src_view[i], [], [t])

    def cast_ffn(i, wg, wu, wd):
        cast_w("wg%d" % i, wg_t[i], wg.rearrange("(kc p) (f c) -> f p kc c", p=128, c=128), NF)
        cast_w("wu%d" % i, wu_t[i], wu.rearrange("(kc p) (f c) -> f p kc c", p=128, c=128), NF)
        cast_w("wd%d" % i, wd_t[i], wd.rearrange("(f p) (dc c) -> dc p f c", p=128, c=128), 8)

    cast_ffn(0, wg1, wu1, wd1)
    win_cols = [0, 128, 256, 384, 512, 640, 768, 896, 2120, 2248, 2376, 2504, 2632, 2760, 2888, 3016]
    t = Tok(); tw["win"] = t
    for i, c0 in enumerate(win_cols):
        dma("pool", win_t[i], w_in[:, c0:c0 + 128].rearrange("(kc p) c -> p kc c", p=128), [], [t])

    scv = f32v(alloc(8), 8)
    tsc = Tok()
    act(scv, vecs[:, V_C:V_C + 8], AF.Silu, [tvec], [tsc])
    wa_off = alloc(2 * 8 * 1152)
    wa_buf = [f32v(wa_off + i * 9216, 9216).rearrange("p (k c) -> p k c", k=8) for i in range(2)]
    twa = [Tok(), Tok()]
    w_ada_v = w_ada.rearrange("(kc p) n -> p kc n", p=128)
    for cb in range(8):
        buf = wa_buf[cb % 2]
        dma("sp", buf, w_ada_v[:, :, cb * 1152:(cb + 1) * 1152], [], [twa[cb % 2]])
        for cc in range(9):
            col = cb * 9 + cc
            for kc in range(8):
                mm(psb[0][:, col:col + 1], buf[:, kc, cc * 128:(cc + 1) * 128], scv[:, kc:kc + 1],
                   kc == 0, kc == 7, [twa[cb % 2], tsc], [pst[0]])
    tt("dve", modT, psb[0][:, 0:72], vecs[:, V_BADA:V_BADA + 72], ALU.add, [pst[0], tvec], [tmod])
    for i, (jsh, jsc, jg, gpre, gpost, gmul) in enumerate(
            [(0, 1, 2, 0, 1, 0.5), (3, 4, 5, 2, 3, 1.0), (6, 7, 8, 4, 5, 0.5)]):
        stt(der[:, D_SC[i]:D_SC[i] + 8], modT[:, jsc * 8:jsc * 8 + 8], 1.0,
            vecs[:, V_G + gpre * 8:V_G + gpre * 8 + 8], ALU.add, ALU.mult, [tmod, tvec], [tder])
        cp("dve", der[:, D_SH[i]:D_SH[i] + 8], modT[:, jsh * 8:jsh * 8 + 8], [tmod], [tder])
        stt(der[:, D_GT[i]:D_GT[i] + 8], modT[:, jg * 8:jg * 8 + 8], gmul,
            vecs[:, V_G + gpost * 8:V_G + gpost * 8 + 8], ALU.mult, ALU.mult, [tmod, tvec], [tder])
    tmp4 = der[:, D_MISC:D_MISC + 4]
    act(tmp4, vecs[:, V_LAM:V_LAM + 4], AF.Exp, [tvec], [tder], scale=-1.0)
    act(tmp4, tmp4, AF.Ln, [tder], [tder], bias=1.0)
    ts("dve", der[:, D_CA:D_CA + 4], tmp4, -8.0, None, ALU.mult, None, [tder], [tder])
    P.op("dve", lambda e: e.memset(lstate, 0.0), [], [tder])

    mb_off = alloc(2 * 32 * 128 // 2)
    mbv = bf16v(mb_off, 2 * 32 * 128).rearrange("p (t b q) -> p t b q", t=2, b=32)
    tmb, tTb = Tok(), Tok()
    dma("sp", mbv, mb_d, [], [tmb])
    for ty in range(2):
        for h in range(8):
            T = Tb[:, ty, h, :]
            ts("dve", T, mbv[:, ty, 0, :], vecs[:, V_RB + h:V_RB + h + 1],
               vecs[:, V_RB + 15 * 8 + h:V_RB + 15 * 8 + h + 1], ALU.mult, ALU.subtract,
               [tmb, tvec], [tTb])
            for b in range(1, 32):
                stt(T, mbv[:, ty, b, :], vecs[:, V_RB + b * 8 + h:V_RB + b * 8 + h + 1], T,
                    ALU.mult, ALU.add, [tmb, tvec, tTb], [tTb])
            ts("dve", T, T, 8.0, None, ALU.mult, None, [tTb], [tTb])
    cast_ffn(1, wg2, wu2, wd2)
    cast_w("wout", wout_t, w_out.rearrange("(kc p) (dc c) -> dc p kc c", p=128, c=128), 8)
    P.barrier()
    cur[0] = base_mark


    def finish():
        P.barrier()
        with nc.Block() as block:
            P.emit(block)
        st.close()
        return nc
    if stop == 'pro':
        return finish()
    def norm_stats(src, nk, N, sq, tsrc, tsq, rstd, trstd, ps_i, dscale):
        act(sq, src, AF.Square, [tsrc], [tsq])
        for k in range(nk):
            mm(psb[ps_i][:, 0:N], onesb, sq[:, k, :], k == 0, k == nk - 1, [tsq, tvec], [pst[ps_i]])
        act(rstd, psb[ps_i][:, 0:N], AF.Sqrt, [pst[ps_i]], [trstd], bias=EPS, scale=dscale)
        P.op("dve", lambda e, o=rstd: e.reciprocal(out=o, in_=o), [trstd], [trstd])

    def ffn(i, N, hb, thb, actb, tact, wslots, dslots, ybuf, tyb, sgb, tsg, bg=None):
        def ld(f):
            s = wslots[f % 3]
            dma("sp", s[0], wg_t[i][f], [tw["wg%d" % i]], [s[2]])
            dma("sp", s[1], wu_t[i][f], [tw["wu%d" % i]], [s[2]])
        ld(0); ld(1)
        for f in range(NF):
            if f + 2 < NF:
                ld(f + 2)
            s = wslots[f % 3]
            pg, pu = (1, 2) if f % 2 == 0 else (3, 4)
            for kc in range(8):
                mm(psb[pg][:, 0:N], s[0][:, kc, :], hb[:, kc, :], kc == 0, kc == 7, [s[2], thb], [pst[pg]])
            for kc in range(8):
                mm(psb[pu][:, 0:N], s[1][:, kc, :], hb[:, kc, :], kc == 0, kc == 7, [s[2], thb], [pst[pu]])
            sg = sgb[f % 2]
            act(sg, psb[pg][:, 0:N], AF.Silu, [pst[pg]], [tsg[f % 2]])
            tt("dve", actb[:, f, :], sg, psb[pu][:, 0:N], ALU.mult, [tsg[f % 2], pst[pu]], [tact[f]])
            if bg is not None:
                for _ in range(2):
                    next(bg, None)
        if bg is not None:
            for _ in bg:
                pass

        def ldd(dc):
            s = dslots[dc % 2]
            dma("sp", s[0], wd_t[i][dc], [tw["wd%d" % i]], [s[1]])
        ldd(0)
        for dc in range(8):
            if dc + 1 < 8:
                ldd(dc + 1)
            s = dslots[dc % 2]
            pa = 5 + dc % 2
            for f in range(NF):
                mm(psb[pa][:, 0:N], s[0][:, f, :], actb[:, f, :], f == 0, f == NF - 1,
                   [s[1], tact[f]], [pst[pa]])
            cp("act" if dc % 2 == 0 else "dve", ybuf[:, dc, :], psb[pa][:, 0:N], [pst[pa]], [tyb])

    N = 512
    xt = f32v(alloc(8 * N), 8 * N).rearrange("p (k n) -> p k n", k=8)
    hb = bf16v(alloc(4 * N), 8 * N).rearrange("p (k n) -> p k n", k=8)
    sq = bf16v(alloc(4 * N), 8 * N).rearrange("p (k n) -> p k n", k=8)
    actb = bf16v(alloc(NF * N // 2), NF * N).rearrange("p (f n) -> p f n", f=NF)
    yoff = alloc(8 * N)
    ybuf = f32v(yoff, 8 * N).rearrange("p (k n) -> p k n", k=8)
    ltoff = alloc(6 * N)
    lt = [f32v(ltoff + i * N, N) for i in range(6)]
    wqi = f32v(yoff, 8 * 512).rearrange("p (k c) -> p k c", k=8)
    h2 = f32v(alloc(8 * N), 8 * N).rearrange("p (k n) -> p k n", k=8)
    rstd = f32v(alloc(N), N)
    sgb = [f32v(alloc(N), N) for _ in range(2)]
    tsg = [Tok(), Tok()]
    wslots = []
    for i in range(3):
        o = alloc(1024)
        wslots.append((bf16v(o, 1024).rearrange("p (k c) -> p k c", k=8),
                       bf16v(o + 512, 1024).rearrange("p (k c) -> p k c", k=8), Tok()))
    dslots = []
    for i in range(2):
        o = alloc(NF * 64)
        dslots.append((bf16v(o, NF * 128).rearrange("p (f c) -> p f c", f=NF), Tok()))
    wislots = []
    for i in range(3):
        o = alloc(512)
        wislots.append((bf16v(o, 1024).rearrange("p (k c) -> p k c", k=8), Tok()))
    wv = bf16v(alloc(8 * 256), 8 * 512).rearrange("p (k c) -> p k c", k=8)
    wki = f32v(alloc(8 * 64), 8 * 64).rearrange("p (k c) -> p k c", k=8)
    wwi = f32v(alloc(8 * 8), 8 * 8).rearrange("p (k c) -> p k c", k=8)
    wblk = bf16v(alloc(2 * 4 * 64), 2 * 4 * 128).rearrange("p (a c q) -> p a c q", a=2, c=4)
    xl = f32v(alloc(4 * (N + 4)), 4 * (N + 4)).rearrange("p (c n) -> p c n", c=4)
    xcb = bf16v(alloc(N // 2), N)
    glo = f32v(alloc(4 * 128), 4 * 128).rearrange("p (c n) -> p c n", c=4)
    lro = f32v(alloc(4 * 128), 4 * 128).rearrange("p (c n) -> p c n", c=4)
    hso = f32v(alloc(128), 128)
    ktile = bf16v(alloc(2 * N), 4 * N).rearrange("p (c n) -> p c n", c=4)
    vaug = bf16v(alloc(520 * 4 // 2), 4 * 520).rearrange("p (b h e) -> p b h e", b=4, h=8)
    kit = f32v(alloc(N), N)
    qtile = bf16v(alloc(4 * 64), 4 * 128).rearrange("p (c n) -> p c n", c=4)
    qit = f32v(alloc(8 * 128), 8 * 128).rearrange("p (h n) -> p h n", h=8)
    mgt = bf16v(alloc(8 * 64), 8 * 128).rearrange("p (c n) -> p c n", c=8)
    xo = f32v(alloc(8 * 128), 8 * 128).rearrange("p (k n) -> p k n", k=8)
    h2o = f32v(alloc(8 * 128), 8 * 128).rearrange("p (k n) -> p k n", k=8)
    hbo = bf16v(alloc(4 * 128), 8 * 128).rearrange("p (k n) -> p k n", k=8)

    (txt, thb, tsq, tyb, th2, trstd, twv, twqi, txl, txcb, tglo, tlro, tkt, tva, tkit, tqt,
     tqit, tmgt, twb, tlst, tws, txo, th2o, thbo, thso) = [Tok() for _ in range(25)]
    tact = [Tok() for _ in range(NF)]
    tlt = Tok()

    dma("pool", wv, w_in[:, 1024:1536].rearrange("(kc p) c -> p kc c", p=128), [], [twv])
    dma("sp", wki, w_in[:, 2048:2112].rearrange("(kc p) c -> p kc c", p=128), [], [twqi])
    dma("sp", wwi, w_in[:, 2112:2120].rearrange("(kc p) c -> p kc c", p=128), [], [twqi])
    dma("pool", wblk, wblk_d, [], [twb])
    P.op("dve", lambda e: e.memset(vaug, 1.0), [], [tva])
    P.op("dve", lambda e: e.memset(xl, 0.0), [], [txl])

    xT_v = xT.rearrange("(kc p) t -> p kc t", p=128)

    def select_own(dst, tdst, src, tsrc, eng="dve"):
        def blk(b):
            if len(src.shape) == 3:
                return src[:, :, b * 128:(b + 1) * 128]
            return src[:, b * 128:(b + 1) * 128]
        ts("dve", dst, blk(0), vecs[:, V_SEL:V_SEL + 1], None, ALU.mult, None, [tsrc, tvec], [tdst])
        for b in range(1, 4):
            stt(dst, blk(b), vecs[:, V_SEL + b:V_SEL + b + 1], dst, ALU.mult, ALU.add,
                [tsrc, tvec, tdst], [tdst])

    def norm_mod(src, tsrc, out_b, tob, sc_ap, sh_ap, out_f=None, tof=None, nk=8, n=N):
        norm_stats(src, nk, n, sq[:, 0:nk, 0:n], tsrc, tsq, rstd[:, 0:n], trstd, 0, 1.0 / (128 * nk))
        for k in range(nk):
            tmpk = ybuf[:, k, 0:n]
            tt("dve", tmpk, src[:, k, 0:n], rstd[:, 0:n], ALU.mult, [tsrc, trstd], [tyb])
            sc = sc_ap[:, k:k + 1]
            sh = sh_ap[:, k:k + 1] if sh_ap is not None else 0.0
            if out_f is not None:
                act(out_f[:, k, 0:n], tmpk, AF.Identity, [tyb, tder, tvec], [tof], bias=sh, scale=sc)
                cp("pool", out_b[:, k, 0:n], out_f[:, k, 0:n], [tof], [tob])
            else:
                act(out_b[:, k, 0:n], tmpk, AF.Identity, [tyb, tder, tvec], [tob], bias=sh, scale=sc)

    def post_res(ci, xres, txres):
        norm_stats(ybuf, 8, N, sq, tyb, tsq, rstd, trstd, 0, 1.0 / D)
        for k in range(8):
            tt("dve", ybuf[:, k, :], ybuf[:, k, :], rstd, ALU.mult, [tyb, trstd], [tyb])
            stt(xres[:, k, :], ybuf[:, k, :], der[:, D_GT[ci] + k:D_GT[ci] + k + 1], xres[:, k, :],
                ALU.mult, ALU.add, [tyb, tder, txres], [txres])

    def ffn2(i, bg=None):
        ffn(i, N, hb, thb, actb, tact, wslots, dslots, ybuf, tyb, sgb, tsg, bg)

    lru_bg = None
    for it in range(NA):
        T0 = it * N
        dma("sp", xt, xT_v[:, :, T0:T0 + N], [], [txt])
        norm_mod(xt, txt, hb, thb, der[:, D_SC[0]:D_SC[0] + 8], der[:, D_SH[0]:D_SH[0] + 8])
        ffn2(0, lru_bg)
        post_res(0, xt, txt)
        select_own(xo, txo, xt, txt)
        dma("sp", x1d[:, :, it * 128:(it + 1) * 128], xo, [txo], [])
        norm_mod(xt, txt, hb, thb, der[:, D_SC[1]:D_SC[1] + 8], der[:, D_SH[1]:D_SH[1] + 8],
                 out_f=h2, tof=th2)
        select_own(h2o, th2o, h2, th2)
        cp("pool", hbo, h2o, [th2o], [thbo])

        order = [4, 5, 6, 7, 8, 9, 10, 11, 12, 13, 14, 15, 0, 1, 2, 3]

        def ldw(ix):
            s = wislots[ix % 3]
            dma("sp", s[0], win_t[order[ix]], [tw["win"]], [s[1]])
        ldw(0); ldw(1)
        for ix in range(16):
            if ix + 2 < 16:
                ldw(ix + 2)
            ch = order[ix]
            s = wislots[ix % 3]
            pb = 1 + ix % 2
            own = ch < 4 or ch >= 12
            for kc in range(8):
                if own:
                    mm(psb[pb][:, 0:128], s[0][:, kc, :], hbo[:, kc, :], kc == 0, kc == 7,
                       [s[1], thbo], [pst[pb]])
                else:
                    mm(psb[pb][:, 0:N], s[0][:, kc, :], hb[:, kc, :], kc == 0, kc == 7,
                       [s[1], thb], [pst[pb]])
            if 4 <= ch < 8:
                cp("act", ktile[:, ch - 4, :], psb[pb][:, 0:N], [pst[pb]], [tkt])
            elif 8 <= ch < 12:
                cp("act", xl[:, ch - 8, 3:3 + N], psb[pb][:, 0:N], [pst[pb]], [txl])
            elif ch >= 12:
                cp("act", glo[:, ch - 12, :], psb[pb][:, 0:128], [pst[pb]], [tglo])
            else:
                cp("act", qtile[:, ch, :], psb[pb][:, 0:128], [pst[pb]], [tqt])
        dma("sp", Kd[:, :, T0:T0 + N], ktile, [tkt], [])
        dma("sp", qd[:, :, it * 128:(it + 1) * 128], qtile, [tqt], [])
        for tb in range(4):
            pb = 3 + tb % 2
            for kc in range(8):
                mm(psb[pb][:, 0:512], hb[:, kc, tb * 128:(tb + 1) * 128], wv[:, kc, :], kc == 0, kc == 7,
                   [thb, twv], [pst[pb]])
            cp("dve" if tb % 2 else "act", vaug[:, tb, :, 0:64],
               psb[pb][:, 0:512].rearrange("p (h e) -> p h e", h=8), [pst[pb]], [tva])
        dma("sp", Vd[T0:T0 + N, :].rearrange("(b p) e -> p b e", p=128),
            vaug.rearrange("p b h e -> p b (h e)"), [tva], [])
        for kc in range(8):
            mm(psb[5][0:64, 0:N], wki[:, kc, :], h2[:, kc, :], kc == 0, kc == 7, [twqi, th2], [pst[5]])
        cp("act", kit[0:64, :], psb[5][0:64, 0:N], [pst[5]], [tkit])
        dma("sp", KId[:, T0:T0 + N], kit[0:64, :], [tkit], [])
        dma("sp", wqi, w_in[:, 1536:2048].rearrange("(kc p) c -> p kc c", p=128), [], [tyb])
        for h in range(8):
            pb = 6 + h % 2
            for kc in range(8):
                mm(psb[pb][0:64, 0:128], wqi[:, kc, h * 64:(h + 1) * 64], h2o[:, kc, :], kc == 0, kc == 7,
                   [tyb, th2o], [pst[pb]])
            cp("dve" if h % 2 else "act", qit[0:64, h, :], psb[pb][0:64, 0:128], [pst[pb]], [tqit])
        dma("sp", qid[:, it, :, :], qit[0:64, :, :], [tqit], [])
        for kc in range(8):
            mm(psb[5][:, 0:8], h2o[:, kc, :], wwi[:, kc, :], kc == 0, kc == 7, [th2o, twqi], [pst[5]])
        act(wabs[:, it * 8:it * 8 + 8], psb[5][:, 0:8], AF.Abs, [pst[5]], [tws])
        act(wsgn[:, it * 8:it * 8 + 8], psb[5][:, 0:8], AF.Sign, [pst[5]], [tws])

        def lru_gen(it=it):
            xc, rg, ig, aa, bb, hs = lt
            for c in range(4):
                cw = lambda j, c=c: vecs[:, V_CW + j * 4 + c:V_CW + j * 4 + c + 1]
                ts("dve", xc, xl[:, c, 3:3 + N], cw(3), vecs[:, V_CB + c:V_CB + c + 1], ALU.mult, ALU.add,
                   [txl, tvec], [tlt])
                for j in range(3):
                    stt(xc, xl[:, c, j:j + N], cw(j), xc, ALU.mult, ALU.add, [txl, tvec, tlt], [tlt])
                cp("pool", xcb, xc, [tlt], [txcb])
                yield
                mm(psb[7][:, 0:N], wblk[:, 0, c, :], xcb, True, True, [twb, txcb], [pst[7]])
                act(rg, psb[7][:, 0:N], AF.Sigmoid, [pst[7], tvec], [tlt], bias=vecs[:, V_BA + c:V_BA + c + 1])
                mm(psb[7][:, 0:N], wblk[:, 1, c, :], xcb, True, True, [twb, txcb], [pst[7]])
                act(ig, psb[7][:, 0:N], AF.Sigmoid, [pst[7], tvec], [tlt], bias=vecs[:, V_BI + c:V_BI + c + 1])
                act(aa, rg, AF.Exp, [tlt, tder], [tlt], scale=der[:, D_CA + c:D_CA + c + 1])
                tt("dve", rg, aa, aa, ALU.mult, [tlt], [tlt])
                act(rg, rg, AF.Sqrt, [tlt], [tlt], bias=1.0, scale=-1.0)
                tt("dve", bb, ig, xc, ALU.mult, [tlt], [tlt])
                tt("dve", bb, bb, rg, ALU.mult, [tlt], [tlt])
                yield
                P.op("dve", lambda e, o=hs, a=aa, b=bb, s=lstate[:, c:c + 1]:
                     e.tensor_tensor_scan(out=o, data0=a, data1=b, initial=s, op0=ALU.mult, op1=ALU.add),
                     [tlt, tlst, tder], [tlt])
                cp("dve", lstate[:, c:c + 1], hs[:, N - 1:N], [tlt], [tlst])
                cp("dve", xl[:, c, 0:3], xl[:, c, N:N + 3], [txl, tlt], [txl])
                select_own(hso, thso, hs, tlt)
                yield
                g = glo[:, c, :]
                t1 = lro[:, c, :]
                tt("dve", t1, g, g, ALU.mult, [tglo], [tlro])
                ts("dve", t1, t1, 0.044715, 1.0, ALU.mult, ALU.add, [tlro], [tlro])
                tt("dve", t1, t1, g, ALU.mult, [tlro, tglo], [tlro])
                act(t1, t1, AF.Sigmoid, [tlro], [tlro], scale=1.5957691216057308)
                tt("dve", t1, t1, g, ALU.mult, [tlro, tglo], [tlro])
                tt("dve", t1, t1, hso, ALU.mult, [tlro, thso], [tlro])
                yield
            norm_mod(lro, tlro, mgt[:, 4:8, :], tmgt, vecs[:, V_LOG:V_LOG + 4], None, nk=4, n=128)
            dma("sp", mgd[:, 4:8, it * 128:(it + 1) * 128], mgt[:, 4:8, :], [tmgt], [])
            yield
        lru_bg = lru_gen()
    for _ in lru_bg:
        pass
    P.barrier()
    mark_a = cur[0]
    cur[0] = base_mark
    if stop == 'A':
        return finish()

    NKB = NA * 4
    KI = f32v(alloc(NKB * 128), NKB * 128)
    qown = bf16v(alloc(2 * NA * 128), 4 * NA * 128).rearrange("p (c n) -> p c n", c=4)
    Sb = f32v(alloc(NKB * 128), NKB * 128)
    maskb = [bf16v(alloc(NKB * 64), NKB * 128) for _ in range(4)]
    Rt = [f32v(alloc(512), 512) for _ in range(2)]
    qi = [f32v(alloc(1024), 1024).rearrange("p (h n) -> p h n", h=8) for _ in range(2)]
    kts = [bf16v(alloc(256), 512).rearrange("p (c n) -> p c n", c=4) for _ in range(4)]
    vts = [bf16v(alloc(260), 520).rearrange("p (h e) -> p h e", h=8) for _ in range(4)]
    mTs = [bf16v(alloc(128), 256) for _ in range(2)]
    Es = [bf16v(alloc(128), 256) for _ in range(4)]
    Pms = [bf16v(alloc(128), 256) for _ in range(4)]
    attn = f32v(alloc(512), 512)
    attnb = bf16v(alloc(256), 512)
    mgB = bf16v(alloc(256), 512).rearrange("p (c n) -> p c n", c=4)
    pm4 = f32v(alloc(512), 512)
    TbC = bf16v(alloc(5 * 8 * 64), 5 * 8 * 128).rearrange("p (a h q) -> p a h q", a=5, h=8)
    bis = f32v(alloc(32), 32)
    fin = f32v(alloc(32), 32)
    scr = f32v(alloc(8), 8)
    steps = f32v(alloc(16), 16)
    psb5b = psb[5][:, 0:256].bitcast(BF16)
    tKI, tqown, tS, tattn, tattnb, tbis, tsteps, tpm, tmgB, tTbC, tfin, tscr, tscr2 = [Tok() for _ in range(13)]
    tmask = [Tok() for _ in range(4)]
    tR = [Tok(), Tok()]
    tqi = [Tok(), Tok()]
    tkv = [Tok() for _ in range(4)]
    tmT = [Tok(), Tok()]
    tE = [Tok() for _ in range(4)]
    tPm = [Tok() for _ in range(4)]
    tST = [Tok() for _ in range(4)]
    dma("sp", KI[0:64, :], KId[:, 0:NKB * 128], [], [tKI])
    dma("sp", qown, qd[:, :, 0:NA * 128], [], [tqown])
    dma("sp", pm4, posm_d.rearrange("p k n -> p (k n)"), [], [tpm])
    for a in range(5):
        for h in range(8):
            ts("dve", TbC[:, a, h, :], Tb[:, 0, h, :], vecs[:, V_SD + a:V_SD + a + 1], None, ALU.mult, None,
               [tTb, tvec], [tTbC])
            stt(TbC[:, a, h, :], Tb[:, 1, h, :], vecs[:, V_SP + a:V_SP + a + 1], TbC[:, a, h, :],
                ALU.mult, ALU.add, [tTb, tvec, tTbC], [tTbC])

    def indexer(j):
        mslot = j % 4
        qs = j % 2
        nk = (j + 1) * 512
        dma("sp", qi[qs][0:64], qid[:, j, :, :], [], [tqi[qs]])
        for kc0 in range(0, nk, 512):
            for h in range(8):
                mm(psb[5][:, 0:512], qi[qs][0:64, h, :], KI[0:64, kc0:kc0 + 512], True, True,
                   [tqi[qs], tKI], [pst[5]])
                R = Rt[h % 2]
                act(R, psb[5][:, 0:512], AF.Relu, [pst[5], tws], [tR[h % 2]],
                    scale=wabs[:, j * 8 + h:j * 8 + h + 1])
                sg = wsgn[:, j * 8 + h:j * 8 + h + 1]
                if h == 0:
                    ts("dve", Sb[:, kc0:kc0 + 512], R, sg, None, ALU.mult, None, [tR[h % 2], tws], [tS])
                else:
                    stt(Sb[:, kc0:kc0 + 512], R, sg, Sb[:, kc0:kc0 + 512], ALU.mult, ALU.add,
                        [tR[h % 2], tws, tS], [tS])
                yield
        P.op("dve", lambda e, o=bis[:, 0:1], i=Sb[:, 0:nk]:
             e.tensor_reduce(out=o, in_=i, axis=AX.X, op=ALU.max, apply_absolute_value=True),
             [tS], [tbis])
        base = j * 512
        tt("dve", Sb[:, base:base + 512], Sb[:, base:base + 512], pm4, ALU.add, [tS, tpm], [tS])
        hi0, lo, w0, mid, cnt, gs = [bis[:, i:i + 1] for i in range(1, 7)]
        ts("dve", hi0, bis[:, 0:1], 1.001, 1e-6, ALU.mult, ALU.add, [tbis], [tbis])
        ts("dve", lo, hi0, -1.0, None, ALU.mult, None, [tbis], [tbis])
        ts("dve", w0, hi0, 2.0, None, ALU.mult, None, [tbis], [tbis])
        ts("dve", steps, vecs[:, V_P2:V_P2 + 16], w0, None, ALU.mult, None, [tbis, tvec], [tsteps])
        yield
        junk = maskb[mslot][:, 0:nk]
        for k in range(NBIS):
            tt("dve", mid, lo, steps[:, k:k + 1], ALU.add, [tbis, tsteps], [tbis])
            ts("dve", junk, Sb[:, 0:nk], mid, None, ALU.is_ge, ALU.add, [tS, tbis], [tmask[mslot], tbis],
               accum=cnt)
            P.op("dve", lambda e: e.memset(scr[:, 0:1], 0.0), [tbis], [tbis])
            ts("dve", gs, cnt, 255.5, steps[:, k:k + 1], ALU.is_ge, ALU.mult, [tbis, tsteps], [tbis])
            tt("dve", lo, lo, gs, ALU.add, [tbis], [tbis])
            yield
        ts("dve", junk, Sb[:, 0:nk], lo, None, ALU.is_ge, None, [tS, tbis], [tmask[mslot]])
        yield

    def drain(gen, n=None):
        if gen is None:
            return
        try:
            if n is None:
                while True:
                    next(gen)
            else:
                for _ in range(n):
                    next(gen)
        except StopIteration:
            pass

    def chain(*gens):
        for g_ in gens:
            for _ in g_:
                yield

    def attention(j0, nq, bg):
        nkb_of = [(j0 + t + 1) * 4 for t in range(nq)]
        nkb = nkb_of[-1]
        first = [True] * 4
        c1 = nq * 128

        def ldkv(kb):
            s = kb % 4
            dma("sp", kts[s], Kd[:, :, kb * 128:(kb + 1) * 128], [], [tkv[s]])
            dma("sp", vts[s].rearrange("p h e -> p (h e)"), Vd[kb * 128:(kb + 1) * 128, :], [], [tkv[s]])

        def t0_of(kb):
            t0 = 0
            while kb >= nkb_of[t0]:
                t0 += 1
            return t0
        steps = [(kb, h) for kb in range(nkb) for h in range(8)]
        LAG = 2

        def front(i):
            kb, h = steps[i]
            s = kb % 4
            t0 = t0_of(kb)
            c0 = t0 * 128
            mT = mTs[kb % 2]
            if h == 0:
                if kb == 0:
                    ldkv(0)
                    ldkv(1)
                if kb + 2 < nkb:
                    ldkv(kb + 2)
                for t in range(t0, nq):
                    ms = (j0 + t) % 4
                    tr(psb5b[:, t * 128:(t + 1) * 128], maskb[ms][:, kb * 128:(kb + 1) * 128],
                       [tmask[ms], tvec], [pst[5]])
                cp("act", mT[:, c0:c1], psb5b[:, c0:c1], [pst[5]], [tmT[kb % 2]])
            po = (h % 2) * 64
            slot4 = i % 4
            pbank = (0, 6, 7)[i % 3]
            pcol = 0
            ps_s = psb[pbank][:, pcol + c0:pcol + c1]
            mm(ps_s, kts[s][po:po + 64, h // 2, :],
               qown[po:po + 64, h // 2, j0 * 128 + c0:j0 * 128 + c1],
               True, True, [tkv[s], tqown], [pst[pbank]])
            for t in range(t0, nq):
                a = kb - 4 * (j0 + t) + 1
                if 0 <= a <= 4:
                    sl = psb[pbank][:, pcol + t * 128:pcol + (t + 1) * 128]
                    tt("dve", sl, sl, TbC[:, a, h, :], ALU.add, [pst[pbank], tTbC], [pst[pbank]])
            E = Es[slot4]
            act(E[:, c0:c1], ps_s, AF.Exp, [pst[pbank], tvec], [tE[slot4]],
                bias=vecs[:, V_RB + 15 * 8 + h:V_RB + 15 * 8 + h + 1], scale=0.125)
            Pm = Pms[slot4]
            tt("pool", Pm[:, c0:c1], E[:, c0:c1], mT[:, c0:c1], ALU.mult,
               [tE[slot4], tmT[kb % 2]], [tPm[slot4]])

        def back(i):
            kb, h = steps[i]
            s = kb % 4
            slot4 = i % 4
            Pm = Pms[slot4]
            for t in range(t0_of(kb), nq):
                bank = 1 + t * 2 + (h // 4)
                col = (h % 4) * 65
                st_ = first[t * 2 + h // 4]
                first[t * 2 + h // 4] = False
                P.op("pe", lambda e, o=psb[bank][:, col:col + 65], l=Pm[:, t * 128:(t + 1) * 128],
                     r=vts[s][:, h, :], s_=st_:
                     e.matmul(o, lhsT=l, rhs=r, start=s_, stop=False, skip_group_check=True),
                     [tPm[slot4], tkv[s]], [pst[bank]])

        for i in range(len(steps) + LAG):
            if i < len(steps):
                front(i)
            if i >= LAG:
                back(i - LAG)
            drain(bg, 1)
        for t in range(nq):
            jt = j0 + t
            for hh in range(2):
                bank = 1 + t * 2 + hh
                v = psb[bank][:, 0:260].rearrange("p (h e) -> p h e", h=4)
                rc = fin[:, 8 + hh * 4:12 + hh * 4]
                cp("dve", rc, v[:, :, 64], [pst[bank]], [tfin])
                P.op("dve", lambda e, o=rc: e.reciprocal(out=o, in_=o), [tfin], [tfin])
                for h4 in range(4):
                    h = hh * 4 + h4
                    ts("dve", attn[:, h * 64:(h + 1) * 64], v[:, h4, 0:64], fin[:, 8 + h:9 + h], None,
                       ALU.mult, None, [pst[bank], tfin], [tattn])
            act(attnb, attn, AF.Square, [tattn], [tattnb, tfin], accum=fin[:, 16:17])
            P.op("act", lambda e: e.copy(out=scr[:, 4:5], in_=scr[:, 5:6]), [tfin], [tfin])
            act(fin[:, 17:18], fin[:, 16:17], AF.Sqrt, [tfin], [tfin], bias=EPS, scale=1.0 / 512)
            P.op("dve", lambda e, o=fin[:, 17:18]: e.reciprocal(out=o, in_=o), [tfin], [tfin])
            ts("dve", attnb, attn, fin[:, 17:18], None, ALU.mult, None, [tattn, tfin, tattnb], [tattnb])
            for c in range(4):
                tr(psb5b[:, c * 128:(c + 1) * 128], attnb[:, c * 128:(c + 1) * 128], [tattnb, tvec], [pst[5]])
            for c in range(4):
                act(mgB[:, c, :], psb5b[:, c * 128:(c + 1) * 128], AF.Identity, [pst[5], tvec], [tmgB],
                    scale=vecs[:, V_AOG + c:V_AOG + c + 1])
            dma("sp", mgd[:, 0:4, jt * 128:(jt + 1) * 128], mgB, [tmgB], [])

    P.op("dve", lambda e: e.memset(scr, 0.0), [], [tbis])
    sbs = list(range(0, NA, 2))
    drain(chain(*[indexer(j) for j in range(sbs[0], min(sbs[0] + 2, NA))]))
    for si, j0 in enumerate(sbs):
        nq = min(2, NA - j0)
        bg = None
        if si + 1 < len(sbs):
            j1 = sbs[si + 1]
            bg = chain(*[indexer(j) for j in range(j1, min(j1 + 2, NA))])
        attention(j0, nq, bg)
        drain(bg)

    P.barrier()
    cur[0] = mark_a
    if stop == 'B':
        return finish()

    outT_v = outT.rearrange("(kc p) t -> p kc t", p=128)
    for ct in range(NA // 4):
        C0 = ct * N
        dma("sp", xt, x1d[:, :, C0:C0 + N], [], [txt])
        dma("sp", hb, mgd[:, :, C0:C0 + N], [], [thb])

        def ldo(dc):
            s = wislots[dc % 3]
            dma("sp", s[0], wout_t[dc], [tw["wout"]], [s[1]])
        ldo(0); ldo(1)
        for dc in range(8):
            if dc + 2 < 8:
                ldo(dc + 2)
            s = wislots[dc % 3]
            pb = 1 + dc % 2
            for kc in range(8):
                mm(psb[pb][:, 0:N], s[0][:, kc, :], hb[:, kc, :], kc == 0, kc == 7, [s[1], thb], [pst[pb]])
            cp("act" if dc % 2 == 0 else "dve", ybuf[:, dc, :], psb[pb][:, 0:N], [pst[pb]], [tyb])
        post_res(1, xt, txt)
        norm_mod(xt, txt, hb, thb, der[:, D_SC[2]:D_SC[2] + 8], der[:, D_SH[2]:D_SH[2] + 8])
        ffn2(1)
        post_res(2, xt, txt)
        dma("sp", outT_v[:, :, C0:C0 + N], xt, [txt], [])
    P.barrier()
    with nc.Block() as block:
        P.emit(block)
    st.close()
    return nc


def _t5_bucket_np(rel):
    rel = np.asarray(rel, np.int32)
    nb = 16
    max_exact = 8
    ret = np.where(rel > 0, nb, 0).astype(np.int32)
    n = np.abs(rel)
    nf = np.maximum(n, 1).astype(np.float32)
    large = max_exact + (np.log(nf / np.float32(max_exact)) / np.float32(math.log(128 / max_exact))
                         * np.float32(nb - max_exact)).astype(np.int32)
    large = np.minimum(large, nb - 1)
    return ret + np.where(n < max_exact, n, large)


_NC_CACHE = {}


def _host_inputs(inputs, NA=16):
    f = lambda a: np.ascontiguousarray(np.asarray(a, np.float32))
    x = f(inputs["x"]); c = f(inputs["c"])

    def col8(v):
        return v.reshape(8, 128).T

    def col4(v):
        return v.reshape(4, 128).T
    kl = np.arange(128)[:, None]; ql = np.arange(128)[None, :]
    bd = _t5_bucket_np(kl - ql)
    bp = _t5_bucket_np(kl - ql - 128)
    mb = np.zeros((128, 2, 32, 128), np.float32)
    for b in range(32):
        mb[:, 0, b, :] = (bd == b)
        mb[:, 1, b, :] = (bp == b)
    mb = mb.astype(ml_dtypes.bfloat16)
    ident = np.eye(128, dtype=np.float32).astype(ml_dtypes.bfloat16)
    wblk = np.zeros((128, 2, 4, 128), np.float32)
    for ai, w in enumerate((f(inputs["lru_w_a"])[0], f(inputs["lru_w_i"])[0])):
        for g in range(8):
            cch, half = g // 2, g % 2
            wblk[half * 64:(half + 1) * 64, ai, cch, half * 64:(half + 1) * 64] = w[g]
    shared = {
        "w_ada": f(inputs["w_ada"])[0],
        "wg1": f(inputs["ffn1_w_gate"])[0], "wu1": f(inputs["ffn1_w_up"])[0], "wd1": f(inputs["ffn1_w_down"])[0],
        "wg2": f(inputs["ffn2_w_gate"])[0], "wu2": f(inputs["ffn2_w_up"])[0], "wd2": f(inputs["ffn2_w_down"])[0],
        "w_in": f(inputs["w_in"])[0], "w_out": f(inputs["w_out"])[0],
        "wblk": wblk, "mb": mb, "ident": ident,
    }
    xTs = [np.ascontiguousarray(x[b].T) for b in range(2)]
    in_maps = []
    for core in range(8):
        b, r = core // 4, core % 4
        v = np.zeros((128, NV), np.float32)
        v[:, V_C:V_C + 8] = col8(c[b])
        v[:, V_BADA:V_BADA + 72] = f(inputs["b_ada"])[0].reshape(72, 128).T
        for i, nm in enumerate(["ffn1_pre_g", "ffn1_post_g", "mix_pre_g", "mix_post_g", "ffn2_pre_g", "ffn2_post_g"]):
            v[:, V_G + 8 * i:V_G + 8 * i + 8] = col8(f(inputs[nm])[0])
        v[:, V_AOG:V_AOG + 4] = col4(f(inputs["attn_out_g"])[0])
        v[:, V_LOG:V_LOG + 4] = col4(f(inputs["lru_out_g"])[0])
        cw = f(inputs["lru_conv_w"])[0]
        for j in range(4):
            v[:, V_CW + 4 * j:V_CW + 4 * j + 4] = col4(cw[j])
        v[:, V_CB:V_CB + 4] = col4(f(inputs["lru_conv_b"])[0])
        v[:, V_BA:V_BA + 4] = col4(f(inputs["lru_b_a"])[0])
        v[:, V_BI:V_BI + 4] = col4(f(inputs["lru_b_i"])[0])
        v[:, V_LAM:V_LAM + 4] = col4(f(inputs["lru_lambda"])[0])
        v[:, V_RB:V_RB + 256] = f(inputs["rel_bias"]).reshape(1, 256)
        v[:, V_P2:V_P2 + 16] = (0.5 ** np.arange(1, 17))[None, :]
        v[:, V_SEL + r] = 1.0
        v[:, V_SD + r + 1] = 1.0
        v[:, V_SP + r] = 1.0
        posm = np.zeros((128, 4, 128), np.float32)
        for kb in range(4):
            if kb > r:
                posm[:, kb, :] = -BIG
            elif kb == r:
                posm[0:64, kb, 64:128] = -BIG
        m = dict(shared)
        m["xT"] = xTs[b]
        m["vecs"] = v
        m["posm"] = posm
        in_maps.append(m)
    return in_maps


def kernel(**inputs):
    NA = 16
    if NA not in _NC_CACHE:
        _NC_CACHE[NA] = build(NA)
    nc = _NC_CACHE[NA]
    in_maps = _host_inputs(inputs, NA)
    res = run_bass_kernel_spmd(nc, in_maps, core_ids=list(range(8)))
    out = np.zeros((2, S, D), np.float32)
    for core in range(8):
        b, r = core // 4, core % 4
        o = np.asarray(res.results[core]["outT"], np.float32)
        o = o.T.reshape(NBLK, 128, D)
        for j in range(NBLK):
            g = 4 * j + r
            out[b, g * 128:(g + 1) * 128, :] = o[j]
    return out
```

```python
import math
import numpy as np
import ml_dtypes
from contextlib import ExitStack
import concourse.bass as bass
import concourse.mybir as mybir
from concourse.bass_utils import run_bass_kernel_spmd

F32 = mybir.dt.float32
BF16 = mybir.dt.bfloat16
ALU = mybir.AluOpType
AF = mybir.ActivationFunctionType
AX = mybir.AxisListType

D = 1024
S = 8192
DFF = 2816
NF = DFF // 128
DIN = 3144
NBLK = 16
EPS = 1e-6
NBIS = 16
BIG = 3.0e38

V_C = 0
V_BADA = 8
V_G = 80
V_AOG = 128
V_LOG = 132
V_CW = 136
V_CB = 152
V_BA = 156
V_BI = 160
V_LAM = 164
V_RB = 168
V_P2 = 424
V_SEL = 440
V_SD = 444
V_SP = 449
NV = 456


class Tok:
    __slots__ = ("w", "r")

    def __init__(self):
        self.w = None
        self.r = []


class Prog:
    COMPUTE = ("pe", "act", "dve", "pool")
    NDMASEM = 24

    def __init__(self, nc, stack):
        self.nc = nc
        self.streams = {k: [] for k in ("pe", "act", "dve", "pool", "sp")}
        self.count = {k: 0 for k in self.COMPUTE}
        self.sems = {k: stack.enter_context(nc.semaphore("s_" + k)) for k in self.COMPUTE}
        self.dsems = [stack.enter_context(nc.semaphore("d%d" % i)) for i in range(self.NDMASEM)]
        self.dcount = [0] * self.NDMASEM
        self.dnext = 0
        self.waited = {k: {} for k in self.streams}
        self.ninstr = 0

    def _deps(self, stream, reads, writes):
        out = {}

        def add(kv):
            k, v = kv
            if k == stream and k == "pe":
                return
            if out.get(k, 0) < v:
                out[k] = v
        for t in reads:
            if t.w is not None:
                add(t.w)
        for t in writes:
            if t.w is not None:
                add(t.w)
            for kv in t.r:
                if kv[0] == stream and not isinstance(kv[0], int):
                    continue
                add(kv)
        waits = []
        wd = self.waited[stream]
        for k, v in out.items():
            if wd.get(k, 0) >= v:
                continue
            wd[k] = v
            waits.append((k, v))
        return waits

    def _semof(self, k):
        return self.dsems[k] if isinstance(k, int) else self.sems[k]

    def op(self, eng, fn, reads=(), writes=()):
        waits = self._deps(eng, reads, writes)
        self.count[eng] += 1
        idx = self.count[eng]
        self.streams[eng].append((waits, fn, ("c", eng)))
        for t in reads:
            t.r.append((eng, idx))
            if len(t.r) > 600:
                t.r = t.r[-400:]
        for t in writes:
            t.w = (eng, idx)
            t.r = []
        self.ninstr += 1

    def dma(self, q, fn, reads=(), writes=()):
        s = self.dnext
        self.dnext = (self.dnext + 1) % self.NDMASEM
        waits = self._deps(q, reads, writes)
        prev = self.dcount[s]
        if prev > 0 and self.waited[q].get(s, 0) < prev:
            self.waited[q][s] = prev
            waits.append((s, prev))
        self.dcount[s] += 16
        val = self.dcount[s]
        self.streams[q].append((waits, fn, ("d", s)))
        for t in reads:
            t.r.append((s, val))
        for t in writes:
            t.w = (s, val)
            t.r = []
        self.ninstr += 1

    def barrier(self):
        for st in self.streams:
            waits = []
            for k in self.COMPUTE:
                v = self.count[k]
                if v > 0 and self.waited[st].get(k, 0) < v and k != st:
                    self.waited[st][k] = v
                    waits.append((k, v))
            for s in range(self.NDMASEM):
                v = self.dcount[s]
                if v > 0 and self.waited[st].get(s, 0) < v:
                    self.waited[st][s] = v
                    waits.append((s, v))
            if waits:
                self.streams[st].append((waits, None, None))

    def emit(self, block):
        prog = self

        def run(stream, h):
            for waits, fn, kind in prog.streams[stream]:
                for (k, v) in waits:
                    h.wait_ge(prog._semof(k), v)
                if fn is None:
                    continue
                ins = fn(h)
                if kind[0] == "c":
                    ins.then_inc(prog.sems[kind[1]], 1)
                else:
                    ins.then_inc(prog.dsems[kind[1]], 16)

        @block.tensor
        def _(e):
            run("pe", e)

        @block.scalar
        def _(e):
            run("act", e)

        @block.vector
        def _(e):
            run("dve", e)

        @block.gpsimd
        def _(e):
            run("pool", e)

        @block.sync
        def _(e):
            run("sp", e)


def build(NA=16, stop=None, debug=False):
    nc = bass.Bass("TRN2", target_bir_lowering=False)
    st = ExitStack()
    P = Prog(nc, st)

    def dram_in(name, shape, dt=F32):
        return nc.dram_tensor(name, list(shape), dt, kind="ExternalInput").ap()

    def dram_tmp(name, shape, dt):
        if debug and name in ("Kd", "Vd", "KId", "x1d", "qd", "qid", "mgd"):
            return nc.dram_tensor(name, list(shape), dt, kind="ExternalOutput").ap()
        return nc.dram_tensor(name, list(shape), dt).ap()

    xT = dram_in("xT", [D, S])
    vecs_d = dram_in("vecs", [128, NV])
    w_ada = dram_in("w_ada", [D, 9 * D])
    wg1 = dram_in("wg1", [D, DFF]); wu1 = dram_in("wu1", [D, DFF]); wd1 = dram_in("wd1", [DFF, D])
    wg2 = dram_in("wg2", [D, DFF]); wu2 = dram_in("wu2", [D, DFF]); wd2 = dram_in("wd2", [DFF, D])
    w_in = dram_in("w_in", [D, DIN])
    w_out = dram_in("w_out", [D, D])
    wblk_d = dram_in("wblk", [128, 2, 4, 128])
    mb_d = dram_in("mb", [128, 2, 32, 128], BF16)
    ident_d = dram_in("ident", [128, 128], BF16)
    posm_d = dram_in("posm", [128, 4, 128])
    outT = nc.dram_tensor("outT", [D, NBLK * 128], F32, kind="ExternalOutput").ap()

    wg_t = [dram_tmp("wg_t%d" % i, [NF, 128, 8, 128], BF16) for i in range(2)]
    wu_t = [dram_tmp("wu_t%d" % i, [NF, 128, 8, 128], BF16) for i in range(2)]
    wd_t = [dram_tmp("wd_t%d" % i, [8, 128, NF, 128], BF16) for i in range(2)]
    win_t = dram_tmp("win_t", [16, 128, 8, 128], BF16)
    wout_t = dram_tmp("wout_t", [8, 128, 8, 128], BF16)
    Kd = dram_tmp("Kd", [128, 4, S], BF16)
    Vd = dram_tmp("Vd", [S, 520], BF16)
    KId = dram_tmp("KId", [64, S], F32)
    x1d = dram_tmp("x1d", [128, 8, NBLK * 128], F32)
    qd = dram_tmp("qd", [128, 4, NBLK * 128], BF16)
    qid = dram_tmp("qid", [64, NBLK, 8, 128], F32)
    mgd = dram_tmp("mgd", [128, 8, NBLK * 128], BF16)

    AW = 52600
    arena = st.enter_context(nc.sbuf_tensor("arena", [128, AW], F32))
    cur = [0]

    def alloc(words):
        a = cur[0]
        cur[0] += words
        assert cur[0] <= AW, ("sbuf overflow", cur[0])
        return a

    def f32v(off, n):
        return arena[:, off:off + n]

    def bf16v(off, nbf):
        return arena[:, off:off + nbf // 2].bitcast(BF16)

    vecs = f32v(alloc(NV), NV)
    der = f32v(alloc(128), 128)
    modT = f32v(alloc(72), 72)
    wabs = f32v(alloc(NBLK * 8), NBLK * 8)
    wsgn = f32v(alloc(NBLK * 8), NBLK * 8)
    identb = bf16v(alloc(64), 128)
    onesb = bf16v(alloc(64), 128)
    Tb = f32v(alloc(2048), 2048).rearrange("p (t h q) -> p t h q", t=2, h=8)
    lstate = f32v(alloc(4), 4)
    base_mark = cur[0]

    D_SC = [0, 16, 32]
    D_SH = [8, 24, 40]
    D_GT = [48, 56, 64]
    D_CA = 72
    D_MISC = 76

    psb = [st.enter_context(nc.psum_tensor("ps%d" % i, [128, 512], F32)) for i in range(8)]
    pst = [Tok() for _ in range(8)]

    tvec, tder, tmod = Tok(), Tok(), Tok()

    def mm(out, lhsT, rhs, start, stop, reads, writes):
        P.op("pe", lambda e, o=out, l=lhsT, r=rhs, s=start, t=stop:
             e.matmul(o, lhsT=l, rhs=r, start=s, stop=t), reads, writes)

    def tr(out, in_, reads, writes):
        P.op("pe", lambda e, o=out, i=in_: e.transpose(o, i, identb), reads, writes)

    def act(out, in_, func, reads, writes, bias=0.0, scale=1.0, accum=None):
        if accum is None:
            P.op("act", lambda e, o=out, i=in_, f=func, b=bias, s=scale:
                 e.activation(out=o, in_=i, func=f, bias=b, scale=s), reads, writes)
        else:
            P.op("act", lambda e, o=out, i=in_, f=func, b=bias, s=scale, a=accum:
                 e.activation(out=o, in_=i, func=f, bias=b, scale=s, accum_out=a), reads, writes)

    def tt(eng, out, in0, in1, op, reads, writes):
        P.op(eng, lambda e, o=out, a=in0, b=in1, p=op: e.tensor_tensor(out=o, in0=a, in1=b, op=p),
             reads, writes)

    def ts(eng, out, in0, s1, s2, op0, op1, reads, writes, accum=None):
        if op1 is None:
            op1 = ALU.bypass
        if accum is None:
            P.op(eng, lambda e, o=out, a=in0, x=s1, y=s2, p=op0, q=op1:
                 e.tensor_scalar(out=o, in0=a, scalar1=x, scalar2=y, op0=p, op1=q), reads, writes)
        else:
            P.op(eng, lambda e, o=out, a=in0, x=s1, y=s2, p=op0, q=op1, ac=accum:
                 e.tensor_scalar(out=o, in0=a, scalar1=x, scalar2=y, op0=p, op1=q, accum_out=ac),
                 reads, writes)

    def stt(out, in0, scalar, in1, op0, op1, reads, writes):
        P.op("dve", lambda e, o=out, a=in0, s=scalar, b=in1, p=op0, q=op1:
             e.scalar_tensor_tensor(out=o, in0=a, scalar=s, in1=b, op0=p, op1=q), reads, writes)

    def cp(eng, out, in_, reads, writes):
        if eng == "act":
            P.op("act", lambda e, o=out, i=in_: e.copy(out=o, in_=i), reads, writes)
        else:
            P.op(eng, lambda e, o=out, i=in_: e.tensor_copy(out=o, in_=i), reads, writes)

    def dma(q, out, in_, reads, writes):
        P.dma(q, lambda e, o=out, i=in_: e.dma_start(out=o, in_=i), reads, writes)

    dma("sp", vecs, vecs_d, [], [tvec])
    dma("sp", identb, ident_d, [], [tvec])
    P.op("dve", lambda e: e.memset(onesb, 1.0), [], [tvec])

    tw = {}

    def cast_w(name, dst, src_view, n):
        t = Tok()
        tw[name] = t
        for i in range(n):
            dma("pool", dst[i], src_view[i], [], [t])

    def cast_ffn(i, wg, wu, wd):
        cast_w("wg%d" % i, wg_t[i], wg.rearrange("(kc p) (f c) -> f p kc c", p=128, c=128), NF)
        cast_w("wu%d" % i, wu_t[i], wu.rearrange("(kc p) (f c) -> f p kc c", p=128, c=128), NF)
        cast_w("wd%d" % i, wd_t[i], wd.rearrange("(f p) (dc c) -> dc p f c", p=128, c=128), 8)

    cast_ffn(0, wg1, wu1, wd1)
    win_cols = [0, 128, 256, 384, 512, 640, 768, 896, 2120, 2248, 2376, 2504, 2632, 2760, 2888, 3016]
    t = Tok(); tw["win"] = t
    for i, c0 in enumerate(win_cols):
        dma("pool", win_t[i], w_in[:, c0:c0 + 128].rearrange("(kc p) c -> p kc c", p=128), [], [t])

    scv = f32v(alloc(8), 8)
    tsc = Tok()
    act(scv, vecs[:, V_C:V_C + 8], AF.Silu, [tvec], [tsc])
    wa_off = alloc(2 * 8 * 1152)
    wa_buf = [f32v(wa_off + i * 9216, 9216).rearrange("p (k c) -> p k c", k=8) for i in range(2)]
    twa = [Tok(), Tok()]
    w_ada_v = w_ada.rearrange("(kc p) n -> p kc n", p=128)
    for cb in range(8):
        buf = wa_buf[cb % 2]
        dma("sp", buf, w_ada_v[:, :, cb * 1152:(cb + 1) * 1152], [], [twa[cb % 2]])
        for cc in range(9):
            col = cb * 9 + cc
            for kc in range(8):
                mm(psb[0][:, col:col + 1], buf[:, kc, cc * 128:(cc + 1) * 128], scv[:, kc:kc + 1],
                   kc == 0, kc == 7, [twa[cb % 2], tsc], [pst[0]])
    tt("dve", modT, psb[0][:, 0:72], vecs[:, V_BADA:V_BADA + 72], ALU.add, [pst[0], tvec], [tmod])
    for i, (jsh, jsc, jg, gpre, gpost, gmul) in enumerate(
            [(0, 1, 2, 0, 1, 0.5), (3, 4, 5, 2, 3, 1.0), (6, 7, 8, 4, 5, 0.5)]):
        stt(der[:, D_SC[i]:D_SC[i] + 8], modT[:, jsc * 8:jsc * 8 + 8], 1.0,
            vecs[:, V_G + gpre * 8:V_G + gpre * 8 + 8], ALU.add, ALU.mult, [tmod, tvec], [tder])
        cp("dve", der[:, D_SH[i]:D_SH[i] + 8], modT[:, jsh * 8:jsh * 8 + 8], [tmod], [tder])
        stt(der[:, D_GT[i]:D_GT[i] + 8], modT[:, jg * 8:jg * 8 + 8], gmul,
            vecs[:, V_G + gpost * 8:V_G + gpost * 8 + 8], ALU.mult, ALU.mult, [tmod, tvec], [tder])
    tmp4 = der[:, D_MISC:D_MISC + 4]
    act(tmp4, vecs[:, V_LAM:V_LAM + 4], AF.Exp, [tvec], [tder], scale=-1.0)
    act(tmp4, tmp4, AF.Ln, [tder], [tder], bias=1.0)
    ts("dve", der[:, D_CA:D_CA + 4], tmp4, -8.0, None, ALU.mult, None, [tder], [tder])
    P.op("dve", lambda e: e.memset(lstate, 0.0), [], [tder])

    mb_off = alloc(2 * 32 * 128 // 2)
    mbv = bf16v(mb_off, 2 * 32 * 128).rearrange("p (t b q) -> p t b q", t=2, b=32)
    tmb, tTb = Tok(), Tok()
    dma("sp", mbv, mb_d, [], [tmb])
    for ty in range(2):
        for h in range(8):
            T = Tb[:, ty, h, :]
            ts("dve", T, mbv[:, ty, 0, :], vecs[:, V_RB + h:V_RB + h + 1],
               vecs[:, V_RB + 15 * 8 + h:V_RB + 15 * 8 + h + 1], ALU.mult, ALU.subtract,
               [tmb, tvec], [tTb])
            for b in range(1, 32):
                stt(T, mbv[:, ty, b, :], vecs[:, V_RB + b * 8 + h:V_RB + b * 8 + h + 1], T,
                    ALU.mult, ALU.add, [tmb, tvec, tTb], [tTb])
            ts("dve", T, T, 8.0, None, ALU.mult, None, [tTb], [tTb])
    cast_ffn(1, wg2, wu2, wd2)
    cast_w("wout", wout_t, w_out.rearrange("(kc p) (dc c) -> dc p kc c", p=128, c=128), 8)
    P.barrier()
    cur[0] = base_mark


    def finish():
        P.barrier()
        with nc.Block() as block:
            P.emit(block)
        st.close()
        return nc
    if stop == 'pro':
        return finish()
    def norm_stats(src, nk, N, sq, tsrc, tsq, rstd, trstd, ps_i, dscale):
        act(sq, src, AF.Square, [tsrc], [tsq])
        for k in range(nk):
            mm(psb[ps_i][:, 0:N], onesb, sq[:, k, :], k == 0, k == nk - 1, [tsq, tvec], [pst[ps_i]])
        act(rstd, psb[ps_i][:, 0:N], AF.Sqrt, [pst[ps_i]], [trstd], bias=EPS, scale=dscale)
        P.op("dve", lambda e, o=rstd: e.reciprocal(out=o, in_=o), [trstd], [trstd])

    def ffn(i, N, hb, thb, actb, tact, wslots, dslots, ybuf, tyb, sgb, tsg, bg=None):
        def ld(f):
            s = wslots[f % 3]
            dma("sp", s[0], wg_t[i][f], [tw["wg%d" % i]], [s[2]])
            dma("sp", s[1], wu_t[i][f], [tw["wu%d" % i]], [s[2]])
        ld(0); ld(1)
        for f in range(NF):
            if f + 2 < NF:
                ld(f + 2)
            s = wslots[f % 3]
            pg, pu = (1, 2) if f % 2 == 0 else (3, 4)
            for kc in range(8):
                mm(psb[pg][:, 0:N], s[0][:, kc, :], hb[:, kc, :], kc == 0, kc == 7, [s[2], thb], [pst[pg]])
            for kc in range(8):
                mm(psb[pu][:, 0:N], s[1][:, kc, :], hb[:, kc, :], kc == 0, kc == 7, [s[2], thb], [pst[pu]])
            sg = sgb[f % 2]
            act(sg, psb[pg][:, 0:N], AF.Silu, [pst[pg]], [tsg[f % 2]])
            tt("dve", actb[:, f, :], sg, psb[pu][:, 0:N], ALU.mult, [tsg[f % 2], pst[pu]], [tact[f]])
            if bg is not None:
                for _ in range(2):
                    next(bg, None)
        if bg is not None:
            for _ in bg:
                pass

        def ldd(dc):
            s = dslots[dc % 2]
            dma("sp", s[0], wd_t[i][dc], [tw["wd%d" % i]], [s[1]])
        ldd(0)
        for dc in range(8):
            if dc + 1 < 8:
                ldd(dc + 1)
            s = dslots[dc % 2]
            pa = 5 + dc % 2
            for f in range(NF):
                mm(psb[pa][:, 0:N], s[0][:, f, :], actb[:, f, :], f == 0, f == NF - 1,
                   [s[1], tact[f]], [pst[pa]])
            cp("act" if dc % 2 == 0 else "dve", ybuf[:, dc, :], psb[pa][:, 0:N], [pst[pa]], [tyb])

    N = 512
    xt = f32v(alloc(8 * N), 8 * N).rearrange("p (k n) -> p k n", k=8)
    hb = bf16v(alloc(4 * N), 8 * N).rearrange("p (k n) -> p k n", k=8)
    sq = bf16v(alloc(4 * N), 8 * N).rearrange("p (k n) -> p k n", k=8)
    actb = bf16v(alloc(NF * N // 2), NF * N).rearrange("p (f n) -> p f n", f=NF)
    yoff = alloc(8 * N)
    ybuf = f32v(yoff, 8 * N).rearrange("p (k n) -> p k n", k=8)
    ltoff = alloc(6 * N)
    lt = [f32v(ltoff + i * N, N) for i in range(6)]
    wqi = f32v(yoff, 8 * 512).rearrange("p (k c) -> p k c", k=8)
    h2 = f32v(alloc(8 * N), 8 * N).rearrange("p (k n) -> p k n", k=8)
    rstd = f32v(alloc(N), N)
    sgb = [f32v(alloc(N), N) for _ in range(2)]
    tsg = [Tok(), Tok()]
    wslots = []
    for i in range(3):
        o = alloc(1024)
        wslots.append((bf16v(o, 1024).rearrange("p (k c) -> p k c", k=8),
                       bf16v(o + 512, 1024).rearrange("p (k c) -> p k c", k=8), Tok()))
    dslots = []
    for i in range(2):
        o = alloc(NF * 64)
        dslots.append((bf16v(o, NF * 128).rearrange("p (f c) -> p f c", f=NF), Tok()))
    wislots = []
    for i in range(3):
        o = alloc(512)
        wislots.append((bf16v(o, 1024).rearrange("p (k c) -> p k c", k=8), Tok()))
    wv = bf16v(alloc(8 * 256), 8 * 512).rearrange("p (k c) -> p k c", k=8)
    wki = f32v(alloc(8 * 64), 8 * 64).rearrange("p (k c) -> p k c", k=8)
    wwi = f32v(alloc(8 * 8), 8 * 8).rearrange("p (k c) -> p k c", k=8)
    wblk = bf16v(alloc(2 * 4 * 64), 2 * 4 * 128).rearrange("p (a c q) -> p a c q", a=2, c=4)
    xl = f32v(alloc(4 * (N + 4)), 4 * (N + 4)).rearrange("p (c n) -> p c n", c=4)
    xcb = bf16v(alloc(N // 2), N)
    glo = f32v(alloc(4 * 128), 4 * 128).rearrange("p (c n) -> p c n", c=4)
    lro = f32v(alloc(4 * 128), 4 * 128).rearrange("p (c n) -> p c n", c=4)
    hso = f32v(alloc(128), 128)
    ktile = bf16v(alloc(2 * N), 4 * N).rearrange("p (c n) -> p c n", c=4)
    vaug = bf16v(alloc(520 * 4 // 2), 4 * 520).rearrange("p (b h e) -> p b h e", b=4, h=8)
    kit = f32v(alloc(N), N)
    qtile = bf16v(alloc(4 * 64), 4 * 128).rearrange("p (c n) -> p c n", c=4)
    qit = f32v(alloc(8 * 128), 8 * 128).rearrange("p (h n) -> p h n", h=8)
    mgt = bf16v(alloc(8 * 64), 8 * 128).rearrange("p (c n) -> p c n", c=8)
    xo = f32v(alloc(8 * 128), 8 * 128).rearrange("p (k n) -> p k n", k=8)
    h2o = f32v(alloc(8 * 128), 8 * 128).rearrange("p (k n) -> p k n", k=8)
    hbo = bf16v(alloc(4 * 128), 8 * 128).rearrange("p (k n) -> p k n", k=8)

    (txt, thb, tsq, tyb, th2, trstd, twv, twqi, txl, txcb, tglo, tlro, tkt, tva, tkit, tqt,
     tqit, tmgt, twb, tlst, tws, txo, th2o, thbo, thso) = [Tok() for _ in range(25)]
    tact = [Tok() for _ in range(NF)]
    tlt = Tok()

    dma("pool", wv, w_in[:, 1024:1536].rearrange("(kc p) c -> p kc c", p=128), [], [twv])
    dma("sp", wki, w_in[:, 2048:2112].rearrange("(kc p) c -> p kc c", p=128), [], [twqi])
    dma("sp", wwi, w_in[:, 2112:2120].rearrange("(kc p) c -> p kc c", p=128), [], [twqi])
    dma("pool", wblk, wblk_d, [], [twb])
    P.op("dve", lambda e: e.memset(vaug, 1.0), [], [tva])
    P.op("dve", lambda e: e.memset(xl, 0.0), [], [txl])

    xT_v = xT.rearrange("(kc p) t -> p kc t", p=128)

    def select_own(dst, tdst, src, tsrc, eng="dve"):
        def blk(b):
            if len(src.shape) == 3:
                return src[:, :, b * 128:(b + 1) * 128]
            return src[:, b * 128:(b + 1) * 128]
        ts("dve", dst, blk(0), vecs[:, V_SEL:V_SEL + 1], None, ALU.mult, None, [tsrc, tvec], [tdst])
        for b in range(1, 4):
            stt(dst, blk(b), vecs[:, V_SEL + b:V_SEL + b + 1], dst, ALU.mult, ALU.add,
                [tsrc, tvec, tdst], [tdst])

    def norm_mod(src, tsrc, out_b, tob, sc_ap, sh_ap, out_f=None, tof=None, nk=8, n=N):
        norm_stats(src, nk, n, sq[:, 0:nk, 0:n], tsrc, tsq, rstd[:, 0:n], trstd, 0, 1.0 / (128 * nk))
        for k in range(nk):
            tmpk = ybuf[:, k, 0:n]
            tt("dve", tmpk, src[:, k, 0:n], rstd[:, 0:n], ALU.mult, [tsrc, trstd], [tyb])
            sc = sc_ap[:, k:k + 1]
            sh = sh_ap[:, k:k + 1] if sh_ap is not None else 0.0
            if out_f is not None:
                act(out_f[:, k, 0:n], tmpk, AF.Identity, [tyb, tder, tvec], [tof], bias=sh, scale=sc)
                cp("pool", out_b[:, k, 0:n], out_f[:, k, 0:n], [tof], [tob])
            else:
                act(out_b[:, k, 0:n], tmpk, AF.Identity, [tyb, tder, tvec], [tob], bias=sh, scale=sc)

    def post_res(ci, xres, txres):
        norm_stats(ybuf, 8, N, sq, tyb, tsq, rstd, trstd, 0, 1.0 / D)
        for k in range(8):
            tt("dve", ybuf[:, k, :], ybuf[:, k, :], rstd, ALU.mult, [tyb, trstd], [tyb])
            stt(xres[:, k, :], ybuf[:, k, :], der[:, D_GT[ci] + k:D_GT[ci] + k + 1], xres[:, k, :],
                ALU.mult, ALU.add, [tyb, tder, txres], [txres])

    def ffn2(i, bg=None):
        ffn(i, N, hb, thb, actb, tact, wslots, dslots, ybuf, tyb, sgb, tsg, bg)

    lru_bg = None
    for it in range(NA):
        T0 = it * N
        dma("sp", xt, xT_v[:, :, T0:T0 + N], [], [txt])
        norm_mod(xt, txt, hb, thb, der[:, D_SC[0]:D_SC[0] + 8], der[:, D_SH[0]:D_SH[0] + 8])
        ffn2(0, lru_bg)
        post_res(0, xt, txt)
        select_own(xo, txo, xt, txt)
        dma("sp", x1d[:, :, it * 128:(it + 1) * 128], xo, [txo], [])
        norm_mod(xt, txt, hb, thb, der[:, D_SC[1]:D_SC[1] + 8], der[:, D_SH[1]:D_SH[1] + 8],
                 out_f=h2, tof=th2)
        select_own(h2o, th2o, h2, th2)
        cp("pool", hbo, h2o, [th2o], [thbo])

        order = [4, 5, 6, 7, 8, 9, 10, 11, 12, 13, 14, 15, 0, 1, 2, 3]

        def ldw(ix):
            s = wislots[ix % 3]
            dma("sp", s[0], win_t[order[ix]], [tw["win"]], [s[1]])
        ldw(0); ldw(1)
        for ix in range(16):
            if ix + 2 < 16:
                ldw(ix + 2)
            ch = order[ix]
            s = wislots[ix % 3]
            pb = 1 + ix % 2
            own = ch < 4 or ch >= 12
            for kc in range(8):
                if own:
                    mm(psb[pb][:, 0:128], s[0][:, kc, :], hbo[:, kc, :], kc == 0, kc == 7,
                       [s[1], thbo], [pst[pb]])
                else:
                    mm(psb[pb][:, 0:N], s[0][:, kc, :], hb[:, kc, :], kc == 0, kc == 7,
                       [s[1], thb], [pst[pb]])
            if 4 <= ch < 8:
                cp("act", ktile[:, ch - 4, :], psb[pb][:, 0:N], [pst[pb]], [tkt])
            elif 8 <= ch < 12:
                cp("act", xl[:, ch - 8, 3:3 + N], psb[pb][:, 0:N], [pst[pb]], [txl])
            elif ch >= 12:
                cp("act", glo[:, ch - 12, :], psb[pb][:, 0:128], [pst[pb]], [tglo])
            else:
                cp("act", qtile[:, ch, :], psb[pb][:, 0:128], [pst[pb]], [tqt])
        dma("sp", Kd[:, :, T0:T0 + N], ktile, [tkt], [])
        dma("sp", qd[:, :, it * 128:(it + 1) * 128], qtile, [tqt], [])
        for tb in range(4):
            pb = 3 + tb % 2
            for kc in range(8):
                mm(psb[pb][:, 0:512], hb[:, kc, tb * 128:(tb + 1) * 128], wv[:, kc, :], kc == 0, kc == 7,
                   [thb, twv], [pst[pb]])
            cp("dve" if tb % 2 else "act", vaug[:, tb, :, 0:64],
               psb[pb][:, 0:512].rearrange("p (h e) -> p h e", h=8), [pst[pb]], [tva])
        dma("sp", Vd[T0:T0 + N, :].rearrange("(b p) e -> p b e", p=128),
            vaug.rearrange("p b h e -> p b (h e)"), [tva], [])
        for kc in range(8):
            mm(psb[5][0:64, 0:N], wki[:, kc, :], h2[:, kc, :], kc == 0, kc == 7, [twqi, th2], [pst[5]])
        cp("act", kit[0:64, :], psb[5][0:64, 0:N], [pst[5]], [tkit])
        dma("sp", KId[:, T0:T0 + N], kit[0:64, :], [tkit], [])
        dma("sp", wqi, w_in[:, 1536:2048].rearrange("(kc p) c -> p kc c", p=128), [], [tyb])
        for h in range(8):
            pb = 6 + h % 2
            for kc in range(8):
                mm(psb[pb][0:64, 0:128], wqi[:, kc, h * 64:(h + 1) * 64], h2o[:, kc, :], kc == 0, kc == 7,
                   [tyb, th2o], [pst[pb]])
            cp("dve" if h % 2 else "act", qit[0:64, h, :], psb[pb][0:64, 0:128], [pst[pb]], [tqit])
        dma("sp", qid[:, it, :, :], qit[0:64, :, :], [tqit], [])
        for kc in range(8):
            mm(psb[5][:, 0:8], h2o[:, kc, :], wwi[:, kc, :], kc == 0, kc == 7, [th2o, twqi], [pst[5]])
        act(wabs[:, it * 8:it * 8 + 8], psb[5][:, 0:8], AF.Abs, [pst[5]], [tws])
        act(wsgn[:, it * 8:it * 8 + 8], psb[5][:, 0:8], AF.Sign, [pst[5]], [tws])

        def lru_gen(it=it):
            xc, rg, ig, aa, bb, hs = lt
            for c in range(4):
                cw = lambda j, c=c: vecs[:, V_CW + j * 4 + c:V_CW + j * 4 + c + 1]
                ts("dve", xc, xl[:, c, 3:3 + N], cw(3), vecs[:, V_CB + c:V_CB + c + 1], ALU.mult, ALU.add,
                   [txl, tvec], [tlt])
                for j in range(3):
                    stt(xc, xl[:, c, j:j + N], cw(j), xc, ALU.mult, ALU.add, [txl, tvec, tlt], [tlt])
                cp("pool", xcb, xc, [tlt], [txcb])
                yield
                mm(psb[7][:, 0:N], wblk[:, 0, c, :], xcb, True, True, [twb, txcb], [pst[7]])
                act(rg, psb[7][:, 0:N], AF.Sigmoid, [pst[7], tvec], [tlt], bias=vecs[:, V_BA + c:V_BA + c + 1])
                mm(psb[7][:, 0:N], wblk[:, 1, c, :], xcb, True, True, [twb, txcb], [pst[7]])
                act(ig, psb[7][:, 0:N], AF.Sigmoid, [pst[7], tvec], [tlt], bias=vecs[:, V_BI + c:V_BI + c + 1])
                act(aa, rg, AF.Exp, [tlt, tder], [tlt], scale=der[:, D_CA + c:D_CA + c + 1])
                tt("dve", rg, aa, aa, ALU.mult, [tlt], [tlt])
                act(rg, rg, AF.Sqrt, [tlt], [tlt], bias=1.0, scale=-1.0)
                tt("dve", bb, ig, xc, ALU.mult, [tlt], [tlt])
                tt("dve", bb, bb, rg, ALU.mult, [tlt], [tlt])
                yield
                P.op("dve", lambda e, o=hs, a=aa, b=bb, s=lstate[:, c:c + 1]:
                     e.tensor_tensor_scan(out=o, data0=a, data1=b, initial=s, op0=ALU.mult, op1=ALU.add),
                     [tlt, tlst, tder], [tlt])
                cp("dve", lstate[:, c:c + 1], hs[:, N - 1:N], [tlt], [tlst])
                cp("dve", xl[:, c, 0:3], xl[:, c, N:N + 3], [txl, tlt], [txl])
                select_own(hso, thso, hs, tlt)
                yield
                g = glo[:, c, :]
                t1 = lro[:, c, :]
                tt("dve", t1, g, g, ALU.mult, [tglo], [tlro])
                ts("dve", t1, t1, 0.044715, 1.0, ALU.mult, ALU.add, [tlro], [tlro])
                tt("dve", t1, t1, g, ALU.mult, [tlro, tglo], [tlro])
                act(t1, t1, AF.Sigmoid, [tlro], [tlro], scale=1.5957691216057308)
                tt("dve", t1, t1, g, ALU.mult, [tlro, tglo], [tlro])
                tt("dve", t1, t1, hso, ALU.mult, [tlro, thso], [tlro])
                yield
            norm_mod(lro, tlro, mgt[:, 4:8, :], tmgt, vecs[:, V_LOG:V_LOG + 4], None, nk=4, n=128)
            dma("sp", mgd[:, 4:8, it * 128:(it + 1) * 128], mgt[:, 4:8, :], [tmgt], [])
            yield
        lru_bg = lru_gen()
    for _ in lru_bg:
        pass
    P.barrier()
    mark_a = cur[0]
    cur[0] = base_mark
    if stop == 'A':
        return finish()

    NKB = NA * 4
    KI = f32v(alloc(NKB * 128), NKB * 128)
    qown = bf16v(alloc(2 * NA * 128), 4 * NA * 128).rearrange("p (c n) -> p c n", c=4)
    Sb = f32v(alloc(NKB * 128), NKB * 128)
    maskb = [bf16v(alloc(NKB * 64), NKB * 128) for _ in range(4)]
    Rt = [f32v(alloc(512), 512) for _ in range(2)]
    qi = [f32v(alloc(1024), 1024).rearrange("p (h n) -> p h n", h=8) for _ in range(2)]
    kts = [bf16v(alloc(256), 512).rearrange("p (c n) -> p c n", c=4) for _ in range(4)]
    vts = [bf16v(alloc(260), 520).rearrange("p (h e) -> p h e", h=8) for _ in range(4)]
    mTs = [bf16v(alloc(128), 256) for _ in range(2)]
    Es = [bf16v(alloc(128), 256) for _ in range(4)]
    Pms = [bf16v(alloc(128), 256) for _ in range(4)]
    attn = f32v(alloc(512), 512)
    attnb = bf16v(alloc(256), 512)
    mgB = bf16v(alloc(256), 512).rearrange("p (c n) -> p c n", c=4)
    pm4 = f32v(alloc(512), 512)
    TbC = bf16v(alloc(5 * 8 * 64), 5 * 8 * 128).rearrange("p (a h q) -> p a h q", a=5, h=8)
    bis = f32v(alloc(32), 32)
    fin = f32v(alloc(32), 32)
    scr = f32v(alloc(8), 8)
    steps = f32v(alloc(16), 16)
    psb5b = psb[5][:, 0:256].bitcast(BF16)
    tKI, tqown, tS, tattn, tattnb, tbis, tsteps, tpm, tmgB, tTbC, tfin, tscr, tscr2 = [Tok() for _ in range(13)]
    tmask = [Tok() for _ in range(4)]
    tR = [Tok(), Tok()]
    tqi = [Tok(), Tok()]
    tkv = [Tok() for _ in range(4)]
    tmT = [Tok(), Tok()]
    tE = [Tok() for _ in range(4)]
    tPm = [Tok() for _ in range(4)]
    tST = [Tok() for _ in range(4)]
    dma("sp", KI[0:64, :], KId[:, 0:NKB * 128], [], [tKI])
    dma("sp", qown, qd[:, :, 0:NA * 128], [], [tqown])
    dma("sp", pm4, posm_d.rearrange("p k n -> p (k n)"), [], [tpm])
    for a in range(5):
        for h in range(8):
            ts("dve", TbC[:, a, h, :], Tb[:, 0, h, :], vecs[:, V_SD + a:V_SD + a + 1], None, ALU.mult, None,
               [tTb, tvec], [tTbC])
            stt(TbC[:, a, h, :], Tb[:, 1, h, :], vecs[:, V_SP + a:V_SP + a + 1], TbC[:, a, h, :],
                ALU.mult, ALU.add, [tTb, tvec, tTbC], [tTbC])

    def indexer(j):
        mslot = j % 4
        qs = j % 2
        nk = (j + 1) * 512
        dma("sp", qi[qs][0:64], qid[:, j, :, :], [], [tqi[qs]])
        for kc0 in range(0, nk, 512):
            for h in range(8):
                mm(psb[5][:, 0:512], qi[qs][0:64, h, :], KI[0:64, kc0:kc0 + 512], True, True,
                   [tqi[qs], tKI], [pst[5]])
                R = Rt[h % 2]
                act(R, psb[5][:, 0:512], AF.Relu, [pst[5], tws], [tR[h % 2]],
                    scale=wabs[:, j * 8 + h:j * 8 + h + 1])
                sg = wsgn[:, j * 8 + h:j * 8 + h + 1]
                if h == 0:
                    ts("dve", Sb[:, kc0:kc0 + 512], R, sg, None, ALU.mult, None, [tR[h % 2], tws], [tS])
                else:
                    stt(Sb[:, kc0:kc0 + 512], R, sg, Sb[:, kc0:kc0 + 512], ALU.mult, ALU.add,
                        [tR[h % 2], tws, tS], [tS])
                yield
        P.op("dve", lambda e, o=bis[:, 0:1], i=Sb[:, 0:nk]:
             e.tensor_reduce(out=o, in_=i, axis=AX.X, op=ALU.max, apply_absolute_value=True),
             [tS], [tbis])
        base = j * 512
        tt("dve", Sb[:, base:base + 512], Sb[:, base:base + 512], pm4, ALU.add, [tS, tpm], [tS])
        hi0, lo, w0, mid, cnt, gs = [bis[:, i:i + 1] for i in range(1, 7)]
        ts("dve", hi0, bis[:, 0:1], 1.001, 1e-6, ALU.mult, ALU.add, [tbis], [tbis])
        ts("dve", lo, hi0, -1.0, None, ALU.mult, None, [tbis], [tbis])
        ts("dve", w0, hi0, 2.0, None, ALU.mult, None, [tbis], [tbis])
        ts("dve", steps, vecs[:, V_P2:V_P2 + 16], w0, None, ALU.mult, None, [tbis, tvec], [tsteps])
        yield
        junk = maskb[mslot][:, 0:nk]
        for k in range(NBIS):
            tt("dve", mid, lo, steps[:, k:k + 1], ALU.add, [tbis, tsteps], [tbis])
            ts("dve", junk, Sb[:, 0:nk], mid, None, ALU.is_ge, ALU.add, [tS, tbis], [tmask[mslot], tbis],
               accum=cnt)
            P.op("dve", lambda e: e.memset(scr[:, 0:1], 0.0), [tbis], [tbis])
            ts("dve", gs, cnt, 255.5, steps[:, k:k + 1], ALU.is_ge, ALU.mult, [tbis, tsteps], [tbis])
            tt("dve", lo, lo, gs, ALU.add, [tbis], [tbis])
            yield
        ts("dve", junk, Sb[:, 0:nk], lo, None, ALU.is_ge, None, [tS, tbis], [tmask[mslot]])
        yield

    def drain(gen, n=None):
        if gen is None:
            return
        try:
            if n is None:
                while True:
                    next(gen)
            else:
                for _ in range(n):
                    next(gen)
        except StopIteration:
            pass

    def chain(*gens):
        for g_ in gens:
            for _ in g_:
                yield

    def attention(j0, nq, bg, bg_yields=0):
        nkb_of = [(j0 + t + 1) * 4 for t in range(nq)]
        nkb = nkb_of[-1]
        first = [True] * 4
        c1 = nq * 128

        def ldkv(kb):
            s = kb % 4
            dma("sp", kts[s], Kd[:, :, kb * 128:(kb + 1) * 128], [], [tkv[s]])
            dma("sp", vts[s].rearrange("p h e -> p (h e)"), Vd[kb * 128:(kb + 1) * 128, :], [], [tkv[s]])

        def t0_of(kb):
            t0 = 0
            while kb >= nkb_of[t0]:
                t0 += 1
            return t0
        steps = [(kb, h) for kb in range(nkb) for h in range(8)]
        LAG = 2

        def front(i):
            kb, h = steps[i]
            s = kb % 4
            t0 = t0_of(kb)
            c0 = t0 * 128
            mT = mTs[kb % 2]
            if h == 0:
                if kb == 0:
                    ldkv(0)
                    ldkv(1)
                if kb + 2 < nkb:
                    ldkv(kb + 2)
                for t in range(t0, nq):
                    ms = (j0 + t) % 4
                    tr(psb5b[:, t * 128:(t + 1) * 128], maskb[ms][:, kb * 128:(kb + 1) * 128],
                       [tmask[ms], tvec], [pst[5]])
                cp("act", mT[:, c0:c1], psb5b[:, c0:c1], [pst[5]], [tmT[kb % 2]])
            po = (h % 2) * 64
            slot4 = i % 4
            pbank = (0, 6, 7)[i % 3]
            pcol = 0
            ps_s = psb[pbank][:, pcol + c0:pcol + c1]
            mm(ps_s, kts[s][po:po + 64, h // 2, :],
               qown[po:po + 64, h // 2, j0 * 128 + c0:j0 * 128 + c1],
               True, True, [tkv[s], tqown], [pst[pbank]])
            for t in range(t0, nq):
                a = kb - 4 * (j0 + t) + 1
                if 0 <= a <= 4:
                    sl = psb[pbank][:, pcol + t * 128:pcol + (t + 1) * 128]
                    tt("dve", sl, sl, TbC[:, a, h, :], ALU.add, [pst[pbank], tTbC], [pst[pbank]])
            E = Es[slot4]
            act(E[:, c0:c1], ps_s, AF.Exp, [pst[pbank], tvec], [tE[slot4]],
                bias=vecs[:, V_RB + 15 * 8 + h:V_RB + 15 * 8 + h + 1], scale=0.125)
            Pm = Pms[slot4]
            tt("pool", Pm[:, c0:c1], E[:, c0:c1], mT[:, c0:c1], ALU.mult,
               [tE[slot4], tmT[kb % 2]], [tPm[slot4]])

        def back(i):
            kb, h = steps[i]
            s = kb % 4
            slot4 = i % 4
            Pm = Pms[slot4]
            for t in range(t0_of(kb), nq):
                bank = 1 + t * 2 + (h // 4)
                col = (h % 4) * 65
                st_ = first[t * 2 + h // 4]
                first[t * 2 + h // 4] = False
                P.op("pe", lambda e, o=psb[bank][:, col:col + 65], l=Pm[:, t * 128:(t + 1) * 128],
                     r=vts[s][:, h, :], s_=st_:
                     e.matmul(o, lhsT=l, rhs=r, start=s_, stop=False, skip_group_check=True),
                     [tPm[slot4], tkv[s]], [pst[bank]])

        rate = float(bg_yields) / float(len(steps)) * 1.05
        accd = 0.0
        for i in range(len(steps) + LAG):
            if i < len(steps):
                front(i)
            if i >= LAG:
                back(i - LAG)
            accd += rate
            while accd >= 1.0:
                drain(bg, 1)
                accd -= 1.0
        for t in range(nq):
            jt = j0 + t
            for hh in range(2):
                bank = 1 + t * 2 + hh
                v = psb[bank][:, 0:260].rearrange("p (h e) -> p h e", h=4)
                rc = fin[:, 8 + hh * 4:12 + hh * 4]
                cp("dve", rc, v[:, :, 64], [pst[bank]], [tfin])
                P.op("dve", lambda e, o=rc: e.reciprocal(out=o, in_=o), [tfin], [tfin])
                for h4 in range(4):
                    h = hh * 4 + h4
                    ts("dve", attn[:, h * 64:(h + 1) * 64], v[:, h4, 0:64], fin[:, 8 + h:9 + h], None,
                       ALU.mult, None, [pst[bank], tfin], [tattn])
            act(attnb, attn, AF.Square, [tattn], [tattnb, tfin], accum=fin[:, 16:17])
            P.op("act", lambda e: e.copy(out=scr[:, 4:5], in_=scr[:, 5:6]), [tfin], [tfin])
            act(fin[:, 17:18], fin[:, 16:17], AF.Sqrt, [tfin], [tfin], bias=EPS, scale=1.0 / 512)
            P.op("dve", lambda e, o=fin[:, 17:18]: e.reciprocal(out=o, in_=o), [tfin], [tfin])
            ts("dve", attnb, attn, fin[:, 17:18], None, ALU.mult, None, [tattn, tfin, tattnb], [tattnb])
            for c in range(4):
                tr(psb5b[:, c * 128:(c + 1) * 128], attnb[:, c * 128:(c + 1) * 128], [tattnb, tvec], [pst[5]])
            for c in range(4):
                act(mgB[:, c, :], psb5b[:, c * 128:(c + 1) * 128], AF.Identity, [pst[5], tvec], [tmgB],
                    scale=vecs[:, V_AOG + c:V_AOG + c + 1])
            dma("sp", mgd[:, 0:4, jt * 128:(jt + 1) * 128], mgB, [tmgB], [])

    P.op("dve", lambda e: e.memset(scr, 0.0), [], [tbis])
    sbs = list(range(0, NA, 2))
    drain(chain(*[indexer(j) for j in range(sbs[0], min(sbs[0] + 2, NA))]))
    for si, j0 in enumerate(sbs):
        nq = min(2, NA - j0)
        bg = None
        if si + 1 < len(sbs):
            j1 = sbs[si + 1]
            bg = chain(*[indexer(j) for j in range(j1, min(j1 + 2, NA))])
            ny = sum((j + 1) * 8 + NBIS + 2 for j in range(j1, min(j1 + 2, NA)))
        else:
            ny = 0
        attention(j0, nq, bg, ny)
        drain(bg)

    P.barrier()
    cur[0] = mark_a
    if stop == 'B':
        return finish()

    outT_v = outT.rearrange("(kc p) t -> p kc t", p=128)
    for ct in range(NA // 4):
        C0 = ct * N
        dma("sp", xt, x1d[:, :, C0:C0 + N], [], [txt])
        dma("sp", hb, mgd[:, :, C0:C0 + N], [], [thb])

        def ldo(dc):
            s = wislots[dc % 3]
            dma("sp", s[0], wout_t[dc], [tw["wout"]], [s[1]])
        ldo(0); ldo(1)
        for dc in range(8):
            if dc + 2 < 8:
                ldo(dc + 2)
            s = wislots[dc % 3]
            pb = 1 + dc % 2
            for kc in range(8):
                mm(psb[pb][:, 0:N], s[0][:, kc, :], hb[:, kc, :], kc == 0, kc == 7, [s[1], thb], [pst[pb]])
            cp("act" if dc % 2 == 0 else "dve", ybuf[:, dc, :], psb[pb][:, 0:N], [pst[pb]], [tyb])
        post_res(1, xt, txt)
        norm_mod(xt, txt, hb, thb, der[:, D_SC[2]:D_SC[2] + 8], der[:, D_SH[2]:D_SH[2] + 8])
        ffn2(1)
        post_res(2, xt, txt)
        dma("sp", outT_v[:, :, C0:C0 + N], xt, [txt], [])
    P.barrier()
    with nc.Block() as block:
        P.emit(block)
    st.close()
    return nc


def _t5_bucket_np(rel):
    rel = np.asarray(rel, np.int32)
    nb = 16
    max_exact = 8
    ret = np.where(rel > 0, nb, 0).astype(np.int32)
    n = np.abs(rel)
    nf = np.maximum(n, 1).astype(np.float32)
    large = max_exact + (np.log(nf / np.float32(max_exact)) / np.float32(math.log(128 / max_exact))
                         * np.float32(nb - max_exact)).astype(np.int32)
    large = np.minimum(large, nb - 1)
    return ret + np.where(n < max_exact, n, large)


_NC_CACHE = {}


def _host_inputs(inputs, NA=16):
    f = lambda a: np.ascontiguousarray(np.asarray(a, np.float32))
    x = f(inputs["x"]); c = f(inputs["c"])

    def col8(v):
        return v.reshape(8, 128).T

    def col4(v):
        return v.reshape(4, 128).T
    kl = np.arange(128)[:, None]; ql = np.arange(128)[None, :]
    bd = _t5_bucket_np(kl - ql)
    bp = _t5_bucket_np(kl - ql - 128)
    mb = np.zeros((128, 2, 32, 128), np.float32)
    for b in range(32):
        mb[:, 0, b, :] = (bd == b)
        mb[:, 1, b, :] = (bp == b)
    mb = mb.astype(ml_dtypes.bfloat16)
    ident = np.eye(128, dtype=np.float32).astype(ml_dtypes.bfloat16)
    wblk = np.zeros((128, 2, 4, 128), np.float32)
    for ai, w in enumerate((f(inputs["lru_w_a"])[0], f(inputs["lru_w_i"])[0])):
        for g in range(8):
            cch, half = g // 2, g % 2
            wblk[half * 64:(half + 1) * 64, ai, cch, half * 64:(half + 1) * 64] = w[g]
    shared = {
        "w_ada": f(inputs["w_ada"])[0],
        "wg1": f(inputs["ffn1_w_gate"])[0], "wu1": f(inputs["ffn1_w_up"])[0], "wd1": f(inputs["ffn1_w_down"])[0],
        "wg2": f(inputs["ffn2_w_gate"])[0], "wu2": f(inputs["ffn2_w_up"])[0], "wd2": f(inputs["ffn2_w_down"])[0],
        "w_in": f(inputs["w_in"])[0], "w_out": f(inputs["w_out"])[0],
        "wblk": wblk, "mb": mb, "ident": ident,
    }
    xTs = [np.ascontiguousarray(x[b].T) for b in range(2)]
    in_maps = []
    for core in range(8):
        b, r = core // 4, core % 4
        v = np.zeros((128, NV), np.float32)
        v[:, V_C:V_C + 8] = col8(c[b])
        v[:, V_BADA:V_BADA + 72] = f(inputs["b_ada"])[0].reshape(72, 128).T
        for i, nm in enumerate(["ffn1_pre_g", "ffn1_post_g", "mix_pre_g", "mix_post_g", "ffn2_pre_g", "ffn2_post_g"]):
            v[:, V_G + 8 * i:V_G + 8 * i + 8] = col8(f(inputs[nm])[0])
        v[:, V_AOG:V_AOG + 4] = col4(f(inputs["attn_out_g"])[0])
        v[:, V_LOG:V_LOG + 4] = col4(f(inputs["lru_out_g"])[0])
        cw = f(inputs["lru_conv_w"])[0]
        for j in range(4):
            v[:, V_CW + 4 * j:V_CW + 4 * j + 4] = col4(cw[j])
        v[:, V_CB:V_CB + 4] = col4(f(inputs["lru_conv_b"])[0])
        v[:, V_BA:V_BA + 4] = col4(f(inputs["lru_b_a"])[0])
        v[:, V_BI:V_BI + 4] = col4(f(inputs["lru_b_i"])[0])
        v[:, V_LAM:V_LAM + 4] = col4(f(inputs["lru_lambda"])[0])
        v[:, V_RB:V_RB + 256] = f(inputs["rel_bias"]).reshape(1, 256)
        v[:, V_P2:V_P2 + 16] = (0.5 ** np.arange(1, 17))[None, :]
        v[:, V_SEL + r] = 1.0
        v[:, V_SD + r + 1] = 1.0
        v[:, V_SP + r] = 1.0
        posm = np.zeros((128, 4, 128), np.float32)
        for kb in range(4):
            if kb > r:
                posm[:, kb, :] = -BIG
            elif kb == r:
                posm[0:64, kb, 64:128] = -BIG
        m = dict(shared)
        m["xT"] = xTs[b]
        m["vecs"] = v
        m["posm"] = posm
        in_maps.append(m)
    return in_maps


def kernel(**inputs):
    NA = 16
    if NA not in _NC_CACHE:
        _NC_CACHE[NA] = build(NA)
    nc = _NC_CACHE[NA]
    in_maps = _host_inputs(inputs, NA)
    res = run_bass_kernel_spmd(nc, in_maps, core_ids=list(range(8)))
    out = np.zeros((2, S, D), np.float32)
    for core in range(8):
        b, r = core // 4, core % 4
        o = np.asarray(res.results[core]["outT"], np.float32)
        o = o.T.reshape(NBLK, 128, D)
        for j in range(NBLK):
            g = 4 * j + r
            out[b, g * 128:(g + 1) * 128, :] = o[j]
    return out
```

```python
import math
import numpy as np
import ml_dtypes
from contextlib import ExitStack
import concourse.bass as bass
import concourse.mybir as mybir
from concourse.bass_utils import run_bass_kernel_spmd

F32 = mybir.dt.float32
BF16 = mybir.dt.bfloat16
ALU = mybir.AluOpType
AF = mybir.ActivationFunctionType
AX = mybir.AxisListType

D = 1024
S = 8192
DFF = 2816
NF = DFF // 128
DIN = 3144
NBLK = 16
EPS = 1e-6
NBIS = 16
BIG = 3.0e38

V_C = 0
V_BADA = 8
V_G = 80
V_AOG = 128
V_LOG = 132
V_CW = 136
V_CB = 152
V_BA = 156
V_BI = 160
V_LAM = 164
V_RB = 168
V_P2 = 424
V_SEL = 440
V_SD = 444
V_SP = 449
NV = 456


class Tok:
    __slots__ = ("w", "r")

    def __init__(self):
        self.w = None
        self.r = []


class Prog:
    COMPUTE = ("pe", "act", "dve", "pool")
    NDMASEM = 24

    def __init__(self, nc, stack):
        self.nc = nc
        self.streams = {k: [] for k in ("pe", "act", "dve", "pool", "sp")}
        self.count = {k: 0 for k in self.COMPUTE}
        self.sems = {k: stack.enter_context(nc.semaphore("s_" + k)) for k in self.COMPUTE}
        self.dsems = [stack.enter_context(nc.semaphore("d%d" % i)) for i in range(self.NDMASEM)]
        self.dcount = [0] * self.NDMASEM
        self.dnext = 0
        self.waited = {k: {} for k in self.streams}
        self.ninstr = 0

    def _deps(self, stream, reads, writes):
        out = {}

        def add(kv):
            k, v = kv
            if k == stream and k == "pe":
                return
            if out.get(k, 0) < v:
                out[k] = v
        for t in reads:
            if t.w is not None:
                add(t.w)
        for t in writes:
            if t.w is not None:
                add(t.w)
            for kv in t.r:
                if kv[0] == stream and not isinstance(kv[0], int):
                    continue
                add(kv)
        waits = []
        wd = self.waited[stream]
        for k, v in out.items():
            if wd.get(k, 0) >= v:
                continue
            wd[k] = v
            waits.append((k, v))
        return waits

    def _semof(self, k):
        return self.dsems[k] if isinstance(k, int) else self.sems[k]

    def op(self, eng, fn, reads=(), writes=()):
        waits = self._deps(eng, reads, writes)
        self.count[eng] += 1
        idx = self.count[eng]
        self.streams[eng].append((waits, fn, ("c", eng)))
        for t in reads:
            t.r.append((eng, idx))
            if len(t.r) > 600:
                t.r = t.r[-400:]
        for t in writes:
            t.w = (eng, idx)
            t.r = []
        self.ninstr += 1

    def dma(self, q, fn, reads=(), writes=()):
        s = self.dnext
        self.dnext = (self.dnext + 1) % self.NDMASEM
        waits = self._deps(q, reads, writes)
        prev = self.dcount[s]
        if prev > 0 and self.waited[q].get(s, 0) < prev:
            self.waited[q][s] = prev
            waits.append((s, prev))
        self.dcount[s] += 16
        val = self.dcount[s]
        self.streams[q].append((waits, fn, ("d", s)))
        for t in reads:
            t.r.append((s, val))
        for t in writes:
            t.w = (s, val)
            t.r = []
        self.ninstr += 1

    def barrier(self):
        for st in self.streams:
            waits = []
            for k in self.COMPUTE:
                v = self.count[k]
                if v > 0 and self.waited[st].get(k, 0) < v and k != st:
                    self.waited[st][k] = v
                    waits.append((k, v))
            for s in range(self.NDMASEM):
                v = self.dcount[s]
                if v > 0 and self.waited[st].get(s, 0) < v:
                    self.waited[st][s] = v
                    waits.append((s, v))
            if waits:
                self.streams[st].append((waits, None, None))

    def emit(self, block):
        prog = self

        def run(stream, h):
            for waits, fn, kind in prog.streams[stream]:
                for (k, v) in waits:
                    h.wait_ge(prog._semof(k), v)
                if fn is None:
                    continue
                ins = fn(h)
                if kind[0] == "c":
                    ins.then_inc(prog.sems[kind[1]], 1)
                else:
                    ins.then_inc(prog.dsems[kind[1]], 16)

        @block.tensor
        def _(e):
            run("pe", e)

        @block.scalar
        def _(e):
            run("act", e)

        @block.vector
        def _(e):
            run("dve", e)

        @block.gpsimd
        def _(e):
            run("pool", e)

        @block.sync
        def _(e):
            run("sp", e)


def build(NA=16, stop=None, debug=False):
    nc = bass.Bass("TRN2", target_bir_lowering=False)
    st = ExitStack()
    P = Prog(nc, st)

    def dram_in(name, shape, dt=F32):
        return nc.dram_tensor(name, list(shape), dt, kind="ExternalInput").ap()

    def dram_tmp(name, shape, dt):
        if debug and name in ("Kd", "Vd", "KId", "x1d", "qd", "qid", "mgd"):
            return nc.dram_tensor(name, list(shape), dt, kind="ExternalOutput").ap()
        return nc.dram_tensor(name, list(shape), dt).ap()

    xT = dram_in("xT", [D, S])
    vecs_d = dram_in("vecs", [128, NV])
    w_ada = dram_in("w_ada", [D, 9 * D])
    wg1 = dram_in("wg1", [D, DFF]); wu1 = dram_in("wu1", [D, DFF]); wd1 = dram_in("wd1", [DFF, D])
    wg2 = dram_in("wg2", [D, DFF]); wu2 = dram_in("wu2", [D, DFF]); wd2 = dram_in("wd2", [DFF, D])
    w_in = dram_in("w_in", [D, DIN])
    w_out = dram_in("w_out", [D, D])
    wblk_d = dram_in("wblk", [128, 2, 4, 128])
    mb_d = dram_in("mb", [128, 2, 32, 128], BF16)
    ident_d = dram_in("ident", [128, 128], BF16)
    posm_d = dram_in("posm", [128, 4, 128])
    outT = nc.dram_tensor("outT", [D, NBLK * 128], F32, kind="ExternalOutput").ap()

    wg_t = [dram_tmp("wg_t%d" % i, [NF, 128, 8, 128], BF16) for i in range(2)]
    wu_t = [dram_tmp("wu_t%d" % i, [NF, 128, 8, 128], BF16) for i in range(2)]
    wd_t = [dram_tmp("wd_t%d" % i, [8, 128, NF, 128], BF16) for i in range(2)]
    win_t = dram_tmp("win_t", [16, 128, 8, 128], BF16)
    wout_t = dram_tmp("wout_t", [8, 128, 8, 128], BF16)
    Kd = dram_tmp("Kd", [128, 4, S], BF16)
    Vd = dram_tmp("Vd", [S, 520], BF16)
    KId = dram_tmp("KId", [64, S], F32)
    x1d = dram_tmp("x1d", [128, 8, NBLK * 128], F32)
    qd = dram_tmp("qd", [128, 4, NBLK * 128], BF16)
    qid = dram_tmp("qid", [64, NBLK, 8, 128], F32)
    mgd = dram_tmp("mgd", [128, 8, NBLK * 128], BF16)

    AW = 52600
    arena = st.enter_context(nc.sbuf_tensor("arena", [128, AW], F32))
    cur = [0]

    def alloc(words):
        a = cur[0]
        cur[0] += words
        assert cur[0] <= AW, ("sbuf overflow", cur[0])
        return a

    def f32v(off, n):
        return arena[:, off:off + n]

    def bf16v(off, nbf):
        return arena[:, off:off + nbf // 2].bitcast(BF16)

    vecs = f32v(alloc(NV), NV)
    der = f32v(alloc(128), 128)
    modT = f32v(alloc(72), 72)
    wabs = f32v(alloc(NBLK * 8), NBLK * 8)
    wsgn = f32v(alloc(NBLK * 8), NBLK * 8)
    identb = bf16v(alloc(64), 128)
    onesb = bf16v(alloc(64), 128)
    Tb = f32v(alloc(2048), 2048).rearrange("p (t h q) -> p t h q", t=2, h=8)
    lstate = f32v(alloc(4), 4)
    base_mark = cur[0]

    D_SC = [0, 16, 32]
    D_SH = [8, 24, 40]
    D_GT = [48, 56, 64]
    D_CA = 72
    D_MISC = 76

    psb = [st.enter_context(nc.psum_tensor("ps%d" % i, [128, 512], F32)) for i in range(8)]
    pst = [Tok() for _ in range(8)]

    tvec, tder, tmod = Tok(), Tok(), Tok()

    def mm(out, lhsT, rhs, start, stop, reads, writes):
        P.op("pe", lambda e, o=out, l=lhsT, r=rhs, s=start, t=stop:
             e.matmul(o, lhsT=l, rhs=r, start=s, stop=t), reads, writes)

    def tr(out, in_, reads, writes):
        P.op("pe", lambda e, o=out, i=in_: e.transpose(o, i, identb), reads, writes)

    def act(out, in_, func, reads, writes, bias=0.0, scale=1.0, accum=None):
        if accum is None:
            P.op("act", lambda e, o=out, i=in_, f=func, b=bias, s=scale:
                 e.activation(out=o, in_=i, func=f, bias=b, scale=s), reads, writes)
        else:
            P.op("act", lambda e, o=out, i=in_, f=func, b=bias, s=scale, a=accum:
                 e.activation(out=o, in_=i, func=f, bias=b, scale=s, accum_out=a), reads, writes)

    def tt(eng, out, in0, in1, op, reads, writes):
        P.op(eng, lambda e, o=out, a=in0, b=in1, p=op: e.tensor_tensor(out=o, in0=a, in1=b, op=p),
             reads, writes)

    def ts(eng, out, in0, s1, s2, op0, op1, reads, writes, accum=None):
        if op1 is None:
            op1 = ALU.bypass
        if accum is None:
            P.op(eng, lambda e, o=out, a=in0, x=s1, y=s2, p=op0, q=op1:
                 e.tensor_scalar(out=o, in0=a, scalar1=x, scalar2=y, op0=p, op1=q), reads, writes)
        else:
            P.op(eng, lambda e, o=out, a=in0, x=s1, y=s2, p=op0, q=op1, ac=accum:
                 e.tensor_scalar(out=o, in0=a, scalar1=x, scalar2=y, op0=p, op1=q, accum_out=ac),
                 reads, writes)

    def stt(out, in0, scalar, in1, op0, op1, reads, writes):
        P.op("dve", lambda e, o=out, a=in0, s=scalar, b=in1, p=op0, q=op1:
             e.scalar_tensor_tensor(out=o, in0=a, scalar=s, in1=b, op0=p, op1=q), reads, writes)

    def cp(eng, out, in_, reads, writes):
        if eng == "act":
            P.op("act", lambda e, o=out, i=in_: e.copy(out=o, in_=i), reads, writes)
        else:
            P.op(eng, lambda e, o=out, i=in_: e.tensor_copy(out=o, in_=i), reads, writes)

    def dma(q, out, in_, reads, writes):
        P.dma(q, lambda e, o=out, i=in_: e.dma_start(out=o, in_=i), reads, writes)

    dma("sp", vecs, vecs_d, [], [tvec])
    dma("sp", identb, ident_d, [], [tvec])
    P.op("dve", lambda e: e.memset(onesb, 1.0), [], [tvec])

    tw = {}

    def cast_w(name, dst, src_view, n):
        t = Tok()
        tw[name] = t
        for i in range(n):
            dma("pool", dst[i], src_view[i], [], [t])

    def cast_ffn(i, wg, wu, wd):
        cast_w("wg%d" % i, wg_t[i], wg.rearrange("(kc p) (f c) -> f p kc c", p=128, c=128), NF)
        cast_w("wu%d" % i, wu_t[i], wu.rearrange("(kc p) (f c) -> f p kc c", p=128, c=128), NF)
        cast_w("wd%d" % i, wd_t[i], wd.rearrange("(f p) (dc c) -> dc p f c", p=128, c=128), 8)

    cast_ffn(0, wg1, wu1, wd1)
    win_cols = [0, 128, 256, 384, 512, 640, 768, 896, 2120, 2248, 2376, 2504, 2632, 2760, 2888, 3016]
    t = Tok(); tw["win"] = t
    for i, c0 in enumerate(win_cols):
        dma("pool", win_t[i], w_in[:, c0:c0 + 128].rearrange("(kc p) c -> p kc c", p=128), [], [t])

    scv = f32v(alloc(8), 8)
    tsc = Tok()
    act(scv, vecs[:, V_C:V_C + 8], AF.Silu, [tvec], [tsc])
    wa_off = alloc(2 * 8 * 1152)
    wa_buf = [f32v(wa_off + i * 9216, 9216).rearrange("p (k c) -> p k c", k=8) for i in range(2)]
    twa = [Tok(), Tok()]
    w_ada_v = w_ada.rearrange("(kc p) n -> p kc n", p=128)
    for cb in range(8):
        buf = wa_buf[cb % 2]
        dma("sp", buf, w_ada_v[:, :, cb * 1152:(cb + 1) * 1152], [], [twa[cb % 2]])
        for cc in range(9):
            col = cb * 9 + cc
            for kc in range(8):
                mm(psb[0][:, col:col + 1], buf[:, kc, cc * 128:(cc + 1) * 128], scv[:, kc:kc + 1],
                   kc == 0, kc == 7, [twa[cb % 2], tsc], [pst[0]])
    tt("dve", modT, psb[0][:, 0:72], vecs[:, V_BADA:V_BADA + 72], ALU.add, [pst[0], tvec], [tmod])
    for i, (jsh, jsc, jg, gpre, gpost, gmul) in enumerate(
            [(0, 1, 2, 0, 1, 0.5), (3, 4, 5, 2, 3, 1.0), (6, 7, 8, 4, 5, 0.5)]):
        stt(der[:, D_SC[i]:D_SC[i] + 8], modT[:, jsc * 8:jsc * 8 + 8], 1.0,
            vecs[:, V_G + gpre * 8:V_G + gpre * 8 + 8], ALU.add, ALU.mult, [tmod, tvec], [tder])
        cp("dve", der[:, D_SH[i]:D_SH[i] + 8], modT[:, jsh * 8:jsh * 8 + 8], [tmod], [tder])
        stt(der[:, D_GT[i]:D_GT[i] + 8], modT[:, jg * 8:jg * 8 + 8], gmul,
            vecs[:, V_G + gpost * 8:V_G + gpost * 8 + 8], ALU.mult, ALU.mult, [tmod, tvec], [tder])
    tmp4 = der[:, D_MISC:D_MISC + 4]
    act(tmp4, vecs[:, V_LAM:V_LAM + 4], AF.Exp, [tvec], [tder], scale=-1.0)
    act(tmp4, tmp4, AF.Ln, [tder], [tder], bias=1.0)
    ts("dve", der[:, D_CA:D_CA + 4], tmp4, -8.0, None, ALU.mult, None, [tder], [tder])
    P.op("dve", lambda e: e.memset(lstate, 0.0), [], [tder])

    mb_off = alloc(2 * 32 * 128 // 2)
    mbv = bf16v(mb_off, 2 * 32 * 128).rearrange("p (t b q) -> p t b q", t=2, b=32)
    tmb, tTb = Tok(), Tok()
    dma("sp", mbv, mb_d, [], [tmb])
    for ty in range(2):
        for h in range(8):
            T = Tb[:, ty, h, :]
            ts("dve", T, mbv[:, ty, 0, :], vecs[:, V_RB + h:V_RB + h + 1],
               vecs[:, V_RB + 15 * 8 + h:V_RB + 15 * 8 + h + 1], ALU.mult, ALU.subtract,
               [tmb, tvec], [tTb])
            for b in range(1, 32):
                stt(T, mbv[:, ty, b, :], vecs[:, V_RB + b * 8 + h:V_RB + b * 8 + h + 1], T,
                    ALU.mult, ALU.add, [tmb, tvec, tTb], [tTb])
            ts("dve", T, T, 8.0, None, ALU.mult, None, [tTb], [tTb])
    cast_ffn(1, wg2, wu2, wd2)
    cast_w("wout", wout_t, w_out.rearrange("(kc p) (dc c) -> dc p kc c", p=128, c=128), 8)
    P.barrier()
    cur[0] = base_mark


    def finish():
        P.barrier()
        with nc.Block() as block:
            P.emit(block)
        st.close()
        return nc
    if stop == 'pro':
        return finish()
    def norm_stats(src, nk, N, sq, tsrc, tsq, rstd, trstd, ps_i, dscale):
        act(sq, src, AF.Square, [tsrc], [tsq])
        for k in range(nk):
            mm(psb[ps_i][:, 0:N], onesb, sq[:, k, :], k == 0, k == nk - 1, [tsq, tvec], [pst[ps_i]])
        act(rstd, psb[ps_i][:, 0:N], AF.Sqrt, [pst[ps_i]], [trstd], bias=EPS, scale=dscale)
        P.op("dve", lambda e, o=rstd: e.reciprocal(out=o, in_=o), [trstd], [trstd])

    def ffn(i, N, hb, thb, actb, tact, wslots, dslots, ybuf, tyb, sgb, tsg, bg=None):
        def ld(f):
            s = wslots[f % 3]
            dma("sp", s[0], wg_t[i][f], [tw["wg%d" % i]], [s[2]])
            dma("sp", s[1], wu_t[i][f], [tw["wu%d" % i]], [s[2]])
        ld(0); ld(1)
        for f in range(NF):
            if f + 2 < NF:
                ld(f + 2)
            s = wslots[f % 3]
            pg, pu = (1, 2) if f % 2 == 0 else (3, 4)
            for kc in range(8):
                mm(psb[pg][:, 0:N], s[0][:, kc, :], hb[:, kc, :], kc == 0, kc == 7, [s[2], thb], [pst[pg]])
            for kc in range(8):
                mm(psb[pu][:, 0:N], s[1][:, kc, :], hb[:, kc, :], kc == 0, kc == 7, [s[2], thb], [pst[pu]])
            sg = sgb[f % 2]
            act(sg, psb[pg][:, 0:N], AF.Silu, [pst[pg]], [tsg[f % 2]])
            tt("dve", actb[:, f, :], sg, psb[pu][:, 0:N], ALU.mult, [tsg[f % 2], pst[pu]], [tact[f]])
            if bg is not None:
                for _ in range(2):
                    next(bg, None)
        if bg is not None:
            for _ in bg:
                pass

        def ldd(dc):
            s = dslots[dc % 2]
            dma("sp", s[0], wd_t[i][dc], [tw["wd%d" % i]], [s[1]])
        ldd(0)
        for dc in range(8):
            if dc + 1 < 8:
                ldd(dc + 1)
            s = dslots[dc % 2]
            pa = 5 + dc % 2
            for f in range(NF):
                mm(psb[pa][:, 0:N], s[0][:, f, :], actb[:, f, :], f == 0, f == NF - 1,
                   [s[1], tact[f]], [pst[pa]])
            cp("act" if dc % 2 == 0 else "dve", ybuf[:, dc, :], psb[pa][:, 0:N], [pst[pa]], [tyb])

    N = 512
    xt = f32v(alloc(8 * N), 8 * N).rearrange("p (k n) -> p k n", k=8)
    hb = bf16v(alloc(4 * N), 8 * N).rearrange("p (k n) -> p k n", k=8)
    sq = bf16v(alloc(4 * N), 8 * N).rearrange("p (k n) -> p k n", k=8)
    actb = bf16v(alloc(NF * N // 2), NF * N).rearrange("p (f n) -> p f n", f=NF)
    yoff = alloc(8 * N)
    ybuf = f32v(yoff, 8 * N).rearrange("p (k n) -> p k n", k=8)
    ltoff = alloc(6 * N)
    lt = [f32v(ltoff + i * N, N) for i in range(6)]
    wqi = f32v(yoff, 8 * 512).rearrange("p (k c) -> p k c", k=8)
    h2 = f32v(alloc(8 * N), 8 * N).rearrange("p (k n) -> p k n", k=8)
    rstd = f32v(alloc(N), N)
    sgb = [f32v(alloc(N), N) for _ in range(2)]
    tsg = [Tok(), Tok()]
    wslots = []
    for i in range(3):
        o = alloc(1024)
        wslots.append((bf16v(o, 1024).rearrange("p (k c) -> p k c", k=8),
                       bf16v(o + 512, 1024).rearrange("p (k c) -> p k c", k=8), Tok()))
    dslots = []
    for i in range(2):
        o = alloc(NF * 64)
        dslots.append((bf16v(o, NF * 128).rearrange("p (f c) -> p f c", f=NF), Tok()))
    wislots = []
    for i in range(3):
        o = alloc(512)
        wislots.append((bf16v(o, 1024).rearrange("p (k c) -> p k c", k=8), Tok()))
    wv = bf16v(alloc(8 * 256), 8 * 512).rearrange("p (k c) -> p k c", k=8)
    wki = f32v(alloc(8 * 64), 8 * 64).rearrange("p (k c) -> p k c", k=8)
    wwi = f32v(alloc(8 * 8), 8 * 8).rearrange("p (k c) -> p k c", k=8)
    wblk = bf16v(alloc(2 * 4 * 64), 2 * 4 * 128).rearrange("p (a c q) -> p a c q", a=2, c=4)
    xl = f32v(alloc(4 * (N + 4)), 4 * (N + 4)).rearrange("p (c n) -> p c n", c=4)
    xcb = bf16v(alloc(N // 2), N)
    glo = f32v(alloc(4 * 128), 4 * 128).rearrange("p (c n) -> p c n", c=4)
    lro = f32v(alloc(4 * 128), 4 * 128).rearrange("p (c n) -> p c n", c=4)
    hso = f32v(alloc(128), 128)
    ktile = bf16v(alloc(2 * N), 4 * N).rearrange("p (c n) -> p c n", c=4)
    vaug = bf16v(alloc(520 * 4 // 2), 4 * 520).rearrange("p (b h e) -> p b h e", b=4, h=8)
    kit = f32v(alloc(N), N)
    qtile = bf16v(alloc(4 * 64), 4 * 128).rearrange("p (c n) -> p c n", c=4)
    qit = f32v(alloc(8 * 128), 8 * 128).rearrange("p (h n) -> p h n", h=8)
    mgt = bf16v(alloc(8 * 64), 8 * 128).rearrange("p (c n) -> p c n", c=8)
    xo = f32v(alloc(8 * 128), 8 * 128).rearrange("p (k n) -> p k n", k=8)
    h2o = f32v(alloc(8 * 128), 8 * 128).rearrange("p (k n) -> p k n", k=8)
    hbo = bf16v(alloc(4 * 128), 8 * 128).rearrange("p (k n) -> p k n", k=8)

    (txt, thb, tsq, tyb, th2, trstd, twv, twqi, txl, txcb, tglo, tlro, tkt, tva, tkit, tqt,
     tqit, tmgt, twb, tlst, tws, txo, th2o, thbo, thso) = [Tok() for _ in range(25)]
    tact = [Tok() for _ in range(NF)]
    tlt = Tok()

    dma("pool", wv, w_in[:, 1024:1536].rearrange("(kc p) c -> p kc c", p=128), [], [twv])
    dma("sp", wki, w_in[:, 2048:2112].rearrange("(kc p) c -> p kc c", p=128), [], [twqi])
    dma("sp", wwi, w_in[:, 2112:2120].rearrange("(kc p) c -> p kc c", p=128), [], [twqi])
    dma("pool", wblk, wblk_d, [], [twb])
    P.op("dve", lambda e: e.memset(vaug, 1.0), [], [tva])
    P.op("dve", lambda e: e.memset(xl, 0.0), [], [txl])

    xT_v = xT.rearrange("(kc p) t -> p kc t", p=128)

    def select_own(dst, tdst, src, tsrc, eng="dve"):
        def blk(b):
            if len(src.shape) == 3:
                return src[:, :, b * 128:(b + 1) * 128]
            return src[:, b * 128:(b + 1) * 128]
        ts("dve", dst, blk(0), vecs[:, V_SEL:V_SEL + 1], None, ALU.mult, None, [tsrc, tvec], [tdst])
        for b in range(1, 4):
            stt(dst, blk(b), vecs[:, V_SEL + b:V_SEL + b + 1], dst, ALU.mult, ALU.add,
                [tsrc, tvec, tdst], [tdst])

    def norm_mod(src, tsrc, out_b, tob, sc_ap, sh_ap, out_f=None, tof=None, nk=8, n=N):
        norm_stats(src, nk, n, sq[:, 0:nk, 0:n], tsrc, tsq, rstd[:, 0:n], trstd, 0, 1.0 / (128 * nk))
        for k in range(nk):
            tmpk = ybuf[:, k, 0:n]
            tt("dve", tmpk, src[:, k, 0:n], rstd[:, 0:n], ALU.mult, [tsrc, trstd], [tyb])
            sc = sc_ap[:, k:k + 1]
            sh = sh_ap[:, k:k + 1] if sh_ap is not None else 0.0
            if out_f is not None:
                act(out_f[:, k, 0:n], tmpk, AF.Identity, [tyb, tder, tvec], [tof], bias=sh, scale=sc)
                cp("pool", out_b[:, k, 0:n], out_f[:, k, 0:n], [tof], [tob])
            else:
                act(out_b[:, k, 0:n], tmpk, AF.Identity, [tyb, tder, tvec], [tob], bias=sh, scale=sc)

    def post_res(ci, xres, txres):
        norm_stats(ybuf, 8, N, sq, tyb, tsq, rstd, trstd, 0, 1.0 / D)
        for k in range(8):
            tt("dve", ybuf[:, k, :], ybuf[:, k, :], rstd, ALU.mult, [tyb, trstd], [tyb])
            stt(xres[:, k, :], ybuf[:, k, :], der[:, D_GT[ci] + k:D_GT[ci] + k + 1], xres[:, k, :],
                ALU.mult, ALU.add, [tyb, tder, txres], [txres])

    def ffn2(i, bg=None):
        ffn(i, N, hb, thb, actb, tact, wslots, dslots, ybuf, tyb, sgb, tsg, bg)

    lru_bg = None
    for it in range(NA):
        T0 = it * N
        dma("sp", xt, xT_v[:, :, T0:T0 + N], [], [txt])
        norm_mod(xt, txt, hb, thb, der[:, D_SC[0]:D_SC[0] + 8], der[:, D_SH[0]:D_SH[0] + 8])
        ffn2(0, lru_bg)
        post_res(0, xt, txt)
        select_own(xo, txo, xt, txt)
        dma("sp", x1d[:, :, it * 128:(it + 1) * 128], xo, [txo], [])
        norm_mod(xt, txt, hb, thb, der[:, D_SC[1]:D_SC[1] + 8], der[:, D_SH[1]:D_SH[1] + 8],
                 out_f=h2, tof=th2)
        select_own(h2o, th2o, h2, th2)
        cp("pool", hbo, h2o, [th2o], [thbo])

        order = [4, 5, 6, 7, 8, 9, 10, 11, 12, 13, 14, 15, 0, 1, 2, 3]

        def ldw(ix):
            s = wislots[ix % 3]
            dma("sp", s[0], win_t[order[ix]], [tw["win"]], [s[1]])
        ldw(0); ldw(1)
        for ix in range(16):
            if ix + 2 < 16:
                ldw(ix + 2)
            ch = order[ix]
            s = wislots[ix % 3]
            pb = 1 + ix % 2
            own = ch < 4 or ch >= 12
            for kc in range(8):
                if own:
                    mm(psb[pb][:, 0:128], s[0][:, kc, :], hbo[:, kc, :], kc == 0, kc == 7,
                       [s[1], thbo], [pst[pb]])
                else:
                    mm(psb[pb][:, 0:N], s[0][:, kc, :], hb[:, kc, :], kc == 0, kc == 7,
                       [s[1], thb], [pst[pb]])
            if 4 <= ch < 8:
                cp("act", ktile[:, ch - 4, :], psb[pb][:, 0:N], [pst[pb]], [tkt])
            elif 8 <= ch < 12:
                cp("act", xl[:, ch - 8, 3:3 + N], psb[pb][:, 0:N], [pst[pb]], [txl])
            elif ch >= 12:
                cp("act", glo[:, ch - 12, :], psb[pb][:, 0:128], [pst[pb]], [tglo])
            else:
                cp("act", qtile[:, ch, :], psb[pb][:, 0:128], [pst[pb]], [tqt])
        dma("sp", Kd[:, :, T0:T0 + N], ktile, [tkt], [])
        dma("sp", qd[:, :, it * 128:(it + 1) * 128], qtile, [tqt], [])
        for tb in range(4):
            pb = 3 + tb % 2
            for kc in range(8):
                mm(psb[pb][:, 0:512], hb[:, kc, tb * 128:(tb + 1) * 128], wv[:, kc, :], kc == 0, kc == 7,
                   [thb, twv], [pst[pb]])
            cp("dve" if tb % 2 else "act", vaug[:, tb, :, 0:64],
               psb[pb][:, 0:512].rearrange("p (h e) -> p h e", h=8), [pst[pb]], [tva])
        dma("sp", Vd[T0:T0 + N, :].rearrange("(b p) e -> p b e", p=128),
            vaug.rearrange("p b h e -> p b (h e)"), [tva], [])
        for kc in range(8):
            mm(psb[5][0:64, 0:N], wki[:, kc, :], h2[:, kc, :], kc == 0, kc == 7, [twqi, th2], [pst[5]])
        cp("act", kit[0:64, :], psb[5][0:64, 0:N], [pst[5]], [tkit])
        dma("sp", KId[:, T0:T0 + N], kit[0:64, :], [tkit], [])
        dma("sp", wqi, w_in[:, 1536:2048].rearrange("(kc p) c -> p kc c", p=128), [], [tyb])
        for h in range(8):
            pb = 6 + h % 2
            for kc in range(8):
                mm(psb[pb][0:64, 0:128], wqi[:, kc, h * 64:(h + 1) * 64], h2o[:, kc, :], kc == 0, kc == 7,
                   [tyb, th2o], [pst[pb]])
            cp("dve" if h % 2 else "act", qit[0:64, h, :], psb[pb][0:64, 0:128], [pst[pb]], [tqit])
        dma("sp", qid[:, it, :, :], qit[0:64, :, :], [tqit], [])
        for kc in range(8):
            mm(psb[5][:, 0:8], h2o[:, kc, :], wwi[:, kc, :], kc == 0, kc == 7, [th2o, twqi], [pst[5]])
        act(wabs[:, it * 8:it * 8 + 8], psb[5][:, 0:8], AF.Abs, [pst[5]], [tws])
        act(wsgn[:, it * 8:it * 8 + 8], psb[5][:, 0:8], AF.Sign, [pst[5]], [tws])

        def lru_gen(it=it):
            xc, rg, ig, aa, bb, hs = lt
            for c in range(4):
                cw = lambda j, c=c: vecs[:, V_CW + j * 4 + c:V_CW + j * 4 + c + 1]
                ts("dve", xc, xl[:, c, 3:3 + N], cw(3), vecs[:, V_CB + c:V_CB + c + 1], ALU.mult, ALU.add,
                   [txl, tvec], [tlt])
                for j in range(3):
                    stt(xc, xl[:, c, j:j + N], cw(j), xc, ALU.mult, ALU.add, [txl, tvec, tlt], [tlt])
                cp("pool", xcb, xc, [tlt], [txcb])
                yield
                mm(psb[7][:, 0:N], wblk[:, 0, c, :], xcb, True, True, [twb, txcb], [pst[7]])
                act(rg, psb[7][:, 0:N], AF.Sigmoid, [pst[7], tvec], [tlt], bias=vecs[:, V_BA + c:V_BA + c + 1])
                mm(psb[7][:, 0:N], wblk[:, 1, c, :], xcb, True, True, [twb, txcb], [pst[7]])
                act(ig, psb[7][:, 0:N], AF.Sigmoid, [pst[7], tvec], [tlt], bias=vecs[:, V_BI + c:V_BI + c + 1])
                act(aa, rg, AF.Exp, [tlt, tder], [tlt], scale=der[:, D_CA + c:D_CA + c + 1])
                tt("dve", rg, aa, aa, ALU.mult, [tlt], [tlt])
                act(rg, rg, AF.Sqrt, [tlt], [tlt], bias=1.0, scale=-1.0)
                tt("dve", bb, ig, xc, ALU.mult, [tlt], [tlt])
                tt("dve", bb, bb, rg, ALU.mult, [tlt], [tlt])
                yield
                P.op("dve", lambda e, o=hs, a=aa, b=bb, s=lstate[:, c:c + 1]:
                     e.tensor_tensor_scan(out=o, data0=a, data1=b, initial=s, op0=ALU.mult, op1=ALU.add),
                     [tlt, tlst, tder], [tlt])
                cp("dve", lstate[:, c:c + 1], hs[:, N - 1:N], [tlt], [tlst])
                cp("dve", xl[:, c, 0:3], xl[:, c, N:N + 3], [txl, tlt], [txl])
                select_own(hso, thso, hs, tlt)
                yield
                g = glo[:, c, :]
                t1 = lro[:, c, :]
                tt("dve", t1, g, g, ALU.mult, [tglo], [tlro])
                ts("dve", t1, t1, 0.044715, 1.0, ALU.mult, ALU.add, [tlro], [tlro])
                tt("dve", t1, t1, g, ALU.mult, [tlro, tglo], [tlro])
                act(t1, t1, AF.Sigmoid, [tlro], [tlro], scale=1.5957691216057308)
                tt("dve", t1, t1, g, ALU.mult, [tlro, tglo], [tlro])
                tt("dve", t1, t1, hso, ALU.mult, [tlro, thso], [tlro])
                yield
            norm_mod(lro, tlro, mgt[:, 4:8, :], tmgt, vecs[:, V_LOG:V_LOG + 4], None, nk=4, n=128)
            dma("sp", mgd[:, 4:8, it * 128:(it + 1) * 128], mgt[:, 4:8, :], [tmgt], [])
            yield
        lru_bg = lru_gen()
    for _ in lru_bg:
        pass
    P.barrier()
    mark_a = cur[0]
    cur[0] = base_mark
    if stop == 'A':
        return finish()

    NKB = NA * 4
    KI = f32v(alloc(NKB * 128), NKB * 128)
    qown = bf16v(alloc(2 * NA * 128), 4 * NA * 128).rearrange("p (c n) -> p c n", c=4)
    Sb = f32v(alloc(NKB * 128), NKB * 128)
    maskb = [bf16v(alloc(NKB * 64), NKB * 128) for _ in range(4)]
    Rt = [f32v(alloc(512), 512) for _ in range(2)]
    qi = [f32v(alloc(1024), 1024).rearrange("p (h n) -> p h n", h=8) for _ in range(2)]
    kts = [bf16v(alloc(256), 512).rearrange("p (c n) -> p c n", c=4) for _ in range(4)]
    vts = [bf16v(alloc(260), 520).rearrange("p (h e) -> p h e", h=8) for _ in range(4)]
    mTs = [bf16v(alloc(128), 256) for _ in range(2)]
    Es = [bf16v(alloc(128), 256) for _ in range(4)]
    Pms = [bf16v(alloc(128), 256) for _ in range(4)]
    attn = f32v(alloc(512), 512)
    attnb = bf16v(alloc(256), 512)
    mgB = bf16v(alloc(256), 512).rearrange("p (c n) -> p c n", c=4)
    pm4 = f32v(alloc(512), 512)
    TbC = bf16v(alloc(5 * 8 * 64), 5 * 8 * 128).rearrange("p (a h q) -> p a h q", a=5, h=8)
    bis = f32v(alloc(32), 32)
    fin = f32v(alloc(32), 32)
    scr = f32v(alloc(8), 8)
    steps = f32v(alloc(16), 16)
    psb5b = psb[5][:, 0:256].bitcast(BF16)
    tKI, tqown, tS, tattn, tattnb, tbis, tsteps, tpm, tmgB, tTbC, tfin, tscr, tscr2 = [Tok() for _ in range(13)]
    tmask = [Tok() for _ in range(4)]
    tR = [Tok(), Tok()]
    tqi = [Tok(), Tok()]
    tkv = [Tok() for _ in range(4)]
    tmT = [Tok(), Tok()]
    tE = [Tok() for _ in range(4)]
    tPm = [Tok() for _ in range(4)]
    tST = [Tok() for _ in range(4)]
    dma("sp", KI[0:64, :], KId[:, 0:NKB * 128], [], [tKI])
    dma("sp", qown, qd[:, :, 0:NA * 128], [], [tqown])
    dma("sp", pm4, posm_d.rearrange("p k n -> p (k n)"), [], [tpm])
    for a in range(5):
        for h in range(8):
            ts("dve", TbC[:, a, h, :], Tb[:, 0, h, :], vecs[:, V_SD + a:V_SD + a + 1], None, ALU.mult, None,
               [tTb, tvec], [tTbC])
            stt(TbC[:, a, h, :], Tb[:, 1, h, :], vecs[:, V_SP + a:V_SP + a + 1], TbC[:, a, h, :],
                ALU.mult, ALU.add, [tTb, tvec, tTbC], [tTbC])

    def indexer(j):
        mslot = j % 4
        qs = j % 2
        nk = (j + 1) * 512
        dma("sp", qi[qs][0:64], qid[:, j, :, :], [], [tqi[qs]])
        for kc0 in range(0, nk, 512):
            for h in range(8):
                mm(psb[5][:, 0:512], qi[qs][0:64, h, :], KI[0:64, kc0:kc0 + 512], True, True,
                   [tqi[qs], tKI], [pst[5]])
                R = Rt[h % 2]
                act(R, psb[5][:, 0:512], AF.Relu, [pst[5], tws], [tR[h % 2]],
                    scale=wabs[:, j * 8 + h:j * 8 + h + 1])
                sg = wsgn[:, j * 8 + h:j * 8 + h + 1]
                if h == 0:
                    ts("dve", Sb[:, kc0:kc0 + 512], R, sg, None, ALU.mult, None, [tR[h % 2], tws], [tS])
                else:
                    stt(Sb[:, kc0:kc0 + 512], R, sg, Sb[:, kc0:kc0 + 512], ALU.mult, ALU.add,
                        [tR[h % 2], tws, tS], [tS])
                yield
        P.op("dve", lambda e, o=bis[:, 0:1], i=Sb[:, 0:nk]:
             e.tensor_reduce(out=o, in_=i, axis=AX.X, op=ALU.max, apply_absolute_value=True),
             [tS], [tbis])
        base = j * 512
        tt("dve", Sb[:, base:base + 512], Sb[:, base:base + 512], pm4, ALU.add, [tS, tpm], [tS])
        hi0, lo, w0, mid, cnt, gs = [bis[:, i:i + 1] for i in range(1, 7)]
        ts("dve", hi0, bis[:, 0:1], 1.001, 1e-6, ALU.mult, ALU.add, [tbis], [tbis])
        ts("dve", lo, hi0, -1.0, None, ALU.mult, None, [tbis], [tbis])
        ts("dve", w0, hi0, 2.0, None, ALU.mult, None, [tbis], [tbis])
        ts("dve", steps, vecs[:, V_P2:V_P2 + 16], w0, None, ALU.mult, None, [tbis, tvec], [tsteps])
        yield
        junk = maskb[mslot][:, 0:nk]
        for k in range(NBIS):
            tt("dve", mid, lo, steps[:, k:k + 1], ALU.add, [tbis, tsteps], [tbis])
            ts("dve", junk, Sb[:, 0:nk], mid, None, ALU.is_ge, ALU.add, [tS, tbis], [tmask[mslot], tbis],
               accum=cnt)
            P.op("dve", lambda e: e.memset(scr[:, 0:1], 0.0), [tbis], [tbis])
            ts("dve", gs, cnt, 255.5, steps[:, k:k + 1], ALU.is_ge, ALU.mult, [tbis, tsteps], [tbis])
            tt("dve", lo, lo, gs, ALU.add, [tbis], [tbis])
            yield
        ts("dve", junk, Sb[:, 0:nk], lo, None, ALU.is_ge, None, [tS, tbis], [tmask[mslot]])
        yield

    def drain(gen, n=None):
        if gen is None:
            return
        try:
            if n is None:
                while True:
                    next(gen)
            else:
                for _ in range(n):
                    next(gen)
        except StopIteration:
            pass

    def chain(*gens):
        for g_ in gens:
            for _ in g_:
                yield

    def attention(j0, nq, bg):
        nkb_of = [(j0 + t + 1) * 4 for t in range(nq)]
        nkb = nkb_of[-1]
        first = [True] * 4
        c1 = nq * 128

        def ldkv(kb):
            s = kb % 4
            dma("sp", kts[s], Kd[:, :, kb * 128:(kb + 1) * 128], [], [tkv[s]])
            dma("sp", vts[s].rearrange("p h e -> p (h e)"), Vd[kb * 128:(kb + 1) * 128, :], [], [tkv[s]])

        def t0_of(kb):
            t0 = 0
            while kb >= nkb_of[t0]:
                t0 += 1
            return t0
        steps = [(kb, h) for kb in range(nkb) for h in range(8)]
        LAG = 2

        def front(i):
            kb, h = steps[i]
            s = kb % 4
            t0 = t0_of(kb)
            c0 = t0 * 128
            mT = mTs[kb % 2]
            if h == 0:
                if kb == 0:
                    ldkv(0)
                    ldkv(1)
                if kb + 2 < nkb:
                    ldkv(kb + 2)
                for t in range(t0, nq):
                    ms = (j0 + t) % 4
                    tr(psb5b[:, t * 128:(t + 1) * 128], maskb[ms][:, kb * 128:(kb + 1) * 128],
                       [tmask[ms], tvec], [pst[5]])
                cp("act", mT[:, c0:c1], psb5b[:, c0:c1], [pst[5]], [tmT[kb % 2]])
            po = (h % 2) * 64
            slot4 = i % 4
            pbank = (0, 6, 7)[i % 3]
            pcol = 0
            ps_s = psb[pbank][:, pcol + c0:pcol + c1]
            mm(ps_s, kts[s][po:po + 64, h // 2, :],
               qown[po:po + 64, h // 2, j0 * 128 + c0:j0 * 128 + c1],
               True, True, [tkv[s], tqown], [pst[pbank]])
            for t in range(t0, nq):
                a = kb - 4 * (j0 + t) + 1
                if 0 <= a <= 4:
                    sl = psb[pbank][:, pcol + t * 128:pcol + (t + 1) * 128]
                    mm(sl, identb, TbC[:, a, h, :], False, True, [tTbC, tvec], [pst[pbank]])
            E = Es[slot4]
            act(E[:, c0:c1], ps_s, AF.Exp, [pst[pbank], tvec], [tE[slot4]],
                bias=vecs[:, V_RB + 15 * 8 + h:V_RB + 15 * 8 + h + 1], scale=0.125)
            Pm = Pms[slot4]
            tt("pool", Pm[:, c0:c1], E[:, c0:c1], mT[:, c0:c1], ALU.mult,
               [tE[slot4], tmT[kb % 2]], [tPm[slot4]])

        def back(i):
            kb, h = steps[i]
            s = kb % 4
            slot4 = i % 4
            Pm = Pms[slot4]
            for t in range(t0_of(kb), nq):
                bank = 1 + t * 2 + (h // 4)
                col = (h % 4) * 65
                st_ = first[t * 2 + h // 4]
                first[t * 2 + h // 4] = False
                P.op("pe", lambda e, o=psb[bank][:, col:col + 65], l=Pm[:, t * 128:(t + 1) * 128],
                     r=vts[s][:, h, :], s_=st_:
                     e.matmul(o, lhsT=l, rhs=r, start=s_, stop=False, skip_group_check=True),
                     [tPm[slot4], tkv[s]], [pst[bank]])

        for i in range(len(steps) + LAG):
            if i < len(steps):
                front(i)
            if i >= LAG:
                back(i - LAG)
            drain(bg, 1)
        for t in range(nq):
            jt = j0 + t
            for hh in range(2):
                bank = 1 + t * 2 + hh
                v = psb[bank][:, 0:260].rearrange("p (h e) -> p h e", h=4)
                rc = fin[:, 8 + hh * 4:12 + hh * 4]
                cp("dve", rc, v[:, :, 64], [pst[bank]], [tfin])
                P.op("dve", lambda e, o=rc: e.reciprocal(out=o, in_=o), [tfin], [tfin])
                for h4 in range(4):
                    h = hh * 4 + h4
                    ts("dve", attn[:, h * 64:(h + 1) * 64], v[:, h4, 0:64], fin[:, 8 + h:9 + h], None,
                       ALU.mult, None, [pst[bank], tfin], [tattn])
            act(attnb, attn, AF.Square, [tattn], [tattnb, tfin], accum=fin[:, 16:17])
            P.op("act", lambda e: e.copy(out=scr[:, 4:5], in_=scr[:, 5:6]), [tfin], [tfin])
            act(fin[:, 17:18], fin[:, 16:17], AF.Sqrt, [tfin], [tfin], bias=EPS, scale=1.0 / 512)
            P.op("dve", lambda e, o=fin[:, 17:18]: e.reciprocal(out=o, in_=o), [tfin], [tfin])
            ts("dve", attnb, attn, fin[:, 17:18], None, ALU.mult, None, [tattn, tfin, tattnb], [tattnb])
            for c in range(4):
                tr(psb5b[:, c * 128:(c + 1) * 128], attnb[:, c * 128:(c + 1) * 128], [tattnb, tvec], [pst[5]])
            for c in range(4):
                act(mgB[:, c, :], psb5b[:, c * 128:(c + 1) * 128], AF.Identity, [pst[5], tvec], [tmgB],
                    scale=vecs[:, V_AOG + c:V_AOG + c + 1])
            dma("sp", mgd[:, 0:4, jt * 128:(jt + 1) * 128], mgB, [tmgB], [])

    P.op("dve", lambda e: e.memset(scr, 0.0), [], [tbis])
    sbs = list(range(0, NA, 2))
    drain(chain(*[indexer(j) for j in range(sbs[0], min(sbs[0] + 2, NA))]))
    for si, j0 in enumerate(sbs):
        nq = min(2, NA - j0)
        bg = None
        if si + 1 < len(sbs):
            j1 = sbs[si + 1]
            bg = chain(*[indexer(j) for j in range(j1, min(j1 + 2, NA))])
        attention(j0, nq, bg)
        drain(bg)

    P.barrier()
    cur[0] = mark_a
    if stop == 'B':
        return finish()

    outT_v = outT.rearrange("(kc p) t -> p kc t", p=128)
    for ct in range(NA // 4):
        C0 = ct * N
        dma("sp", xt, x1d[:, :, C0:C0 + N], [], [txt])
        dma("sp", hb, mgd[:, :, C0:C0 + N], [], [thb])

        def ldo(dc):
            s = wislots[dc % 3]
            dma("sp", s[0], wout_t[dc], [tw["wout"]], [s[1]])
        ldo(0); ldo(1)
        for dc in range(8):
            if dc + 2 < 8:
                ldo(dc + 2)
            s = wislots[dc % 3]
            pb = 1 + dc % 2
            for kc in range(8):
                mm(psb[pb][:, 0:N], s[0][:, kc, :], hb[:, kc, :], kc == 0, kc == 7, [s[1], thb], [pst[pb]])
            cp("act" if dc % 2 == 0 else "dve", ybuf[:, dc, :], psb[pb][:, 0:N], [pst[pb]], [tyb])
        post_res(1, xt, txt)
        norm_mod(xt, txt, hb, thb, der[:, D_SC[2]:D_SC[2] + 8], der[:, D_SH[2]:D_SH[2] + 8])
        ffn2(1)
        post_res(2, xt, txt)
        dma("sp", outT_v[:, :, C0:C0 + N], xt, [txt], [])
    P.barrier()
    with nc.Block() as block:
        P.emit(block)
    st.close()
    return nc


def _t5_bucket_np(rel):
    rel = np.asarray(rel, np.int32)
    nb = 16
    max_exact = 8
    ret = np.where(rel > 0, nb, 0).astype(np.int32)
    n = np.abs(rel)
    nf = np.maximum(n, 1).astype(np.float32)
    large = max_exact + (np.log(nf / np.float32(max_exact)) / np.float32(math.log(128 / max_exact))
                         * np.float32(nb - max_exact)).astype(np.int32)
    large = np.minimum(large, nb - 1)
    return ret + np.where(n < max_exact, n, large)


_NC_CACHE = {}


def _host_inputs(inputs, NA=16):
    f = lambda a: np.ascontiguousarray(np.asarray(a, np.float32))
    x = f(inputs["x"]); c = f(inputs["c"])

    def col8(v):
        return v.reshape(8, 128).T

    def col4(v):
        return v.reshape(4, 128).T
    kl = np.arange(128)[:, None]; ql = np.arange(128)[None, :]
    bd = _t5_bucket_np(kl - ql)
    bp = _t5_bucket_np(kl - ql - 128)
    mb = np.zeros((128, 2, 32, 128), np.float32)
    for b in range(32):
        mb[:, 0, b, :] = (bd == b)
        mb[:, 1, b, :] = (bp == b)
    mb = mb.astype(ml_dtypes.bfloat16)
    ident = np.eye(128, dtype=np.float32).astype(ml_dtypes.bfloat16)
    wblk = np.zeros((128, 2, 4, 128), np.float32)
    for ai, w in enumerate((f(inputs["lru_w_a"])[0], f(inputs["lru_w_i"])[0])):
        for g in range(8):
            cch, half = g // 2, g % 2
            wblk[half * 64:(half + 1) * 64, ai, cch, half * 64:(half + 1) * 64] = w[g]
    shared = {
        "w_ada": f(inputs["w_ada"])[0],
        "wg1": f(inputs["ffn1_w_gate"])[0], "wu1": f(inputs["ffn1_w_up"])[0], "wd1": f(inputs["ffn1_w_down"])[0],
        "wg2": f(inputs["ffn2_w_gate"])[0], "wu2": f(inputs["ffn2_w_up"])[0], "wd2": f(inputs["ffn2_w_down"])[0],
        "w_in": f(inputs["w_in"])[0], "w_out": f(inputs["w_out"])[0],
        "wblk": wblk, "mb": mb, "ident": ident,
    }
    xTs = [np.ascontiguousarray(x[b].T) for b in range(2)]
    in_maps = []
    for core in range(8):
        b, r = core // 4, core % 4
        v = np.zeros((128, NV), np.float32)
        v[:, V_C:V_C + 8] = col8(c[b])
        v[:, V_BADA:V_BADA + 72] = f(inputs["b_ada"])[0].reshape(72, 128).T
        for i, nm in enumerate(["ffn1_pre_g", "ffn1_post_g", "mix_pre_g", "mix_post_g", "ffn2_pre_g", "ffn2_post_g"]):
            v[:, V_G + 8 * i:V_G + 8 * i + 8] = col8(f(inputs[nm])[0])
        v[:, V_AOG:V_AOG + 4] = col4(f(inputs["attn_out_g"])[0])
        v[:, V_LOG:V_LOG + 4] = col4(f(inputs["lru_out_g"])[0])
        cw = f(inputs["lru_conv_w"])[0]
        for j in range(4):
            v[:, V_CW + 4 * j:V_CW + 4 * j + 4] = col4(cw[j])
        v[:, V_CB:V_CB + 4] = col4(f(inputs["lru_conv_b"])[0])
        v[:, V_BA:V_BA + 4] = col4(f(inputs["lru_b_a"])[0])
        v[:, V_BI:V_BI + 4] = col4(f(inputs["lru_b_i"])[0])
        v[:, V_LAM:V_LAM + 4] = col4(f(inputs["lru_lambda"])[0])
        v[:, V_RB:V_RB + 256] = f(inputs["rel_bias"]).reshape(1, 256)
        v[:, V_P2:V_P2 + 16] = (0.5 ** np.arange(1, 17))[None, :]
        v[:, V_SEL + r] = 1.0
        v[:, V_SD + r + 1] = 1.0
        v[:, V_SP + r] = 1.0
        posm = np.zeros((128, 4, 128), np.float32)
        for kb in range(4):
            if kb > r:
                posm[:, kb, :] = -BIG
            elif kb == r:
                posm[0:64, kb, 64:128] = -BIG
        m = dict(shared)
        m["xT"] = xTs[b]
        m["vecs"] = v
        m["posm"] = posm
        in_maps.append(m)
    return in_maps


def kernel(**inputs):
    NA = 16
    if NA not in _NC_CACHE:
        _NC_CACHE[NA] = build(NA)
    nc = _NC_CACHE[NA]
    in_maps = _host_inputs(inputs, NA)
    res = run_bass_kernel_spmd(nc, in_maps, core_ids=list(range(8)))
    out = np.zeros((2, S, D), np.float32)
    for core in range(8):
        b, r = core // 4, core % 4
        o = np.asarray(res.results[core]["outT"], np.float32)
        o = o.T.reshape(NBLK, 128, D)
        for j in range(NBLK):
            g = 4 * j + r
            out[b, g * 128:(g + 1) * 128, :] = o[j]
    return out
```

```python
import math
import numpy as np
import ml_dtypes
from contextlib import ExitStack
import concourse.bass as bass
import concourse.mybir as mybir
from concourse.bass_utils import run_bass_kernel_spmd

F32 = mybir.dt.float32
BF16 = mybir.dt.bfloat16
ALU = mybir.AluOpType
AF = mybir.ActivationFunctionType
AX = mybir.AxisListType

D = 1024
S = 8192
DFF = 2816
NF = DFF // 128
DIN = 3144
NBLK = 16
EPS = 1e-6
NBIS = 16
BIG = 3.0e38

V_C = 0
V_BADA = 8
V_G = 80
V_AOG = 128
V_LOG = 132
V_CW = 136
V_CB = 152
V_BA = 156
V_BI = 160
V_LAM = 164
V_RB = 168
V_P2 = 424
V_SEL = 440
V_SD = 444
V_SP = 449
NV = 456


class Tok:
    __slots__ = ("w", "r")

    def __init__(self):
        self.w = None
        self.r = []


class Prog:
    COMPUTE = ("pe", "act", "dve", "pool")
    NDMASEM = 24

    def __init__(self, nc, stack):
        self.nc = nc
        self.streams = {k: [] for k in ("pe", "act", "dve", "pool", "sp")}
        self.count = {k: 0 for k in self.COMPUTE}
        self.sems = {k: stack.enter_context(nc.semaphore("s_" + k)) for k in self.COMPUTE}
        self.dsems = [stack.enter_context(nc.semaphore("d%d" % i)) for i in range(self.NDMASEM)]
        self.dcount = [0] * self.NDMASEM
        self.dnext = 0
        self.waited = {k: {} for k in self.streams}
        self.ninstr = 0

    def _deps(self, stream, reads, writes):
        out = {}

        def add(kv):
            k, v = kv
            if k == stream and k == "pe":
                return
            if out.get(k, 0) < v:
                out[k] = v
        for t in reads:
            if t.w is not None:
                add(t.w)
        for t in writes:
            if t.w is not None:
                add(t.w)
            for kv in t.r:
                if kv[0] == stream and not isinstance(kv[0], int):
                    continue
                add(kv)
        waits = []
        wd = self.waited[stream]
        for k, v in out.items():
            if wd.get(k, 0) >= v:
                continue
            wd[k] = v
            waits.append((k, v))
        return waits

    def _semof(self, k):
        return self.dsems[k] if isinstance(k, int) else self.sems[k]

    def op(self, eng, fn, reads=(), writes=()):
        waits = self._deps(eng, reads, writes)
        self.count[eng] += 1
        idx = self.count[eng]
        self.streams[eng].append((waits, fn, ("c", eng)))
        for t in reads:
            t.r.append((eng, idx))
            if len(t.r) > 600:
                t.r = t.r[-400:]
        for t in writes:
            t.w = (eng, idx)
            t.r = []
        self.ninstr += 1

    def dma(self, q, fn, reads=(), writes=()):
        s = self.dnext
        self.dnext = (self.dnext + 1) % self.NDMASEM
        waits = self._deps(q, reads, writes)
        prev = self.dcount[s]
        if prev > 0 and self.waited[q].get(s, 0) < prev:
            self.waited[q][s] = prev
            waits.append((s, prev))
        self.dcount[s] += 16
        val = self.dcount[s]
        self.streams[q].append((waits, fn, ("d", s)))
        for t in reads:
            t.r.append((s, val))
        for t in writes:
            t.w = (s, val)
            t.r = []
        self.ninstr += 1

    def barrier(self):
        for st in self.streams:
            waits = []
            for k in self.COMPUTE:
                v = self.count[k]
                if v > 0 and self.waited[st].get(k, 0) < v and k != st:
                    self.waited[st][k] = v
                    waits.append((k, v))
            for s in range(self.NDMASEM):
                v = self.dcount[s]
                if v > 0 and self.waited[st].get(s, 0) < v:
                    self.waited[st][s] = v
                    waits.append((s, v))
            if waits:
                self.streams[st].append((waits, None, None))

    def emit(self, block):
        prog = self

        def run(stream, h):
            for waits, fn, kind in prog.streams[stream]:
                for (k, v) in waits:
                    h.wait_ge(prog._semof(k), v)
                if fn is None:
                    continue
                ins = fn(h)
                if kind[0] == "c":
                    ins.then_inc(prog.sems[kind[1]], 1)
                else:
                    ins.then_inc(prog.dsems[kind[1]], 16)

        @block.tensor
        def _(e):
            run("pe", e)

        @block.scalar
        def _(e):
            run("act", e)

        @block.vector
        def _(e):
            run("dve", e)

        @block.gpsimd
        def _(e):
            run("pool", e)

        @block.sync
        def _(e):
            run("sp", e)


def build(NA=16, stop=None, debug=False):
    nc = bass.Bass("TRN2", target_bir_lowering=False)
    st = ExitStack()
    P = Prog(nc, st)

    def dram_in(name, shape, dt=F32):
        return nc.dram_tensor(name, list(shape), dt, kind="ExternalInput").ap()

    def dram_tmp(name, shape, dt):
        if debug and name in ("Kd", "Vd", "KId", "x1d", "qd", "qid", "mgd"):
            return nc.dram_tensor(name, list(shape), dt, kind="ExternalOutput").ap()
        return nc.dram_tensor(name, list(shape), dt).ap()

    xT = dram_in("xT", [D, S])
    vecs_d = dram_in("vecs", [128, NV])
    w_ada = dram_in("w_ada", [D, 9 * D])
    wg1 = dram_in("wg1", [D, DFF]); wu1 = dram_in("wu1", [D, DFF]); wd1 = dram_in("wd1", [DFF, D])
    wg2 = dram_in("wg2", [D, DFF]); wu2 = dram_in("wu2", [D, DFF]); wd2 = dram_in("wd2", [DFF, D])
    w_in = dram_in("w_in", [D, DIN])
    w_out = dram_in("w_out", [D, D])
    wblk_d = dram_in("wblk", [128, 2, 4, 128])
    mb_d = dram_in("mb", [128, 2, 32, 128], BF16)
    ident_d = dram_in("ident", [128, 128], BF16)
    posm_d = dram_in("posm", [128, 4, 128])
    outT = nc.dram_tensor("outT", [D, NBLK * 128], F32, kind="ExternalOutput").ap()

    wg_t = [dram_tmp("wg_t%d" % i, [NF, 128, 8, 128], BF16) for i in range(2)]
    wu_t = [dram_tmp("wu_t%d" % i, [NF, 128, 8, 128], BF16) for i in range(2)]
    wd_t = [dram_tmp("wd_t%d" % i, [8, 128, NF, 128], BF16) for i in range(2)]
    win_t = dram_tmp("win_t", [16, 128, 8, 128], BF16)
    wout_t = dram_tmp("wout_t", [8, 128, 8, 128], BF16)
    Kd = dram_tmp("Kd", [128, 4, S], BF16)
    Vd = dram_tmp("Vd", [S, 520], BF16)
    KId = dram_tmp("KId", [64, S], F32)
    x1d = dram_tmp("x1d", [128, 8, NBLK * 128], F32)
    qd = dram_tmp("qd", [128, 4, NBLK * 128], BF16)
    qid = dram_tmp("qid", [64, NBLK, 8, 128], F32)
    mgd = dram_tmp("mgd", [128, 8, NBLK * 128], BF16)

    AW = 52600
    arena = st.enter_context(nc.sbuf_tensor("arena", [128, AW], F32))
    cur = [0]

    def alloc(words):
        a = cur[0]
        cur[0] += words
        assert cur[0] <= AW, ("sbuf overflow", cur[0])
        return a

    def f32v(off, n):
        return arena[:, off:off + n]

    def bf16v(off, nbf):
        return arena[:, off:off + nbf // 2].bitcast(BF16)

    vecs = f32v(alloc(NV), NV)
    der = f32v(alloc(128), 128)
    modT = f32v(alloc(72), 72)
    wabs = f32v(alloc(NBLK * 8), NBLK * 8)
    wsgn = f32v(alloc(NBLK * 8), NBLK * 8)
    identb = bf16v(alloc(64), 128)
    onesb = bf16v(alloc(64), 128)
    Tb = f32v(alloc(2048), 2048).rearrange("p (t h q) -> p t h q", t=2, h=8)
    lstate = f32v(alloc(4), 4)
    base_mark = cur[0]

    D_SC = [0, 16, 32]
    D_SH = [8, 24, 40]
    D_GT = [48, 56, 64]
    D_CA = 72
    D_MISC = 76

    psb = [st.enter_context(nc.psum_tensor("ps%d" % i, [128, 512], F32)) for i in range(8)]
    pst = [Tok() for _ in range(8)]

    tvec, tder, tmod = Tok(), Tok(), Tok()

    def mm(out, lhsT, rhs, start, stop, reads, writes):
        P.op("pe", lambda e, o=out, l=lhsT, r=rhs, s=start, t=stop:
             e.matmul(o, lhsT=l, rhs=r, start=s, stop=t), reads, writes)

    def tr(out, in_, reads, writes):
        P.op("pe", lambda e, o=out, i=in_: e.transpose(o, i, identb), reads, writes)

    def act(out, in_, func, reads, writes, bias=0.0, scale=1.0, accum=None):
        if accum is None:
            P.op("act", lambda e, o=out, i=in_, f=func, b=bias, s=scale:
                 e.activation(out=o, in_=i, func=f, bias=b, scale=s), reads, writes)
        else:
            P.op("act", lambda e, o=out, i=in_, f=func, b=bias, s=scale, a=accum:
                 e.activation(out=o, in_=i, func=f, bias=b, scale=s, accum_out=a), reads, writes)

    def tt(eng, out, in0, in1, op, reads, writes):
        P.op(eng, lambda e, o=out, a=in0, b=in1, p=op: e.tensor_tensor(out=o, in0=a, in1=b, op=p),
             reads, writes)

    def ts(eng, out, in0, s1, s2, op0, op1, reads, writes, accum=None):
        if op1 is None:
            op1 = ALU.bypass
        if accum is None:
            P.op(eng, lambda e, o=out, a=in0, x=s1, y=s2, p=op0, q=op1:
                 e.tensor_scalar(out=o, in0=a, scalar1=x, scalar2=y, op0=p, op1=q), reads, writes)
        else:
            P.op(eng, lambda e, o=out, a=in0, x=s1, y=s2, p=op0, q=op1, ac=accum:
                 e.tensor_scalar(out=o, in0=a, scalar1=x, scalar2=y, op0=p, op1=q, accum_out=ac),
                 reads, writes)

    def stt(out, in0, scalar, in1, op0, op1, reads, writes):
        P.op("dve", lambda e, o=out, a=in0, s=scalar, b=in1, p=op0, q=op1:
             e.scalar_tensor_tensor(out=o, in0=a, scalar=s, in1=b, op0=p, op1=q), reads, writes)

    def cp(eng, out, in_, reads, writes):
        if eng == "act":
            P.op("act", lambda e, o=out, i=in_: e.copy(out=o, in_=i), reads, writes)
        else:
            P.op(eng, lambda e, o=out, i=in_: e.tensor_copy(out=o, in_=i), reads, writes)

    def dma(q, out, in_, reads, writes):
        P.dma(q, lambda e, o=out, i=in_: e.dma_start(out=o, in_=i), reads, writes)

    dma("sp", vecs, vecs_d, [], [tvec])
    dma("sp", identb, ident_d, [], [tvec])
    P.op("dve", lambda e: e.memset(onesb, 1.0), [], [tvec])

    tw = {}

    def cast_w(name, dst, src_view, n):
        t = Tok()
        tw[name] = t
        for i in range(n):
            dma("pool", dst[i], src_view[i], [], [t])

    def cast_ffn(i, wg, wu, wd):
        cast_w("wg%d" % i, wg_t[i], wg.rearrange("(kc p) (f c) -> f p kc c", p=128, c=128), NF)
        cast_w("wu%d" % i, wu_t[i], wu.rearrange("(kc p) (f c) -> f p kc c", p=128, c=128), NF)
        cast_w("wd%d" % i, wd_t[i], wd.rearrange("(f p) (dc c) -> dc p f c", p=128, c=128), 8)

    cast_ffn(0, wg1, wu1, wd1)
    win_cols = [0, 128, 256, 384, 512, 640, 768, 896, 2120, 2248, 2376, 2504, 2632, 2760, 2888, 3016]
    t = Tok(); tw["win"] = t
    for i, c0 in enumerate(win_cols):
        dma("pool", win_t[i], w_in[:, c0:c0 + 128].rearrange("(kc p) c -> p kc c", p=128), [], [t])

    scv = f32v(alloc(8), 8)
    tsc = Tok()
    act(scv, vecs[:, V_C:V_C + 8], AF.Silu, [tvec], [tsc])
    wa_off = alloc(2 * 8 * 1152)
    wa_buf = [f32v(wa_off + i * 9216, 9216).rearrange("p (k c) -> p k c", k=8) for i in range(2)]
    twa = [Tok(), Tok()]
    w_ada_v = w_ada.rearrange("(kc p) n -> p kc n", p=128)
    for cb in range(8):
        buf = wa_buf[cb % 2]
        dma("sp", buf, w_ada_v[:, :, cb * 1152:(cb + 1) * 1152], [], [twa[cb % 2]])
        for cc in range(9):
            col = cb * 9 + cc
            for kc in range(8):
                mm(psb[0][:, col:col + 1], buf[:, kc, cc * 128:(cc + 1) * 128], scv[:, kc:kc + 1],
                   kc == 0, kc == 7, [twa[cb % 2], tsc], [pst[0]])
    tt("dve", modT, psb[0][:, 0:72], vecs[:, V_BADA:V_BADA + 72], ALU.add, [pst[0], tvec], [tmod])
    for i, (jsh, jsc, jg, gpre, gpost, gmul) in enumerate(
            [(0, 1, 2, 0, 1, 0.5), (3, 4, 5, 2, 3, 1.0), (6, 7, 8, 4, 5, 0.5)]):
        stt(der[:, D_SC[i]:D_SC[i] + 8], modT[:, jsc * 8:jsc * 8 + 8], 1.0,
            vecs[:, V_G + gpre * 8:V_G + gpre * 8 + 8], ALU.add, ALU.mult, [tmod, tvec], [tder])
        cp("dve", der[:, D_SH[i]:D_SH[i] + 8], modT[:, jsh * 8:jsh * 8 + 8], [tmod], [tder])
        stt(der[:, D_GT[i]:D_GT[i] + 8], modT[:, jg * 8:jg * 8 + 8], gmul,
            vecs[:, V_G + gpost * 8:V_G + gpost * 8 + 8], ALU.mult, ALU.mult, [tmod, tvec], [tder])
    tmp4 = der[:, D_MISC:D_MISC + 4]
    act(tmp4, vecs[:, V_LAM:V_LAM + 4], AF.Exp, [tvec], [tder], scale=-1.0)
    act(tmp4, tmp4, AF.Ln, [tder], [tder], bias=1.0)
    ts("dve", der[:, D_CA:D_CA + 4], tmp4, -8.0, None, ALU.mult, None, [tder], [tder])
    P.op("dve", lambda e: e.memset(lstate, 0.0), [], [tder])

    mb_off = alloc(2 * 32 * 128 // 2)
    mbv = bf16v(mb_off, 2 * 32 * 128).rearrange("p (t b q) -> p t b q", t=2, b=32)
    tmb, tTb = Tok(), Tok()
    dma("sp", mbv, mb_d, [], [tmb])
    for ty in range(2):
        for h in range(8):
            T = Tb[:, ty, h, :]
            ts("dve", T, mbv[:, ty, 0, :], vecs[:, V_RB + h:V_RB + h + 1],
               vecs[:, V_RB + 15 * 8 + h:V_RB + 15 * 8 + h + 1], ALU.mult, ALU.subtract,
               [tmb, tvec], [tTb])
            for b in range(1, 32):
                stt(T, mbv[:, ty, b, :], vecs[:, V_RB + b * 8 + h:V_RB + b * 8 + h + 1], T,
                    ALU.mult, ALU.add, [tmb, tvec, tTb], [tTb])
            ts("dve", T, T, 8.0, None, ALU.mult, None, [tTb], [tTb])
    cast_ffn(1, wg2, wu2, wd2)
    cast_w("wout", wout_t, w_out.rearrange("(kc p) (dc c) -> dc p kc c", p=128, c=128), 8)
    P.barrier()
    cur[0] = base_mark


    def finish():
        P.barrier()
        with nc.Block() as block:
            P.emit(block)
        st.close()
        return nc
    if stop == 'pro':
        return finish()
    def norm_stats(src, nk, N, sq, tsrc, tsq, rstd, trstd, ps_i, dscale):
        act(sq, src, AF.Square, [tsrc], [tsq])
        for k in range(nk):
            mm(psb[ps_i][:, 0:N], onesb, sq[:, k, :], k == 0, k == nk - 1, [tsq, tvec], [pst[ps_i]])
        act(rstd, psb[ps_i][:, 0:N], AF.Sqrt, [pst[ps_i]], [trstd], bias=EPS, scale=dscale)
        P.op("dve", lambda e, o=rstd: e.reciprocal(out=o, in_=o), [trstd], [trstd])

    def ffn(i, N, hb, thb, actb, tact, wslots, dslots, ybuf, tyb, sgb, tsg, bg=None):
        def ld(f):
            s = wslots[f % 3]
            dma("sp", s[0], wg_t[i][f], [tw["wg%d" % i]], [s[2]])
            dma("sp", s[1], wu_t[i][f], [tw["wu%d" % i]], [s[2]])
        ld(0); ld(1)
        for f in range(NF):
            if f + 2 < NF:
                ld(f + 2)
            s = wslots[f % 3]
            pg, pu = (1, 2) if f % 2 == 0 else (3, 4)
            for kc in range(8):
                mm(psb[pg][:, 0:N], s[0][:, kc, :], hb[:, kc, :], kc == 0, kc == 7, [s[2], thb], [pst[pg]])
            for kc in range(8):
                mm(psb[pu][:, 0:N], s[1][:, kc, :], hb[:, kc, :], kc == 0, kc == 7, [s[2], thb], [pst[pu]])
            sg = sgb[f % 2]
            act(sg, psb[pg][:, 0:N], AF.Silu, [pst[pg]], [tsg[f % 2]])
            tt("dve", actb[:, f, :], sg, psb[pu][:, 0:N], ALU.mult, [tsg[f % 2], pst[pu]], [tact[f]])
            if bg is not None:
                for _ in range(2):
                    next(bg, None)
        if bg is not None:
            for _ in bg:
                pass

        def ldd(dc):
            s = dslots[dc % 2]
            dma("sp", s[0], wd_t[i][dc], [tw["wd%d" % i]], [s[1]])
        ldd(0)
        for dc in range(8):
            if dc + 1 < 8:
                ldd(dc + 1)
            s = dslots[dc % 2]
            pa = 5 + dc % 2
            for f in range(NF):
                mm(psb[pa][:, 0:N], s[0][:, f, :], actb[:, f, :], f == 0, f == NF - 1,
                   [s[1], tact[f]], [pst[pa]])
            cp("act" if dc % 2 == 0 else "dve", ybuf[:, dc, :], psb[pa][:, 0:N], [pst[pa]], [tyb])

    N = 512
    xt = f32v(alloc(8 * N), 8 * N).rearrange("p (k n) -> p k n", k=8)
    hb = bf16v(alloc(4 * N), 8 * N).rearrange("p (k n) -> p k n", k=8)
    sq = bf16v(alloc(4 * N), 8 * N).rearrange("p (k n) -> p k n", k=8)
    actb = bf16v(alloc(NF * N // 2), NF * N).rearrange("p (f n) -> p f n", f=NF)
    yoff = alloc(8 * N)
    ybuf = f32v(yoff, 8 * N).rearrange("p (k n) -> p k n", k=8)
    ltoff = alloc(6 * N)
    lt = [f32v(ltoff + i * N, N) for i in range(6)]
    wqi = f32v(yoff, 8 * 512).rearrange("p (k c) -> p k c", k=8)
    h2 = f32v(alloc(8 * N), 8 * N).rearrange("p (k n) -> p k n", k=8)
    rstd = f32v(alloc(N), N)
    sgb = [f32v(alloc(N), N) for _ in range(2)]
    tsg = [Tok(), Tok()]
    wslots = []
    for i in range(3):
        o = alloc(1024)
        wslots.append((bf16v(o, 1024).rearrange("p (k c) -> p k c", k=8),
                       bf16v(o + 512, 1024).rearrange("p (k c) -> p k c", k=8), Tok()))
    dslots = []
    for i in range(2):
        o = alloc(NF * 64)
        dslots.append((bf16v(o, NF * 128).rearrange("p (f c) -> p f c", f=NF), Tok()))
    wislots = []
    for i in range(3):
        o = alloc(512)
        wislots.append((bf16v(o, 1024).rearrange("p (k c) -> p k c", k=8), Tok()))
    wv = bf16v(alloc(8 * 256), 8 * 512).rearrange("p (k c) -> p k c", k=8)
    wki = f32v(alloc(8 * 64), 8 * 64).rearrange("p (k c) -> p k c", k=8)
    wwi = f32v(alloc(8 * 8), 8 * 8).rearrange("p (k c) -> p k c", k=8)
    wblk = bf16v(alloc(2 * 4 * 64), 2 * 4 * 128).rearrange("p (a c q) -> p a c q", a=2, c=4)
    xl = f32v(alloc(4 * (N + 4)), 4 * (N + 4)).rearrange("p (c n) -> p c n", c=4)
    xcb = bf16v(alloc(N // 2), N)
    glo = f32v(alloc(4 * 128), 4 * 128).rearrange("p (c n) -> p c n", c=4)
    lro = f32v(alloc(4 * 128), 4 * 128).rearrange("p (c n) -> p c n", c=4)
    hso = f32v(alloc(128), 128)
    ktile = bf16v(alloc(2 * N), 4 * N).rearrange("p (c n) -> p c n", c=4)
    vaug = bf16v(alloc(520 * 4 // 2), 4 * 520).rearrange("p (b h e) -> p b h e", b=4, h=8)
    kit = f32v(alloc(N), N)
    qtile = bf16v(alloc(4 * 64), 4 * 128).rearrange("p (c n) -> p c n", c=4)
    qit = f32v(alloc(8 * 128), 8 * 128).rearrange("p (h n) -> p h n", h=8)
    mgt = bf16v(alloc(8 * 64), 8 * 128).rearrange("p (c n) -> p c n", c=8)
    xo = f32v(alloc(8 * 128), 8 * 128).rearrange("p (k n) -> p k n", k=8)
    h2o = f32v(alloc(8 * 128), 8 * 128).rearrange("p (k n) -> p k n", k=8)
    hbo = bf16v(alloc(4 * 128), 8 * 128).rearrange("p (k n) -> p k n", k=8)

    (txt, thb, tsq, tyb, th2, trstd, twv, twqi, txl, txcb, tglo, tlro, tkt, tva, tkit, tqt,
     tqit, tmgt, twb, tlst, tws, txo, th2o, thbo, thso) = [Tok() for _ in range(25)]
    tact = [Tok() for _ in range(NF)]
    tlt = Tok()

    dma("pool", wv, w_in[:, 1024:1536].rearrange("(kc p) c -> p kc c", p=128), [], [twv])
    dma("sp", wki, w_in[:, 2048:2112].rearrange("(kc p) c -> p kc c", p=128), [], [twqi])
    dma("sp", wwi, w_in[:, 2112:2120].rearrange("(kc p) c -> p kc c", p=128), [], [twqi])
    dma("pool", wblk, wblk_d, [], [twb])
    P.op("dve", lambda e: e.memset(vaug, 1.0), [], [tva])
    P.op("dve", lambda e: e.memset(xl, 0.0), [], [txl])

    xT_v = xT.rearrange("(kc p) t -> p kc t", p=128)

    def select_own(dst, tdst, src, tsrc, eng="dve"):
        def blk(b):
            if len(src.shape) == 3:
                return src[:, :, b * 128:(b + 1) * 128]
            return src[:, b * 128:(b + 1) * 128]
        ts("dve", dst, blk(0), vecs[:, V_SEL:V_SEL + 1], None, ALU.mult, None, [tsrc, tvec], [tdst])
        for b in range(1, 4):
            stt(dst, blk(b), vecs[:, V_SEL + b:V_SEL + b + 1], dst, ALU.mult, ALU.add,
                [tsrc, tvec, tdst], [tdst])

    def norm_mod(src, tsrc, out_b, tob, sc_ap, sh_ap, out_f=None, tof=None, nk=8, n=N):
        norm_stats(src, nk, n, sq[:, 0:nk, 0:n], tsrc, tsq, rstd[:, 0:n], trstd, 0, 1.0 / (128 * nk))
        for k in range(nk):
            tmpk = ybuf[:, k, 0:n]
            tt("dve", tmpk, src[:, k, 0:n], rstd[:, 0:n], ALU.mult, [tsrc, trstd], [tyb])
            sc = sc_ap[:, k:k + 1]
            sh = sh_ap[:, k:k + 1] if sh_ap is not None else 0.0
            if out_f is not None:
                act(out_f[:, k, 0:n], tmpk, AF.Identity, [tyb, tder, tvec], [tof], bias=sh, scale=sc)
                cp("pool", out_b[:, k, 0:n], out_f[:, k, 0:n], [tof], [tob])
            else:
                act(out_b[:, k, 0:n], tmpk, AF.Identity, [tyb, tder, tvec], [tob], bias=sh, scale=sc)

    def post_res(ci, xres, txres):
        norm_stats(ybuf, 8, N, sq, tyb, tsq, rstd, trstd, 0, 1.0 / D)
        for k in range(8):
            tt("dve", ybuf[:, k, :], ybuf[:, k, :], rstd, ALU.mult, [tyb, trstd], [tyb])
            stt(xres[:, k, :], ybuf[:, k, :], der[:, D_GT[ci] + k:D_GT[ci] + k + 1], xres[:, k, :],
                ALU.mult, ALU.add, [tyb, tder, txres], [txres])

    def ffn2(i, bg=None):
        ffn(i, N, hb, thb, actb, tact, wslots, dslots, ybuf, tyb, sgb, tsg, bg)

    lru_bg = None
    for it in range(NA):
        T0 = it * N
        dma("sp", xt, xT_v[:, :, T0:T0 + N], [], [txt])
        norm_mod(xt, txt, hb, thb, der[:, D_SC[0]:D_SC[0] + 8], der[:, D_SH[0]:D_SH[0] + 8])
        ffn2(0, lru_bg)
        post_res(0, xt, txt)
        select_own(xo, txo, xt, txt)
        dma("sp", x1d[:, :, it * 128:(it + 1) * 128], xo, [txo], [])
        norm_mod(xt, txt, hb, thb, der[:, D_SC[1]:D_SC[1] + 8], der[:, D_SH[1]:D_SH[1] + 8],
                 out_f=h2, tof=th2)
        select_own(h2o, th2o, h2, th2)
        cp("pool", hbo, h2o, [th2o], [thbo])

        order = [4, 5, 6, 7, 8, 9, 10, 11, 12, 13, 14, 15, 0, 1, 2, 3]

        def ldw(ix):
            s = wislots[ix % 3]
            dma("sp", s[0], win_t[order[ix]], [tw["win"]], [s[1]])
        ldw(0); ldw(1)
        for ix in range(16):
            if ix + 2 < 16:
                ldw(ix + 2)
            ch = order[ix]
            s = wislots[ix % 3]
            pb = 1 + ix % 2
            own = ch < 4 or ch >= 12
            for kc in range(8):
                if own:
                    mm(psb[pb][:, 0:128], s[0][:, kc, :], hbo[:, kc, :], kc == 0, kc == 7,
                       [s[1], thbo], [pst[pb]])
                else:
                    mm(psb[pb][:, 0:N], s[0][:, kc, :], hb[:, kc, :], kc == 0, kc == 7,
                       [s[1], thb], [pst[pb]])
            if 4 <= ch < 8:
                cp("act", ktile[:, ch - 4, :], psb[pb][:, 0:N], [pst[pb]], [tkt])
            elif 8 <= ch < 12:
                cp("act", xl[:, ch - 8, 3:3 + N], psb[pb][:, 0:N], [pst[pb]], [txl])
            elif ch >= 12:
                cp("act", glo[:, ch - 12, :], psb[pb][:, 0:128], [pst[pb]], [tglo])
            else:
                cp("act", qtile[:, ch, :], psb[pb][:, 0:128], [pst[pb]], [tqt])
        dma("sp", Kd[:, :, T0:T0 + N], ktile, [tkt], [])
        dma("sp", qd[:, :, it * 128:(it + 1) * 128], qtile, [tqt], [])
        for tb in range(4):
            pb = 3 + tb % 2
            for kc in range(8):
                mm(psb[pb][:, 0:512], hb[:, kc, tb * 128:(tb + 1) * 128], wv[:, kc, :], kc == 0, kc == 7,
                   [thb, twv], [pst[pb]])
            cp("dve" if tb % 2 else "act", vaug[:, tb, :, 0:64],
               psb[pb][:, 0:512].rearrange("p (h e) -> p h e", h=8), [pst[pb]], [tva])
        dma("sp", Vd[T0:T0 + N, :].rearrange("(b p) e -> p b e", p=128),
            vaug.rearrange("p b h e -> p b (h e)"), [tva], [])
        for kc in range(8):
            mm(psb[5][0:64, 0:N], wki[:, kc, :], h2[:, kc, :], kc == 0, kc == 7, [twqi, th2], [pst[5]])
        cp("act", kit[0:64, :], psb[5][0:64, 0:N], [pst[5]], [tkit])
        dma("sp", KId[:, T0:T0 + N], kit[0:64, :], [tkit], [])
        dma("sp", wqi, w_in[:, 1536:2048].rearrange("(kc p) c -> p kc c", p=128), [], [tyb])
        for h in range(8):
            pb = 6 + h % 2
            for kc in range(8):
                mm(psb[pb][0:64, 0:128], wqi[:, kc, h * 64:(h + 1) * 64], h2o[:, kc, :], kc == 0, kc == 7,
                   [tyb, th2o], [pst[pb]])
            cp("dve" if h % 2 else "act", qit[0:64, h, :], psb[pb][0:64, 0:128], [pst[pb]], [tqit])
        dma("sp", qid[:, it, :, :], qit[0:64, :, :], [tqit], [])
        for kc in range(8):
            mm(psb[5][:, 0:8], h2o[:, kc, :], wwi[:, kc, :], kc == 0, kc == 7, [th2o, twqi], [pst[5]])
        act(wabs[:, it * 8:it * 8 + 8], psb[5][:, 0:8], AF.Abs, [pst[5]], [tws])
        act(wsgn[:, it * 8:it * 8 + 8], psb[5][:, 0:8], AF.Sign, [pst[5]], [tws])

        def lru_gen(it=it):
            xc, rg, ig, aa, bb, hs = lt
            for c in range(4):
                cw = lambda j, c=c: vecs[:, V_CW + j * 4 + c:V_CW + j * 4 + c + 1]
                ts("dve", xc, xl[:, c, 3:3 + N], cw(3), vecs[:, V_CB + c:V_CB + c + 1], ALU.mult, ALU.add,
                   [txl, tvec], [tlt])
                for j in range(3):
                    stt(xc, xl[:, c, j:j + N], cw(j), xc, ALU.mult, ALU.add, [txl, tvec, tlt], [tlt])
                cp("pool", xcb, xc, [tlt], [txcb])
                yield
                mm(psb[7][:, 0:N], wblk[:, 0, c, :], xcb, True, True, [twb, txcb], [pst[7]])
                act(rg, psb[7][:, 0:N], AF.Sigmoid, [pst[7], tvec], [tlt], bias=vecs[:, V_BA + c:V_BA + c + 1])
                mm(psb[7][:, 0:N], wblk[:, 1, c, :], xcb, True, True, [twb, txcb], [pst[7]])
                act(ig, psb[7][:, 0:N], AF.Sigmoid, [pst[7], tvec], [tlt], bias=vecs[:, V_BI + c:V_BI + c + 1])
                act(aa, rg, AF.Exp, [tlt, tder], [tlt], scale=der[:, D_CA + c:D_CA + c + 1])
                tt("dve", rg, aa, aa, ALU.mult, [tlt], [tlt])
                act(rg, rg, AF.Sqrt, [tlt], [tlt], bias=1.0, scale=-1.0)
                tt("dve", bb, ig, xc, ALU.mult, [tlt], [tlt])
                tt("dve", bb, bb, rg, ALU.mult, [tlt], [tlt])
                yield
                P.op("dve", lambda e, o=hs, a=aa, b=bb, s=lstate[:, c:c + 1]:
                     e.tensor_tensor_scan(out=o, data0=a, data1=b, initial=s, op0=ALU.mult, op1=ALU.add),
                     [tlt, tlst, tder], [tlt])
                cp("dve", lstate[:, c:c + 1], hs[:, N - 1:N], [tlt], [tlst])
                cp("dve", xl[:, c, 0:3], xl[:, c, N:N + 3], [txl, tlt], [txl])
                select_own(hso, thso, hs, tlt)
                yield
                g = glo[:, c, :]
                t1 = lro[:, c, :]
                tt("dve", t1, g, g, ALU.mult, [tglo], [tlro])
                ts("dve", t1, t1, 0.044715, 1.0, ALU.mult, ALU.add, [tlro], [tlro])
                tt("dve", t1, t1, g, ALU.mult, [tlro, tglo], [tlro])
                act(t1, t1, AF.Sigmoid, [tlro], [tlro], scale=1.5957691216057308)
                tt("dve", t1, t1, g, ALU.mult, [tlro, tglo], [tlro])
                tt("dve", t1, t1, hso, ALU.mult, [tlro, thso], [tlro])
                yield
            norm_mod(lro, tlro, mgt[:, 4:8, :], tmgt, vecs[:, V_LOG:V_LOG + 4], None, nk=4, n=128)
            dma("sp", mgd[:, 4:8, it * 128:(it + 1) * 128], mgt[:, 4:8, :], [tmgt], [])
            yield
        lru_bg = lru_gen()
    for _ in lru_bg:
        pass
    P.barrier()
    mark_a = cur[0]
    cur[0] = base_mark
    if stop == 'A':
        return finish()

    NKB = NA * 4
    KI = f32v(alloc(NKB * 128), NKB * 128)
    qown = bf16v(alloc(2 * NA * 128), 4 * NA * 128).rearrange("p (c n) -> p c n", c=4)
    Sb = f32v(alloc(NKB * 128), NKB * 128)
    maskb = [bf16v(alloc(NKB * 64), NKB * 128) for _ in range(4)]
    Rt = [f32v(alloc(512), 512) for _ in range(2)]
    qi = [f32v(alloc(1024), 1024).rearrange("p (h n) -> p h n", h=8) for _ in range(2)]
    kts = [bf16v(alloc(256), 512).rearrange("p (c n) -> p c n", c=4) for _ in range(4)]
    vts = [bf16v(alloc(260), 520).rearrange("p (h e) -> p h e", h=8) for _ in range(4)]
    mTs = [bf16v(alloc(128), 256) for _ in range(2)]
    Es = [bf16v(alloc(128), 256) for _ in range(4)]
    Pms = [bf16v(alloc(128), 256) for _ in range(4)]
    attn = f32v(alloc(512), 512)
    attnb = bf16v(alloc(256), 512)
    mgB = bf16v(alloc(256), 512).rearrange("p (c n) -> p c n", c=4)
    pm4 = f32v(alloc(512), 512)
    TbC = bf16v(alloc(5 * 8 * 64), 5 * 8 * 128).rearrange("p (a h q) -> p a h q", a=5, h=8)
    bis = f32v(alloc(32), 32)
    fin = f32v(alloc(32), 32)
    scr = f32v(alloc(8), 8)
    steps = f32v(alloc(16), 16)
    psb5b = psb[5][:, 0:256].bitcast(BF16)
    tKI, tqown, tS, tattn, tattnb, tbis, tsteps, tpm, tmgB, tTbC, tfin, tscr, tscr2 = [Tok() for _ in range(13)]
    tmask = [Tok() for _ in range(4)]
    tR = [Tok(), Tok()]
    tqi = [Tok(), Tok()]
    tkv = [Tok() for _ in range(4)]
    tmT = [Tok(), Tok()]
    tE = [Tok() for _ in range(4)]
    tPm = [Tok() for _ in range(4)]
    tST = [Tok() for _ in range(4)]
    dma("sp", KI[0:64, :], KId[:, 0:NKB * 128], [], [tKI])
    dma("sp", qown, qd[:, :, 0:NA * 128], [], [tqown])
    dma("sp", pm4, posm_d.rearrange("p k n -> p (k n)"), [], [tpm])
    for a in range(5):
        for h in range(8):
            ts("dve", TbC[:, a, h, :], Tb[:, 0, h, :], vecs[:, V_SD + a:V_SD + a + 1], None, ALU.mult, None,
               [tTb, tvec], [tTbC])
            stt(TbC[:, a, h, :], Tb[:, 1, h, :], vecs[:, V_SP + a:V_SP + a + 1], TbC[:, a, h, :],
                ALU.mult, ALU.add, [tTb, tvec, tTbC], [tTbC])

    def indexer(j):
        mslot = j % 4
        qs = j % 2
        nk = (j + 1) * 512
        dma("sp", qi[qs][0:64], qid[:, j, :, :], [], [tqi[qs]])
        for kc0 in range(0, nk, 512):
            for h in range(8):
                ib = (5, 7)[h % 2]
                mm(psb[ib][:, 0:512], qi[qs][0:64, h, :], KI[0:64, kc0:kc0 + 512], True, True,
                   [tqi[qs], tKI], [pst[ib]])
                R = Rt[h % 2]
                act(R, psb[ib][:, 0:512], AF.Relu, [pst[ib], tws], [tR[h % 2]],
                    scale=wabs[:, j * 8 + h:j * 8 + h + 1])
                sg = wsgn[:, j * 8 + h:j * 8 + h + 1]
                if h == 0:
                    ts("dve", Sb[:, kc0:kc0 + 512], R, sg, None, ALU.mult, None, [tR[h % 2], tws], [tS])
                else:
                    stt(Sb[:, kc0:kc0 + 512], R, sg, Sb[:, kc0:kc0 + 512], ALU.mult, ALU.add,
                        [tR[h % 2], tws, tS], [tS])
                yield
        P.op("dve", lambda e, o=bis[:, 0:1], i=Sb[:, 0:nk]:
             e.tensor_reduce(out=o, in_=i, axis=AX.X, op=ALU.max, apply_absolute_value=True),
             [tS], [tbis])
        base = j * 512
        tt("dve", Sb[:, base:base + 512], Sb[:, base:base + 512], pm4, ALU.add, [tS, tpm], [tS])
        hi0, lo, w0, mid, cnt, gs = [bis[:, i:i + 1] for i in range(1, 7)]
        ts("dve", hi0, bis[:, 0:1], 1.001, 1e-6, ALU.mult, ALU.add, [tbis], [tbis])
        ts("dve", lo, hi0, -1.0, None, ALU.mult, None, [tbis], [tbis])
        ts("dve", w0, hi0, 2.0, None, ALU.mult, None, [tbis], [tbis])
        ts("dve", steps, vecs[:, V_P2:V_P2 + 16], w0, None, ALU.mult, None, [tbis, tvec], [tsteps])
        yield
        junk = maskb[mslot][:, 0:nk]
        for k in range(NBIS):
            tt("dve", mid, lo, steps[:, k:k + 1], ALU.add, [tbis, tsteps], [tbis])
            ts("dve", junk, Sb[:, 0:nk], mid, None, ALU.is_ge, ALU.add, [tS, tbis], [tmask[mslot], tbis],
               accum=cnt)
            P.op("dve", lambda e: e.memset(scr[:, 0:1], 0.0), [tbis], [tbis])
            ts("dve", gs, cnt, 255.5, steps[:, k:k + 1], ALU.is_ge, ALU.mult, [tbis, tsteps], [tbis])
            tt("dve", lo, lo, gs, ALU.add, [tbis], [tbis])
            yield
        ts("dve", junk, Sb[:, 0:nk], lo, None, ALU.is_ge, None, [tS, tbis], [tmask[mslot]])
        yield

    def drain(gen, n=None):
        if gen is None:
            return
        try:
            if n is None:
                while True:
                    next(gen)
            else:
                for _ in range(n):
                    next(gen)
        except StopIteration:
            pass

    def chain(*gens):
        for g_ in gens:
            for _ in g_:
                yield

    def attention(j0, nq, bg):
        nkb_of = [(j0 + t + 1) * 4 for t in range(nq)]
        nkb = nkb_of[-1]
        first = [True] * 4
        c1 = nq * 128

        def ldkv(kb):
            s = kb % 4
            dma("sp", kts[s], Kd[:, :, kb * 128:(kb + 1) * 128], [], [tkv[s]])
            dma("sp", vts[s].rearrange("p h e -> p (h e)"), Vd[kb * 128:(kb + 1) * 128, :], [], [tkv[s]])

        def t0_of(kb):
            t0 = 0
            while kb >= nkb_of[t0]:
                t0 += 1
            return t0
        steps = [(kb, h) for kb in range(nkb) for h in range(8)]
        LAG = 2

        def front(i):
            kb, h = steps[i]
            s = kb % 4
            t0 = t0_of(kb)
            c0 = t0 * 128
            mT = mTs[kb % 2]
            if h == 0:
                if kb == 0:
                    ldkv(0)
                    ldkv(1)
                if kb + 2 < nkb:
                    ldkv(kb + 2)
                for t in range(t0, nq):
                    ms = (j0 + t) % 4
                    tr(psb5b[:, t * 128:(t + 1) * 128], maskb[ms][:, kb * 128:(kb + 1) * 128],
                       [tmask[ms], tvec], [pst[5]])
                cp("act", mT[:, c0:c1], psb5b[:, c0:c1], [pst[5]], [tmT[kb % 2]])
            po = (h % 2) * 64
            slot4 = i % 4
            pbank = (0, 6)[i % 2]
            pcol = 0
            ps_s = psb[pbank][:, pcol + c0:pcol + c1]
            mm(ps_s, kts[s][po:po + 64, h // 2, :],
               qown[po:po + 64, h // 2, j0 * 128 + c0:j0 * 128 + c1],
               True, True, [tkv[s], tqown], [pst[pbank]])
            for t in range(t0, nq):
                a = kb - 4 * (j0 + t) + 1
                if 0 <= a <= 4:
                    sl = psb[pbank][:, pcol + t * 128:pcol + (t + 1) * 128]
                    mm(sl, identb, TbC[:, a, h, :], False, True, [tTbC, tvec], [pst[pbank]])
            E = Es[slot4]
            act(E[:, c0:c1], ps_s, AF.Exp, [pst[pbank], tvec], [tE[slot4]],
                bias=vecs[:, V_RB + 15 * 8 + h:V_RB + 15 * 8 + h + 1], scale=0.125)
            Pm = Pms[slot4]
            tt("pool", Pm[:, c0:c1], E[:, c0:c1], mT[:, c0:c1], ALU.mult,
               [tE[slot4], tmT[kb % 2]], [tPm[slot4]])

        def back(i):
            kb, h = steps[i]
            s = kb % 4
            slot4 = i % 4
            Pm = Pms[slot4]
            for t in range(t0_of(kb), nq):
                bank = 1 + t * 2 + (h // 4)
                col = (h % 4) * 65
                st_ = first[t * 2 + h // 4]
                first[t * 2 + h // 4] = False
                P.op("pe", lambda e, o=psb[bank][:, col:col + 65], l=Pm[:, t * 128:(t + 1) * 128],
                     r=vts[s][:, h, :], s_=st_:
                     e.matmul(o, lhsT=l, rhs=r, start=s_, stop=False, skip_group_check=True),
                     [tPm[slot4], tkv[s]], [pst[bank]])

        for i in range(len(steps) + LAG):
            if i < len(steps):
                front(i)
            if i >= LAG:
                back(i - LAG)
            drain(bg, 1)
        for t in range(nq):
            jt = j0 + t
            for hh in range(2):
                bank = 1 + t * 2 + hh
                v = psb[bank][:, 0:260].rearrange("p (h e) -> p h e", h=4)
                rc = fin[:, 8 + hh * 4:12 + hh * 4]
                cp("dve", rc, v[:, :, 64], [pst[bank]], [tfin])
                P.op("dve", lambda e, o=rc: e.reciprocal(out=o, in_=o), [tfin], [tfin])
                for h4 in range(4):
                    h = hh * 4 + h4
                    ts("dve", attn[:, h * 64:(h + 1) * 64], v[:, h4, 0:64], fin[:, 8 + h:9 + h], None,
                       ALU.mult, None, [pst[bank], tfin], [tattn])
            act(attnb, attn, AF.Square, [tattn], [tattnb, tfin], accum=fin[:, 16:17])
            P.op("act", lambda e: e.copy(out=scr[:, 4:5], in_=scr[:, 5:6]), [tfin], [tfin])
            act(fin[:, 17:18], fin[:, 16:17], AF.Sqrt, [tfin], [tfin], bias=EPS, scale=1.0 / 512)
            P.op("dve", lambda e, o=fin[:, 17:18]: e.reciprocal(out=o, in_=o), [tfin], [tfin])
            ts("dve", attnb, attn, fin[:, 17:18], None, ALU.mult, None, [tattn, tfin, tattnb], [tattnb])
            for c in range(4):
                tr(psb5b[:, c * 128:(c + 1) * 128], attnb[:, c * 128:(c + 1) * 128], [tattnb, tvec], [pst[5]])
            for c in range(4):
                act(mgB[:, c, :], psb5b[:, c * 128:(c + 1) * 128], AF.Identity, [pst[5], tvec], [tmgB],
                    scale=vecs[:, V_AOG + c:V_AOG + c + 1])
            dma("sp", mgd[:, 0:4, jt * 128:(jt + 1) * 128], mgB, [tmgB], [])

    P.op("dve", lambda e: e.memset(scr, 0.0), [], [tbis])
    sbs = list(range(0, NA, 2))
    drain(chain(*[indexer(j) for j in range(sbs[0], min(sbs[0] + 2, NA))]))
    for si, j0 in enumerate(sbs):
        nq = min(2, NA - j0)
        bg = None
        if si + 1 < len(sbs):
            j1 = sbs[si + 1]
            bg = chain(*[indexer(j) for j in range(j1, min(j1 + 2, NA))])
        attention(j0, nq, bg)
        drain(bg)

    P.barrier()
    cur[0] = mark_a
    if stop == 'B':
        return finish()

    outT_v = outT.rearrange("(kc p) t -> p kc t", p=128)
    for ct in range(NA // 4):
        C0 = ct * N
        dma("sp", xt, x1d[:, :, C0:C0 + N], [], [txt])
        dma("sp", hb, mgd[:, :, C0:C0 + N], [], [thb])

        def ldo(dc):
            s = wislots[dc % 3]
            dma("sp", s[0], wout_t[dc], [tw["wout"]], [s[1]])
        ldo(0); ldo(1)
        for dc in range(8):
            if dc + 2 < 8:
                ldo(dc + 2)
            s = wislots[dc % 3]
            pb = 1 + dc % 2
            for kc in range(8):
                mm(psb[pb][:, 0:N], s[0][:, kc, :], hb[:, kc, :], kc == 0, kc == 7, [s[1], thb], [pst[pb]])
            cp("act" if dc % 2 == 0 else "dve", ybuf[:, dc, :], psb[pb][:, 0:N], [pst[pb]], [tyb])
        post_res(1, xt, txt)
        norm_mod(xt, txt, hb, thb, der[:, D_SC[2]:D_SC[2] + 8], der[:, D_SH[2]:D_SH[2] + 8])
        ffn2(1)
        post_res(2, xt, txt)
        dma("sp", outT_v[:, :, C0:C0 + N], xt, [txt], [])
    P.barrier()
    with nc.Block() as block:
        P.emit(block)
    st.close()
    return nc


def _t5_bucket_np(rel):
    rel = np.asarray(rel, np.int32)
    nb = 16
    max_exact = 8
    ret = np.where(rel > 0, nb, 0).astype(np.int32)
    n = np.abs(rel)
    nf = np.maximum(n, 1).astype(np.float32)
    large = max_exact + (np.log(nf / np.float32(max_exact)) / np.float32(math.log(128 / max_exact))
                         * np.float32(nb - max_exact)).astype(np.int32)
    large = np.minimum(large, nb - 1)
    return ret + np.where(n < max_exact, n, large)


_NC_CACHE = {}


def _host_inputs(inputs, NA=16):
    f = lambda a: np.ascontiguousarray(np.asarray(a, np.float32))
    x = f(inputs["x"]); c = f(inputs["c"])

    def col8(v):
        return v.reshape(8, 128).T

    def col4(v):
        return v.reshape(4, 128).T
    kl = np.arange(128)[:, None]; ql = np.arange(128)[None, :]
    bd = _t5_bucket_np(kl - ql)
    bp = _t5_bucket_np(kl - ql - 128)
    mb = np.zeros((128, 2, 32, 128), np.float32)
    for b in range(32):
        mb[:, 0, b, :] = (bd == b)
        mb[:, 1, b, :] = (bp == b)
    mb = mb.astype(ml_dtypes.bfloat16)
    ident = np.eye(128, dtype=np.float32).astype(ml_dtypes.bfloat16)
    wblk = np.zeros((128, 2, 4, 128), np.float32)
    for ai, w in enumerate((f(inputs["lru_w_a"])[0], f(inputs["lru_w_i"])[0])):
        for g in range(8):
            cch, half = g // 2, g % 2
            wblk[half * 64:(half + 1) * 64, ai, cch, half * 64:(half + 1) * 64] = w[g]
    shared = {
        "w_ada": f(inputs["w_ada"])[0],
        "wg1": f(inputs["ffn1_w_gate"])[0], "wu1": f(inputs["ffn1_w_up"])[0], "wd1": f(inputs["ffn1_w_down"])[0],
        "wg2": f(inputs["ffn2_w_gate"])[0], "wu2": f(inputs["ffn2_w_up"])[0], "wd2": f(inputs["ffn2_w_down"])[0],
        "w_in": f(inputs["w_in"])[0], "w_out": f(inputs["w_out"])[0],
        "wblk": wblk, "mb": mb, "ident": ident,
    }
    xTs = [np.ascontiguousarray(x[b].T) for b in range(2)]
    in_maps = []
    for core in range(8):
        b, r = core // 4, core % 4
        v = np.zeros((128, NV), np.float32)
        v[:, V_C:V_C + 8] = col8(c[b])
        v[:, V_BADA:V_BADA + 72] = f(inputs["b_ada"])[0].reshape(72, 128).T
        for i, nm in enumerate(["ffn1_pre_g", "ffn1_post_g", "mix_pre_g", "mix_post_g", "ffn2_pre_g", "ffn2_post_g"]):
            v[:, V_G + 8 * i:V_G + 8 * i + 8] = col8(f(inputs[nm])[0])
        v[:, V_AOG:V_AOG + 4] = col4(f(inputs["attn_out_g"])[0])
        v[:, V_LOG:V_LOG + 4] = col4(f(inputs["lru_out_g"])[0])
        cw = f(inputs["lru_conv_w"])[0]
        for j in range(4):
            v[:, V_CW + 4 * j:V_CW + 4 * j + 4] = col4(cw[j])
        v[:, V_CB:V_CB + 4] = col4(f(inputs["lru_conv_b"])[0])
        v[:, V_BA:V_BA + 4] = col4(f(inputs["lru_b_a"])[0])
        v[:, V_BI:V_BI + 4] = col4(f(inputs["lru_b_i"])[0])
        v[:, V_LAM:V_LAM + 4] = col4(f(inputs["lru_lambda"])[0])
        v[:, V_RB:V_RB + 256] = f(inputs["rel_bias"]).reshape(1, 256)
        v[:, V_P2:V_P2 + 16] = (0.5 ** np.arange(1, 17))[None, :]
        v[:, V_SEL + r] = 1.0
        v[:, V_SD + r + 1] = 1.0
        v[:, V_SP + r] = 1.0
        posm = np.zeros((128, 4, 128), np.float32)
        for kb in range(4):
            if kb > r:
                posm[:, kb, :] = -BIG
            elif kb == r:
                posm[0:64, kb, 64:128] = -BIG
        m = dict(shared)
        m["xT"] = xTs[b]
        m["vecs"] = v
        m["posm"] = posm
        in_maps.append(m)
    return in_maps


def kernel(**inputs):
    NA = 16
    if NA not in _NC_CACHE:
        _NC_CACHE[NA] = build(NA)
    nc = _NC_CACHE[NA]
    in_maps = _host_inputs(inputs, NA)
    res = run_bass_kernel_spmd(nc, in_maps, core_ids=list(range(8)))
    out = np.zeros((2, S, D), np.float32)
    for core in range(8):
        b, r = core // 4, core % 4
        o = np.asarray(res.results[core]["outT"], np.float32)
        o = o.T.reshape(NBLK, 128, D)
        for j in range(NBLK):
            g = 4 * j + r
            out[b, g * 128:(g + 1) * 128, :] = o[j]
    return out
```
